# Optimizing a Trainium2 kernel written in Bass

```python
import math
import jax, jax.numpy as jnp
from jax import lax
import numpy as np


D_MODEL = 1024
BATCH = 8
SEQ = 4096
DEPTH = 4

N_HEADS_DIFF = 8
HEAD_DIM_DIFF = 64
V_DIM_DIFF = 2 * HEAD_DIM_DIFF
Q_BLOCK = 128
N_HEADS_GLA = 4
KEY_DIM_GLA = D_MODEL // 2 // N_HEADS_GLA
VAL_DIM_GLA = D_MODEL // N_HEADS_GLA
GLA_RANK = 16
GLA_TAU = 16.0
GLA_CHUNK = 64
N_GROUPS = 4
EXPERTS_PER_GROUP = 8
N_EXPERTS = N_GROUPS * EXPERTS_PER_GROUP
TOP_K_IN_GROUP = 2
D_EXPERT = D_MODEL // 2
MOE_BLOCK = 128
LN_EPS = 1e-5
DEEPNORM_ALPHA = (2.0 * DEPTH) ** 0.25
DEEPNORM_BETA = (8.0 * DEPTH) ** -0.25
W_QA = N_HEADS_DIFF * 2 * HEAD_DIM_DIFF
W_KA = N_HEADS_DIFF * 2 * HEAD_DIM_DIFF
W_VA = N_HEADS_DIFF * V_DIM_DIFF
W_QB = N_HEADS_GLA * KEY_DIM_GLA
W_KB = N_HEADS_GLA * KEY_DIM_GLA
W_VB = N_HEADS_GLA * VAL_DIM_GLA
W_GB = N_HEADS_GLA * VAL_DIM_GLA
W_AB = GLA_RANK
W_GATES = 2 * D_MODEL
IN_WIDTHS = (W_QA, W_KA, W_VA, W_QB, W_KB, W_VB, W_GB, W_AB, W_GATES)
D_IN = W_QA + W_KA + W_VA + W_QB + W_KB + W_VB + W_GB + W_AB + W_GATES

kernel_name = 'hybrid_diffattn_gla_hmoe_deepnorm'


def _split_points():
    pts, acc = [], 0
    for w in IN_WIDTHS[:-1]:
        acc += w
        pts.append(acc)
    return pts


def _standardize_f32(x):
    xf = x.astype(jnp.float32)
    mu = jnp.mean(xf, axis=-1, keepdims=True)
    var = jnp.mean(jnp.square(xf - mu), axis=-1, keepdims=True)
    return (xf - mu) * lax.rsqrt(var + LN_EPS)


def _layer_norm(x, g, b):
    return (_standardize_f32(x) * g + b).astype(x.dtype)


def _rms_norm(x, g):
    xf = x.astype(jnp.float32)
    return xf * lax.rsqrt(jnp.mean(jnp.square(xf), axis=-1, keepdims=True) + LN_EPS) * g


def _modulate(x, shift, scale):
    return (_standardize_f32(x) * (1.0 + scale[:, None, :]) + shift[:, None, :]).astype(x.dtype)


def _alibi_slopes(n_heads):
    return jnp.asarray(2.0 ** (-8.0 * np.arange(1, n_heads + 1) / n_heads), dtype=jnp.float32)


def diff_attention(q, k, v, lam):
    slopes = _alibi_slopes(q.shape[1])
    scale = HEAD_DIM_DIFF ** -0.5
    n_blocks = q.shape[3] // Q_BLOCK
    outs = []
    for i in range(n_blocks):
        q0 = i * Q_BLOCK
        kv = q0 + Q_BLOCK
        s = jnp.einsum('bhcqd,bhckd->bhcqk', q[:, :, :, q0:kv], k[:, :, :, :kv]).astype(jnp.float32) * scale
        dist = (jnp.arange(q0, kv)[:, None] - jnp.arange(kv)[None, :]).astype(jnp.float32)
        bias = jnp.where(dist >= 0, -slopes[:, None, None] * dist, -jnp.inf)
        p = jax.nn.softmax(s + bias[None, :, None], axis=-1)
        a = (p[:, :, 0] - lam * p[:, :, 1]).astype(v.dtype)
        outs.append(jnp.einsum('bhqk,bhkd->bhqd', a, v[:, :, :kv]))
    return jnp.concatenate(outs, axis=2)


def gla_chunked(q, k, v, log_a):
    b_, h_, s_, dk = q.shape
    dv = v.shape[-1]
    n_chunks = s_ // GLA_CHUNK

    def to_chunks(t):
        return jnp.moveaxis(t.astype(jnp.float32).reshape(b_, h_, n_chunks, GLA_CHUNK, t.shape[-1]), 2, 0)

    causal = jnp.tril(jnp.ones((GLA_CHUNK, GLA_CHUNK), dtype=bool))

    def step(state, inp):
        qc, kc, vc, ac = inp
        cum = jnp.cumsum(ac, axis=2)
        o_inter = jnp.einsum('bhtk,bhkv->bhtv', qc * jnp.exp(cum), state)
        diff = cum[:, :, :, None, :] - cum[:, :, None, :, :]
        decay = jnp.exp(jnp.where(causal[:, :, None], diff, -jnp.inf))
        att = jnp.einsum('bhtk,bhsk,bhtsk->bhts', qc, kc, decay)
        o_intra = jnp.einsum('bhts,bhsv->bhtv', att, vc)
        last = cum[:, :, -1:, :]
        new_state = jnp.exp(last)[:, :, 0, :, None] * state + jnp.einsum('bhsk,bhsv->bhkv', kc * jnp.exp(last - cum), vc)
        return new_state, o_inter + o_intra

    state0 = jnp.zeros((b_, h_, dk, dv), jnp.float32)
    _, o = lax.scan(step, state0, (to_chunks(q), to_chunks(k), to_chunks(v), to_chunks(log_a)))
    return jnp.moveaxis(o, 0, 2).reshape(b_, h_, s_, dv)


def token_mixer(u, layer_idx, w_in, b_gates, w_alpha, b_alpha, lq1, lk1, lq2, lk2,
                diff_g, gla_g, w_ba, w_bb, w_out):
    b_, s_, _ = u.shape
    proj = u @ w_in
    qa, ka, va, qb, kb, vb, gb, ab, gates = jnp.split(proj, _split_points(), axis=-1)

    qa = qa.reshape(b_, s_, N_HEADS_DIFF, 2, HEAD_DIM_DIFF).transpose(0, 2, 3, 1, 4)
    ka = ka.reshape(b_, s_, N_HEADS_DIFF, 2, HEAD_DIM_DIFF).transpose(0, 2, 3, 1, 4)
    va = va.reshape(b_, s_, N_HEADS_DIFF, V_DIM_DIFF).transpose(0, 2, 1, 3)
    lam_init = 0.8 - 0.6 * math.exp(-0.3 * layer_idx)
    lam = (jnp.exp(jnp.sum(lq1.astype(jnp.float32) * lk1)) - jnp.exp(jnp.sum(lq2.astype(jnp.float32) * lk2)) + lam_init)
    oa = diff_attention(qa, ka, va, lam)
    oa = (_rms_norm(oa, diff_g) * (1.0 - lam_init)).astype(u.dtype)
    oa = oa.transpose(0, 2, 1, 3).reshape(b_, s_, W_VA)

    log_a = jax.nn.log_sigmoid((ab @ w_alpha + b_alpha).astype(jnp.float32)) / GLA_TAU
    heads_k = lambda t: t.reshape(b_, s_, N_HEADS_GLA, -1).transpose(0, 2, 1, 3)
    ob = gla_chunked(heads_k(qb * (KEY_DIM_GLA ** -0.5)), heads_k(kb), heads_k(vb), heads_k(log_a))
    ob = _rms_norm(ob, gla_g).transpose(0, 2, 1, 3).reshape(b_, s_, W_VB)
    ob = (ob * jax.nn.silu(gb.astype(jnp.float32))).astype(u.dtype)

    gate_a, gate_b = jnp.split(jax.nn.sigmoid(gates + b_gates), 2, axis=-1)
    mixed = gate_a * (oa @ w_ba) + gate_b * (ob @ w_bb)
    return mixed @ w_out


def hier_moe(u, w_rg, b_rg, w_re, b_re, wg, wu, wd):
    b_, s_, d_ = u.shape
    n_tok = b_ * s_
    uf = u.reshape(n_tok, d_)
    g_logits = (uf @ w_rg + b_rg).astype(jnp.float32)
    g_prob = jax.nn.softmax(g_logits, axis=-1)
    g_idx = jnp.argmax(g_logits, axis=-1).astype(jnp.int32)
    g_w = jnp.take_along_axis(g_prob, g_idx[:, None], axis=1)[:, 0]
    e_logits = (uf @ w_re + b_re).astype(jnp.float32).reshape(n_tok, N_GROUPS, EXPERTS_PER_GROUP)
    e_in = jnp.take_along_axis(e_logits, g_idx[:, None, None], axis=1)[:, 0]
    top_v, top_i = lax.top_k(e_in, TOP_K_IN_GROUP)
    weights = jax.nn.softmax(top_v, axis=-1) * g_w[:, None]

    expert_id = (g_idx[:, None] * EXPERTS_PER_GROUP + top_i).reshape(-1).astype(jnp.int32)
    token_id = jnp.repeat(jnp.arange(n_tok, dtype=jnp.int32), TOP_K_IN_GROUP)
    weight = weights.reshape(-1)
    n_assign = n_tok * TOP_K_IN_GROUP

    order = jnp.argsort(expert_id)
    se, st, sw = expert_id[order], token_id[order], weight[order]
    counts = jnp.bincount(expert_id, length=N_EXPERTS).astype(jnp.int32)
    starts = jnp.cumsum(counts) - counts
    padded = ((counts + MOE_BLOCK - 1) // MOE_BLOCK) * MOE_BLOCK
    pends = jnp.cumsum(padded)
    pstarts = pends - padded
    dest = pstarts[se] + (jnp.arange(n_assign, dtype=jnp.int32) - starts[se])
    n_rows = ((n_assign + MOE_BLOCK - 1) // MOE_BLOCK) * MOE_BLOCK + N_EXPERTS * MOE_BLOCK
    n_blocks = n_rows // MOE_BLOCK
    buf_tok = jnp.full((n_rows,), n_tok, jnp.int32).at[dest].set(st)
    buf_w = jnp.zeros((n_rows,), jnp.float32).at[dest].set(sw)
    blk_start = jnp.arange(n_blocks, dtype=jnp.int32) * MOE_BLOCK
    blk_e = jnp.minimum(jnp.searchsorted(pends, blk_start, side='right'), N_EXPERTS - 1).astype(jnp.int32)

    u_pad = jnp.concatenate([uf, jnp.zeros((1, d_), uf.dtype)], axis=0)
    xb = u_pad[buf_tok].reshape(n_blocks, MOE_BLOCK, d_)

    def expert_block(args):
        xblk, e = args
        h = jax.nn.silu(xblk @ wg[e]) * (xblk @ wu[e])
        return h @ wd[e]

    yb = lax.map(expert_block, (xb, blk_e)).reshape(n_rows, d_)
    y = jax.ops.segment_sum(yb.astype(jnp.float32) * buf_w[:, None], buf_tok, num_segments=n_tok + 1)[:n_tok]
    return y.astype(u.dtype).reshape(b_, s_, d_)


def setup_inputs(seed: int = 0) -> dict:
    key = jax.random.key(seed)
    ks = jax.random.split(key, 32)
    f32 = jnp.float32

    def nrm(k, shape, scale):
        return jax.random.normal(k, shape, f32) * scale

    d = D_MODEL
    col_scale = jnp.concatenate([
        jnp.ones((W_QA + W_KA,), f32), jnp.full((W_VA,), DEEPNORM_BETA, f32),
        jnp.ones((W_QB + W_KB,), f32), jnp.full((W_VB,), DEEPNORM_BETA, f32),
        jnp.ones((W_GB + W_AB + W_GATES,), f32)])
    return {
        'x': nrm(ks[0], (BATCH, SEQ, d), 1.0),
        'c': nrm(ks[1], (BATCH, d), 1.0),
        'w_ada': nrm(ks[2], (DEPTH, d, 6 * d), d ** -0.5),
        'b_ada': nrm(ks[3], (DEPTH, 6 * d), 0.02),
        'w_in': nrm(ks[4], (DEPTH, d, D_IN), d ** -0.5) * col_scale,
        'b_gates': nrm(ks[5], (DEPTH, W_GATES), 0.02),
        'w_alpha': nrm(ks[6], (DEPTH, GLA_RANK, W_QB), GLA_RANK ** -0.5),
        'b_alpha': nrm(ks[7], (DEPTH, W_QB), 0.02),
        'lambda_q1': nrm(ks[8], (DEPTH, HEAD_DIM_DIFF), 0.1),
        'lambda_k1': nrm(ks[9], (DEPTH, HEAD_DIM_DIFF), 0.1),
        'lambda_q2': nrm(ks[10], (DEPTH, HEAD_DIM_DIFF), 0.1),
        'lambda_k2': nrm(ks[11], (DEPTH, HEAD_DIM_DIFF), 0.1),
        'diff_norm_g': 1.0 + nrm(ks[12], (DEPTH, V_DIM_DIFF), 0.02),
        'gla_norm_g': 1.0 + nrm(ks[13], (DEPTH, VAL_DIM_GLA), 0.02),
        'w_branch_a': nrm(ks[14], (DEPTH, W_VA, d), W_VA ** -0.5),
        'w_branch_b': nrm(ks[15], (DEPTH, W_VB, d), W_VB ** -0.5),
        'w_out': nrm(ks[16], (DEPTH, d, d), d ** -0.5 * DEEPNORM_BETA),
        'ln1_g': 1.0 + nrm(ks[17], (DEPTH, d), 0.02),
        'ln1_b': nrm(ks[18], (DEPTH, d), 0.02),
        'w_router_g': nrm(ks[19], (DEPTH, d, N_GROUPS), d ** -0.5),
        'b_router_g': nrm(ks[20], (DEPTH, N_GROUPS), 0.01),
        'w_router_e': nrm(ks[21], (DEPTH, d, N_EXPERTS), d ** -0.5),
        'b_router_e': nrm(ks[22], (DEPTH, N_EXPERTS), 0.01),
        'w_gate_e': nrm(ks[23], (DEPTH, N_EXPERTS, d, D_EXPERT), d ** -0.5),
        'w_up_e': nrm(ks[24], (DEPTH, N_EXPERTS, d, D_EXPERT), d ** -0.5 * DEEPNORM_BETA),
        'w_down_e': nrm(ks[25], (DEPTH, N_EXPERTS, D_EXPERT, d), D_EXPERT ** -0.5 * DEEPNORM_BETA),
        'ln2_g': 1.0 + nrm(ks[26], (DEPTH, d), 0.02),
        'ln2_b': nrm(ks[27], (DEPTH, d), 0.02),
    }


def reference(x, c, w_ada, b_ada, w_in, b_gates, w_alpha, b_alpha, lambda_q1, lambda_k1,
              lambda_q2, lambda_k2, diff_norm_g, gla_norm_g, w_branch_a, w_branch_b, w_out,
              ln1_g, ln1_b, w_router_g, b_router_g, w_router_e, b_router_e, w_gate_e, w_up_e,
              w_down_e, ln2_g, ln2_b):
    cond = jax.nn.silu(c)
    for l in range(DEPTH):
        ada = cond @ w_ada[l] + b_ada[l]
        sh1, sc1, g1, sh2, sc2, g2 = jnp.split(ada, 6, axis=-1)
        u = _modulate(x, sh1, sc1)
        y = token_mixer(u, l, w_in[l], b_gates[l], w_alpha[l], b_alpha[l],
                        lambda_q1[l], lambda_k1[l], lambda_q2[l], lambda_k2[l],
                        diff_norm_g[l], gla_norm_g[l], w_branch_a[l], w_branch_b[l], w_out[l])
        x = _layer_norm(DEEPNORM_ALPHA * x + g1[:, None, :] * y, ln1_g[l], ln1_b[l])
        u = _modulate(x, sh2, sc2)
        y = hier_moe(u, w_router_g[l], b_router_g[l], w_router_e[l], b_router_e[l],
                     w_gate_e[l], w_up_e[l], w_down_e[l])
        x = _layer_norm(DEEPNORM_ALPHA * x + g2[:, None, :] * y, ln2_g[l], ln2_b[l])
    return x
```

```python
import math
from contextlib import ExitStack
import numpy as np
import concourse.bass as bass
import concourse.mybir as mybir
from concourse.bass_utils import run_bass_kernel_spmd

F32 = mybir.dt.float32
BF16 = mybir.dt.bfloat16
I32 = mybir.dt.int32
AF = mybir.ActivationFunctionType
ALU = mybir.AluOpType
AX = mybir.AxisListType

D = 1024
DEPTH = 4
NH_A = 8
NH_B = 4
DK_B = 128
DV_B = 256
RANK = 16
TAU = 16.0
CH = 64
NG = 4
EPG = 8
NE = 32
DEXP = 512
EPS = 1e-5
ALPHA = (2.0 * DEPTH) ** 0.25
D_IN = 8208
C_QA, C_KA, C_VA, C_QB, C_KB, C_VB, C_GB, C_AB, C_GT = 0, 1024, 2048, 3072, 3584, 4096, 5120, 6144, 6160
BLK = 256

ENGINES = ("pe", "act", "dve", "pool", "sp")
SEM_ROT = 30000


class Buf:
    __slots__ = ("name", "lw", "readers", "dcount")

    def __init__(self, name):
        self.name = name
        self.lw = None
        self.readers = []
        self.dcount = 0


class Ins:
    __slots__ = ("eng", "fn", "deps", "is_dma", "sig", "tok", "part", "dbuf", "barrier")

    def __init__(self, eng, fn, is_dma=False, part=False):
        self.eng = eng
        self.fn = fn
        self.deps = []
        self.is_dma = is_dma
        self.sig = False
        self.tok = None
        self.part = part
        self.dbuf = None
        self.barrier = None


class Prog:
    def __init__(self):
        self.ins = []
        self.bufs = []
        self.last_on = {e: None for e in ENGINES}

    def buf(self, name="b"):
        b = Buf(name)
        self.bufs.append(b)
        return b

    def _deps(self, I, reads, writes):
        deps = []
        for b in reads:
            if b.lw is not None:
                deps.append((b.lw, "raw"))
        for b in writes:
            if b.lw is not None:
                if not (I.is_dma and I.part and b.lw.is_dma and b.lw.part and not b.readers):
                    deps.append((b.lw, "waw"))
            for r in b.readers:
                deps.append((r, "war"))
        out = []
        seen = set()
        for d, kind in deps:
            if d is I or id(d) in seen:
                continue
            if (not d.is_dma) and (not I.is_dma) and d.eng == I.eng:
                if I.eng == "pe":
                    continue
                if kind == "war":
                    continue
            seen.add(id(d))
            out.append(d)
        for d in out:
            d.sig = True
        I.deps = out
        for b in reads:
            b.readers.append(I)
        for b in writes:
            b.lw = I
            b.readers = []

    def op(self, eng, fn, reads=(), writes=()):
        I = Ins(eng, fn)
        self._deps(I, list(reads), list(writes))
        self.ins.append(I)
        self.last_on[eng] = I
        return I

    def dma(self, eng, fn, reads=(), writes=(), part=False, sem=None):
        writes = list(writes)
        assert len(writes) == 1
        I = Ins(eng, fn, is_dma=True, part=part)
        I.dbuf = sem if sem is not None else writes[0]
        self._deps(I, list(reads), writes)
        self.ins.append(I)
        return I

    def barrier(self):
        lasts = [self.last_on[e] for e in ENGINES if self.last_on[e] is not None]
        for d in lasts:
            d.sig = True
        for e in ENGINES:
            I = Ins(e, None)
            I.barrier = (list(lasts), None)
            self.ins.append(I)
        for b in self.bufs:
            b.lw = None
            b.readers = []

    def emit(self, nc, stack):
        esems = {e: [] for e in ENGINES}
        ecount = {e: 0 for e in ENGINES}
        nsem = [0]
        recs = []
        free = []
        brec = {}
        last_use = {}
        for idx, I in enumerate(self.ins):
            if I.is_dma:
                last_use[id(I.dbuf)] = idx

        def new_sem(name):
            nsem[0] += 1
            return stack.enter_context(nc.semaphore(name))

        for idx, I in enumerate(self.ins):
            if I.barrier is not None:
                I.barrier = (I.barrier[0], [(r[0], r[1]) for r in recs if r[1] > 0])
                for bid in list(brec.keys()):
                    if last_use.get(bid, -1) < idx:
                        free.append(brec.pop(bid))
                continue
            if I.is_dma:
                b = I.dbuf
                if id(b) not in brec:
                    if free:
                        brec[id(b)] = free.pop()
                    else:
                        r = [new_sem(f"d{nsem[0]}"), 0]
                        recs.append(r)
                        brec[id(b)] = r
                r = brec[id(b)]
                r[1] += 16
                I.tok = (r[0], r[1])
            elif I.sig:
                e = I.eng
                if not esems[e] or ecount[e] >= SEM_ROT:
                    esems[e].append(new_sem(f"e_{e}{len(esems[e])}"))
                    ecount[e] = 0
                ecount[e] += 1
                I.tok = (esems[e][-1], ecount[e])
        self.n_sems = nsem[0]
        streams = {e: [I for I in self.ins if I.eng == e] for e in ENGINES}

        def run_stream(e, eng):
            waited = {}

            def wait(tok):
                sem, val = tok
                k = id(sem)
                if waited.get(k, 0) >= val:
                    return
                waited[k] = val
                eng.wait_ge(sem, val)

            for I in streams[e]:
                if I.barrier is not None:
                    lasts, dcounts = I.barrier
                    for d in lasts:
                        if d.tok is not None and not (d.eng == e and e == "pe"):
                            wait(d.tok)
                    for hs, cnt in dcounts:
                        wait((hs, cnt))
                    continue
                for d in I.deps:
                    wait(d.tok)
                r = I.fn(eng)
                if I.tok is not None:
                    r.then_inc(I.tok[0], 16 if I.is_dma else 1)

        with nc.Block() as block:
            @block.tensor
            def _(eng):
                run_stream("pe", eng)

            @block.scalar
            def _(eng):
                run_stream("act", eng)

            @block.vector
            def _(eng):
                run_stream("dve", eng)

            @block.gpsimd
            def _(eng):
                run_stream("pool", eng)

            @block.sync
            def _(eng):
                run_stream("sp", eng)


class Arena:
    def __init__(self, ap2d, n):
        self.ap = ap2d
        self.n = n
        self.off = 0
        self.mark = 0

    def reset(self):
        self.off = self.mark

    def keep(self):
        self.mark = self.off

    def push(self):
        self.stack = getattr(self, "stack", [])
        self.stack.append(self.mark)
        self.mark = self.off

    def pop(self):
        self.mark = self.stack.pop()

    def alloc(self, n, parts=128):
        assert self.off + n <= self.n, f"arena overflow {self.off}+{n}>{self.n}"
        a = self.ap[0:parts, self.off:self.off + n]
        self.off += n
        return a

    def alloc3(self, a, b, parts=128):
        return self.alloc(a * b, parts).rearrange("p (a b) -> p a b", b=b)


class K:
    pass


def slopes():
    return [2.0 ** (-8.0 * (i + 1) / NH_A) for i in range(NH_A)]


def host_consts(S):
    c = {}
    c["ident"] = np.eye(128, dtype=np.float32)
    ki = np.arange(128)[:, None]
    qi = np.arange(128)[None, :]
    c["tri"] = (qi >= ki).astype(np.float32)
    c["ustrict"] = (ki < qi).astype(np.float32)
    s64 = np.arange(64)[:, None]
    t64 = np.arange(64)[None, :]
    c["gmask"] = (s64 <= t64).astype(np.float32)
    qpos = np.arange(S) % 512
    c["qaug"] = np.stack([qpos // 16, qpos % 16, np.ones(S)]).astype(np.float32)
    kpos = np.arange(S) % 128
    sl = np.array(slopes())
    c["kaug"] = np.stack([np.stack([np.full(S, -128.0 * s), np.full(S, -8.0 * s), 8.0 * s * kpos])
                          for s in sl]).astype(np.float32)
    dl = np.arange(-3, 33)
    c["biascol"] = np.broadcast_to((-sl[:, None] * 128.0 * dl[None, :]).reshape(1, -1), (128, 8 * 36)).astype(np.float32).copy()
    return c


def build(S, L=DEPTH, dbg=(), phases="all"):
    nc = bass.Bass("TRN2", target_bir_lowering=False)
    NT = S // 128
    NQB = S // 512
    NCH = S // CH
    NSLOT = ((2 * S + NE * (BLK - 1)) // BLK + 1) * BLK
    NBLK = NSLOT // BLK
    P = Prog()
    st = ExitStack()

    def inp(name, shape, dt=F32):
        return nc.dram_tensor(name, list(shape), dt, kind="ExternalInput")

    def scratch(name, shape, dt):
        kind = "ExternalOutput" if name in dbg else "Internal"
        return nc.dram_tensor(name, list(shape), dt, kind=kind)

    x_in = inp("x", [S, D])
    cT = inp("cT", [128, 8])
    w_ada = inp("w_ada", [DEPTH, D, 6 * D])
    b_ada = inp("b_ada", [DEPTH, 6 * D])
    w_in = inp("w_in", [DEPTH, D, D_IN])
    b_gatesT = inp("b_gatesT", [DEPTH, 128, 16])
    w_alpha = inp("w_alpha", [DEPTH, RANK, 512])
    b_alphaT = inp("b_alphaT", [DEPTH, 128, 4])
    lam_in = inp("lam4", [DEPTH, 4, 64])
    diff_g = inp("diff_norm_g", [DEPTH, 128])
    gla_g = inp("gla_norm_g", [DEPTH, 256])
    w_ba = inp("w_branch_a", [DEPTH, D, D])
    w_bb = inp("w_branch_b", [DEPTH, D, D])
    w_out = inp("w_out", [DEPTH, D, D])
    ln1_g = inp("ln1_g", [DEPTH, D])
    ln1_b = inp("ln1_b", [DEPTH, D])
    w_r = inp("w_router", [DEPTH, D, 36])
    b_r = inp("b_router", [DEPTH, 36])
    w_ge = inp("w_gate_e", [DEPTH * NE * 128, 8 * DEXP])
    w_ue = inp("w_up_e", [DEPTH * NE * 128, 8 * DEXP])
    w_de = inp("w_down_e", [DEPTH * NE * 128, 4 * D])
    ln2_g = inp("ln2_g", [DEPTH, D])
    ln2_b = inp("ln2_b", [DEPTH, D])
    c_ident = inp("ident", [128, 128])
    c_tri = inp("tri", [128, 128])
    c_ustrict = inp("ustrict", [128, 128])
    c_gmask = inp("gmask", [64, 64])
    c_qaug = inp("qaug", [3, S])
    c_kaug = inp("kaug", [8, 3, S])
    c_biascol = inp("biascol", [128, 8 * 36])
    c_iota = inp("iota_p", [128, 1])
    out = nc.dram_tensor("out", [S, D], F32, kind="ExternalOutput")

    xA = scratch("xA", [S, D], F32)
    xB = scratch("xB", [S, D], F32)
    adaD = scratch("adaD", [DEPTH, 6 * D], F32)
    qaT = scratch("qaT", [1024, S], BF16)
    kaT = scratch("kaT", [1024, S], BF16)
    qbT = scratch("qbT", [512, S], BF16)
    kbT = scratch("kbT", [512, S], BF16)
    abT = scratch("abT", [16, S], BF16)
    gtT = scratch("gtT", [2048, S], BF16)
    va = scratch("va", [S, 1024], BF16)
    vb = scratch("vb", [S, 1024], BF16)
    gb = scratch("gb", [S, 1024], BF16)
    oaT = scratch("oaT", [1024, S], BF16)
    obT = scratch("obT", [1024, S], BF16)
    u2d = scratch("u2d", [S, D], BF16)
    XB = scratch("XB", [NSLOT, D], BF16)
    YB = scratch("YB", [NSLOT, D], F32)

    NF, NBF, NI = 22016, 57 * 1024, 1024
    arf = Arena(st.enter_context(nc.sbuf_tensor("arena_f", [128, NF], F32))[:, :], NF)
    arb = Arena(st.enter_context(nc.sbuf_tensor("arena_b", [128, NBF], BF16))[:, :], NBF)
    ari = Arena(st.enter_context(nc.sbuf_tensor("arena_i", [128, NI], I32))[:, :], NI)
    psf_t = st.enter_context(nc.psum_tensor("psf", [128, 6, 512], F32))
    psb_t = st.enter_context(nc.psum_tensor("psb", [128, 2, 1024], BF16))
    psf = [psf_t[:, i, :] for i in range(6)]
    psb = [psb_t[:, i, :] for i in range(2)]
    Bpsf = [P.buf(f"psf{i}") for i in range(6)]
    Bpsb = [P.buf(f"psb{i}") for i in range(2)]

    def B(name="b"):
        return P.buf(name)

    def mm(o, lhsT, rhs, start, stop, reads, writes):
        P.op("pe", lambda e: e.matmul(o, lhsT=lhsT, rhs=rhs, start=start, stop=stop), reads, writes)

    def tr(o, in_, ident, reads, writes):
        P.op("pe", lambda e: e.transpose(o, in_, ident), reads, writes)

    def act(o, in_, func, reads, writes, bias=None, scale=None, accum_out=None):
        kw = {}
        if bias is not None:
            kw["bias"] = bias
        if scale is not None:
            kw["scale"] = scale
        if accum_out is not None:
            kw["accum_out"] = accum_out
        P.op("act", lambda e: e.activation(out=o, in_=in_, func=func, **kw), reads, writes)

    def tt(eng, o, in0, in1, op, reads, writes):
        P.op(eng, lambda e: e.tensor_tensor(out=o, in0=in0, in1=in1, op=op), reads, writes)

    def ts(eng, o, in0, s1, s2, op0, op1, reads, writes, accum_out=None):
        kw = {}
        if accum_out is not None:
            kw["accum_out"] = accum_out
        if s2 is None:
            P.op(eng, lambda e: e.tensor_scalar(out=o, in0=in0, scalar1=s1, scalar2=None, op0=op0, **kw), reads, writes)
        else:
            P.op(eng, lambda e: e.tensor_scalar(out=o, in0=in0, scalar1=s1, scalar2=s2, op0=op0, op1=op1, **kw), reads, writes)

    def stt(eng, o, in0, scalar, in1, op0, op1, reads, writes):
        P.op(eng, lambda e: e.scalar_tensor_tensor(out=o, in0=in0, scalar=scalar, in1=in1, op0=op0, op1=op1), reads, writes)

    def cp(eng, o, in_, reads, writes):
        if eng == "act":
            P.op("act", lambda e: e.activation(out=o, in_=in_, func=AF.Copy), reads, writes)
        else:
            P.op(eng, lambda e: e.tensor_copy(out=o, in_=in_), reads, writes)

    def ld(q, o, in_, reads, writes, part=False, sem=None):
        P.dma(q, lambda e: e.dma_start(out=o, in_=in_), reads, writes, part=part, sem=sem)

    def rmax(o, in_, reads, writes):
        P.op("dve", lambda e: e.reduce_max(out=o, in_=in_, axis=AX.X), reads, writes)

    def rsum(o, in_, reads, writes):
        P.op("dve", lambda e: e.reduce_sum(out=o, in_=in_, axis=AX.X), reads, writes)

    def recip(o, in_, reads, writes):
        P.op("dve", lambda e: e.reciprocal(out=o, in_=in_), reads, writes)

    def memset(eng, o, val, writes):
        P.op(eng, lambda e: e.memset(o, val), (), writes)

    epsc = None

    def rsqrt_(o, in_, scale, Bt):
        act(o, in_, AF.Sqrt, [Bt, Bc2[7]], [Bt], bias=epsc[0:o.shape[0], :], scale=scale)
        P.op("dve", lambda e: e.reciprocal(out=o, in_=o), [Bt], [Bt])

    def ln_stats(xt, Bx, tmpf, Bt, tag=""):
        stats = tmpf[:, 0:12].rearrange("p (a b) -> p a b", b=6)
        mv = tmpf[:, 12:14]
        for hh in range(2):
            P.op("dve", (lambda h2: (lambda e: e.bn_stats(out=stats[:, h2, :], in_=xt[:, h2 * 512:(h2 + 1) * 512])))(hh), [Bx], [Bt])
        P.op("dve", lambda e: e.bn_aggr(out=mv, in_=stats), [Bt], [Bt])
        rstd = tmpf[:, 14:15]
        nmr = tmpf[:, 15:16]
        rsqrt_(rstd, mv[:, 1:2], 1.0, Bt)
        stt("dve", nmr, mv[:, 0:1], -1.0, rstd, ALU.mult, ALU.mult, [Bt], [Bt])
        return rstd, nmr

    ident_b = arb.alloc(128)
    ident_f = arf.alloc(128)
    tri_b = arb.alloc(128)
    ustr_b = arb.alloc(128)
    ones_b = arb.alloc(128)
    gmask_f = arf.alloc(64, parts=64)
    biascol = arf.alloc(8 * 36)
    iota_p = arf.alloc(1)
    Bconst = B("const")
    Bc2 = [B("c2_%d" % i) for i in range(8)]
    ld("pool", ident_b, c_ident[:, :], [], [Bc2[0]])
    ld("sp", ident_f, c_ident[:, :], [], [Bc2[1]])
    ld("pool", tri_b, c_tri[:, :], [], [Bc2[2]])
    ld("pool", ustr_b, c_ustrict[:, :], [], [Bc2[3]])
    ld("sp", gmask_f, c_gmask[:, :], [], [Bc2[4]])
    ld("sp", biascol, c_biascol[:, :], [], [Bc2[5]])
    ld("sp", iota_p, c_iota[:, :], [], [Bc2[6]])
    memset("dve", ones_b, 1.0, [Bc2[7]])
    epsc = arf.alloc(1)
    memset("dve", epsc, EPS, [Bc2[7]])
    arf.keep(); arb.keep(); ari.keep()
    P.barrier()
    CONST = Bc2

    def phase_reset():
        P.barrier()
        arf.reset(); arb.reset(); ari.reset()

    def phase_ada():
        ct = arf.alloc(8)
        condT = arf.alloc(8)
        row = arf.alloc(6 * D, parts=1)
        brow = arf.alloc(6 * D, parts=1)
        wt = [arf.alloc3(8, 512) for _ in range(2)]
        Bct, Brow, Bbrow = B(), B(), B()
        Bwt = [B(), B()]
        ld("sp", ct, cT[:, :], [], [Bct])
        act(condT, ct, AF.Silu, [Bct], [Bct])
        for l in range(L):
            ld("sp", brow, b_ada[l:l + 1, :], [], [Bbrow])
            for nb in range(12):
                w = wt[nb % 2]
                Bw = Bwt[nb % 2]
                ld("sp", w, w_ada[l, :, nb * 512:(nb + 1) * 512].rearrange("(kc p) n -> p kc n", p=128), [], [Bw])
                for kc in range(8):
                    mm(psf[nb % 2][0:1, :], condT[:, kc:kc + 1], w[:, kc, :], kc == 0, kc == 7, [Bct, Bw], [Bpsf[nb % 2]])
                tt("dve", row[:, nb * 512:(nb + 1) * 512], psf[nb % 2][0:1, :], brow[:, nb * 512:(nb + 1) * 512], ALU.add,
                   [Bpsf[nb % 2], Bbrow], [Brow])
            for c0 in (1024, 4096):
                ts("dve", row[:, c0:c0 + 1024], row[:, c0:c0 + 1024], 1.0, None, ALU.add, None, [Brow], [Brow])
            ld("sp", adaD[l:l + 1, :], row, [Brow], [Bada], part=True, sem=Brow)

    Bada = B("adaD")

    def phase_proj(l, xsrc):
        uT = arb.alloc3(8, S)
        BuTs = [B("uT") for _ in range(NQB)]
        sc = arf.alloc(D)
        sh = arf.alloc(D)
        Bmod = B("mod")
        ld("sp", sc, adaD[l:l + 1, 1024:2048].partition_broadcast(128), [Bada], [Bmod], part=True)
        ld("sp", sh, adaD[l:l + 1, 0:1024].partition_broadcast(128), [Bada], [Bmod], part=True)
        xt = [arf.alloc(D) for _ in range(2)]
        Bxt = [B(), B()]
        xn = [arf.alloc(D) for _ in range(2)]
        Bxn = [B(), B()]
        ub = [arb.alloc(D) for _ in range(2)]
        Bub = [B(), B()]
        tmp = [arf.alloc(16) for _ in range(2)]
        Btmp = [B(), B()]
        Bx = B("xsrc")
        for t in range(NT):
            i = t % 2
            ld("sp", xt[i], xsrc[t * 128:(t + 1) * 128, :], [Bx], [Bxt[i]])
            rstd, nmr = ln_stats(xt[i], Bxt[i], tmp[i], Btmp[i])
            act(xn[i], xt[i], AF.Identity, [Bxt[i], Btmp[i]], [Bxn[i]], bias=nmr, scale=rstd)
            tt("pool", xn[i], xn[i], sc, ALU.mult, [Bxn[i], Bmod], [Bxn[i]])
            tt("dve", ub[i], xn[i], sh, ALU.add, [Bxn[i], Bmod], [Bub[i]])
            pb = psb[t % 2]
            for kc in range(8):
                tr(pb[:, kc * 128:(kc + 1) * 128], ub[i][:, kc * 128:(kc + 1) * 128], ident_b, [Bub[i]], [Bpsb[t % 2]])
            cp("act", uT[:, :, t * 128:(t + 1) * 128], pb.rearrange("p (a b) -> p a b", b=128), [Bpsb[t % 2]], [BuTs[t // 4]])

        wblk = [arb.alloc3(8, 512) for _ in range(2)]
        Bw = [B(), B()]
        stg = [arb.alloc(S) for _ in range(2)]
        Bstg = [B(), B()]
        stt_ = [arb.alloc3(4, 512) for _ in range(2)]
        Bstt = [B(), B()]
        bgT = arf.alloc(16)
        Bbg = B()
        ld("sp", bgT, b_gatesT[l, :, :], [], [Bbg])
        wab = arb.alloc3(8, 16)
        Bwab = B()
        blocks = []
        for j in range(2):
            blocks.append((C_QA + 512 * j, "fm", qaT, 512 * j, None))
        for j in range(2):
            blocks.append((C_KA + 512 * j, "fm", kaT, 512 * j, None))
        blocks.append((C_QB, "fm", qbT, 0, None))
        blocks.append((C_KB, "fm", kbT, 0, None))
        for j in range(4):
            blocks.append((C_GT + 512 * j, "fm", gtT, 512 * j, "sig"))
        for j in range(2):
            blocks.append((C_VA + 512 * j, "tm", va, 512 * j, None))
        for j in range(2):
            blocks.append((C_VB + 512 * j, "tm", vb, 512 * j, None))
        for j in range(2):
            blocks.append((C_GB + 512 * j, "tm", gb, 512 * j, "silu"))
        Bdst = {id(t_): B() for t_ in (qaT, kaT, qbT, kbT, gtT, va, vb, gb, abT)}
        pi = 0
        si = 0
        for bi, (c0, kind, dst, d0, fn) in enumerate(blocks):
            w = wblk[bi % 2]
            ld("pool", w, w_in[l, :, c0:c0 + 512].rearrange("(kc p) n -> p kc n", p=128), [], [Bw[bi % 2]])
            if kind == "fm":
                for nch in range(4):
                    sg = stg[si % 2]
                    Bs = Bstg[si % 2]
                    si += 1
                    for tb in range(NQB):
                        pp = pi % 4
                        pi += 1
                        for kc in range(8):
                            mm(psf[pp], w[:, kc, nch * 128:(nch + 1) * 128], uT[:, kc, tb * 512:(tb + 1) * 512],
                               kc == 0, kc == 7, [Bw[bi % 2], BuTs[tb]], [Bpsf[pp]])
                        if fn == "sig":
                            gi = (d0 + nch * 128) // 128
                            act(sg[:, tb * 512:(tb + 1) * 512], psf[pp], AF.Sigmoid, [Bpsf[pp], Bbg], [Bs], bias=bgT[:, gi:gi + 1])
                        elif tb % 2 == 0:
                            cp("dve", sg[:, tb * 512:(tb + 1) * 512], psf[pp], [Bpsf[pp]], [Bs])
                        else:
                            cp("act", sg[:, tb * 512:(tb + 1) * 512], psf[pp], [Bpsf[pp]], [Bs])
                    r0 = d0 + nch * 128
                    ld("sp", dst[r0:r0 + 128, :], sg, [Bs], [Bdst[id(dst)]], part=True, sem=Bs)
            else:
                for t4 in range(NT // 4):
                    sg = stt_[si % 2]
                    Bs = Bstt[si % 2]
                    si += 1
                    for tq in range(4):
                        t = t4 * 4 + tq
                        pp = pi % 4
                        pi += 1
                        for kc in range(8):
                            mm(psf[pp], uT[:, kc, t * 128:(t + 1) * 128], w[:, kc, :], kc == 0, kc == 7,
                               [Bw[bi % 2], BuTs[t // 4]], [Bpsf[pp]])
                        if fn == "silu":
                            act(sg[:, tq, :], psf[pp], AF.Silu, [Bpsf[pp]], [Bs])
                        elif tq % 2 == 0:
                            cp("dve", sg[:, tq, :], psf[pp], [Bpsf[pp]], [Bs])
                        else:
                            cp("act", sg[:, tq, :], psf[pp], [Bpsf[pp]], [Bs])
                    ld("sp", dst[t4 * 512:(t4 + 1) * 512, d0:d0 + 512].rearrange("(a p) n -> p a n", p=128), sg, [Bs],
                       [Bdst[id(dst)]], part=True, sem=Bs)
        ld("pool", wab, w_in[l, :, C_AB:C_AB + 16].rearrange("(kc p) n -> p kc n", p=128), [], [Bwab])
        sg = stg[si % 2]
        Bs = Bstg[si % 2]
        for tb in range(NQB):
            pp = pi % 4
            pi += 1
            for kc in range(8):
                mm(psf[pp][0:16, :], wab[:, kc, :], uT[:, kc, tb * 512:(tb + 1) * 512], kc == 0, kc == 7, [Bwab, BuTs[tb]], [Bpsf[pp]])
            cp("dve", sg[0:16, tb * 512:(tb + 1) * 512], psf[pp][0:16, :], [Bpsf[pp]], [Bs])
        ld("sp", abT[:, :], sg[0:16, :], [Bs], [Bdst[id(abT)]], sem=Bs)

    BoaT = B("oaT")

    def phase_attn(l):
        lam_init = 0.8 - 0.6 * math.exp(-0.3 * l)
        lv = arf.alloc(256)
        Blam = B("lam")
        ld("sp", lv, lam_in[l:l + 1, :, :].rearrange("a b c -> a (b c)").partition_broadcast(128), [], [Blam])
        lv4 = lv.rearrange("p (a b c) -> p a b c", a=2, b=2)
        pr = arf.alloc3(2, 64)
        s12 = arf.alloc(2)
        e12 = arf.alloc(2)
        neglam = arf.alloc(1)
        tt("dve", pr, lv4[:, :, 0, :], lv4[:, :, 1, :], ALU.mult, [Blam], [Blam])
        P.op("dve", lambda e: e.reduce_sum(out=s12, in_=pr, axis=AX.X), [Blam], [Blam])
        act(e12, s12, AF.Exp, [Blam], [Blam])
        tt("dve", neglam, e12[:, 0:1], e12[:, 1:2], ALU.subtract, [Blam], [Blam])
        ts("dve", neglam, neglam, lam_init, -1.0, ALU.add, ALU.mult, [Blam], [Blam])
        gA = arf.alloc(128)
        BgA = B("gA")
        ld("sp", gA, diff_g[l:l + 1, :].partition_broadcast(128), [], [BgA])
        ts("dve", gA, gA, 1.0 - lam_init, None, ALU.mult, None, [BgA], [BgA])

        QT = [[arb.alloc(S) for c in range(2)] for p in range(2)]
        KT = [[arb.alloc(S) for c in range(2)] for p in range(2)]
        VA = [arb.alloc3(NT, 129) for p in range(2)]
        BQ = [[B("Q") for c in range(2)] for p in range(2)]
        BK = [[B("K") for c in range(2)] for p in range(2)]
        BV = [B("V") for p in range(2)]
        for p in range(2):
            memset("pool", VA[p][:, :, 128:129], 1.0, [BV[p]])
        PT = [arb.alloc(512) for _ in range(3)]
        BPT = [B("PT") for _ in range(3)]
        o0 = [arf.alloc(128) for _ in range(4)]
        Bo0 = [B("o0") for _ in range(4)]
        sm = [arf.alloc(8) for _ in range(8)]
        Bsm = [B("sm") for _ in range(8)]
        of = [arf.alloc(128) for _ in range(2)]
        Bof = [B("of") for _ in range(2)]
        junk = arf.alloc(128)
        Bjunk = B("junk")
        obf = [arb.alloc(128) for _ in range(2)]
        Bobf = [B("obf") for _ in range(2)]
        ostg = [arb.alloc(512) for _ in range(2)]
        Bostg = [B("ostg") for _ in range(2)]
        Bsrc = B("attn_src")

        def load_head(h):
            p = h % 2
            for c in range(2):
                r0 = h * 128 + c * 64
                ld("sp", QT[p][c][0:64, :], qaT[r0:r0 + 64, :], [Bsrc], [BQ[p][c]], part=True)
                ld("pool", QT[p][c][64:67, :], c_qaug[:, :], [], [BQ[p][c]], part=True)
                ld("sp", KT[p][c][0:64, :], kaT[r0:r0 + 64, :], [Bsrc], [BK[p][c]], part=True)
                ld("pool", KT[p][c][64:67, :], c_kaug[h, :, :], [], [BK[p][c]], part=True)
            ld("sp", VA[p][:, :, 0:128], va[:, h * 128:(h + 1) * 128].rearrange("(t p) d -> p t d", p=128), [Bsrc], [BV[p]])

        load_head(0)
        sl_ = slopes()
        jobs = []
        for h in range(NH_A):
            dmax = int((60.0 / sl_[h] + 127.0) // 128.0)
            for qb in range(NQB):
                for c in range(2):
                    k0 = max(0, 4 * qb - dmax)
                    for kt in range(k0, 4 * (qb + 1)):
                        jobs.append((h, qb, c, kt, k0))
        state = {"pti": 0, "si": 0}
        Sbank = [psf[0], psf[1], psb[1].bitcast(F32)]
        BSbank = [Bpsf[0], Bpsf[1], Bpsb[1]]
        PT4 = PT + [arb.alloc(512)]
        BPT4 = BPT + [B("PT")]
        of4 = [arf.alloc(128) for _ in range(4)]
        Bof4 = [B("of4") for _ in range(4)]
        ssq = [arf.alloc(8) for _ in range(2)]
        Bssq = [B("ssq") for _ in range(2)]
        LA = 2

        def issue_S(job):
            h, qb, c, kt, k0 = job
            p = h % 2
            j = kt - 4 * qb
            col0 = 128 * max(j, 0)
            sb_ = state["si"] % 3
            state["si"] += 1
            mm(Sbank[sb_][:, col0:512], KT[p][c][0:67, kt * 128:(kt + 1) * 128],
               QT[p][c][0:67, qb * 512 + col0:(qb + 1) * 512], True, True, [BK[p][c], BQ[p][c]], [BSbank[sb_]])
            pt = PT4[state["pti"] % 4]
            Bp = BPT4[state["pti"] % 4]
            state["pti"] += 1
            bi_ = h * 36 + (4 * qb - kt + 3)
            act(pt[:, col0:512], Sbank[sb_][:, col0:512], AF.Exp, [BSbank[sb_]], [Bp],
                bias=biascol[:, bi_:bi_ + 1], scale=0.125)
            if j >= 0:
                tt("pool", pt[:, col0:col0 + 128], pt[:, col0:col0 + 128], tri_b, ALU.mult, [Bp], [Bp])
            return pt, Bp

        loaded = {0}
        pend = [issue_S(jobs[i_]) for i_ in range(min(LA, len(jobs)))]
        gi = 0
        for ji, job in enumerate(jobs):
            h, qb, c, kt, k0 = job
            p = h % 2
            if h + 1 < NH_A and (h + 1) not in loaded:
                load_head(h + 1)
                loaded.add(h + 1)
            pt, Bp = pend.pop(0)
            if ji + LA < len(jobs):
                pend.append(issue_S(jobs[ji + LA]))
            j = kt - 4 * qb
            Ob = [2 + 2 * c, 3 + 2 * c]
            O = [psf[Ob[qt // 2]][:, (qt % 2) * 256:(qt % 2) * 256 + 129] for qt in range(4)]
            for qt in range(max(j, 0), 4):
                mm(O[qt], pt[:, qt * 128:(qt + 1) * 128], VA[p][:, kt, :], kt == k0 and qt % 2 == 0, kt == 4 * qb + qt,
                   [Bp, BV[p]], [Bpsf[Ob[qt // 2]]])
            if kt != 4 * qb + 3:
                continue
            g_ = gi % 2
            sq = ssq[g_]
            Bq_ = Bssq[g_]
            if c == 0:
                for qt in range(4):
                    Bo = Bpsf[Ob[qt // 2]]
                    recip(sq[:, 4 + qt:5 + qt], O[qt][:, 128:129], [Bo], [Bq_])
                    ts("dve", o0[qt], O[qt][:, 0:128], sq[:, 4 + qt:5 + qt], None, ALU.mult, None, [Bo, Bq_], [Bo0[qt]])
                continue
            gi += 1
            for qt in range(4):
                Bo = Bpsf[Ob[qt // 2]]
                recip(sq[:, 4 + qt:5 + qt], O[qt][:, 128:129], [Bo], [Bq_])
                tt("dve", sq[:, 4 + qt:5 + qt], sq[:, 4 + qt:5 + qt], neglam, ALU.mult, [Bq_, Blam], [Bq_])
                stt("dve", of4[qt], O[qt][:, 0:128], sq[:, 4 + qt:5 + qt], o0[qt], ALU.mult, ALU.add, [Bo, Bq_, Bo0[qt]], [Bof4[qt]])
                tt("pool", junk, of4[qt], of4[qt], ALU.mult, [Bof4[qt]], [Bjunk])
                rsum(sq[:, qt:qt + 1], junk, [Bjunk], [Bq_])
            act(sq[:, 0:4], sq[:, 0:4], AF.Ln, [Bq_, Bc2[7]], [Bq_], bias=epsc, scale=1.0 / 128.0)
            act(sq[:, 0:4], sq[:, 0:4], AF.Exp, [Bq_], [Bq_], scale=-0.5)
            for qt in range(4):
                e_ = qt % 2
                stt("dve", obf[e_], of4[qt], sq[:, qt:qt + 1], gA, ALU.mult, ALU.mult, [Bof4[qt], Bq_, BgA], [Bobf[e_]])
                tr(psb[0][:, qt * 128:(qt + 1) * 128], obf[e_], ident_b, [Bobf[e_]], [Bpsb[0]])
            tb_ = gi % 2
            sg = ostg[tb_]
            cp("dve", sg, psb[0][:, 0:512], [Bpsb[0]], [Bostg[tb_]])
            ld("sp", oaT[h * 128:(h + 1) * 128, qb * 512:(qb + 1) * 512], sg, [Bostg[tb_]], [BoaT],
               part=True, sem=Bostg[tb_])

    BobT = B("obT")

    def phase_gla(l, HP=2):
        G = 4
        Bsrc = B("gla_src")
        rmask = arf.alloc(S)
        Brm = B("rmask")
        memset("pool", rmask, 1.0, [Brm])
        memset("pool", rmask.rearrange("p (c t) -> p c t", t=CH)[:, :, 0:1], 0.0, [Brm])
        onec = arf.alloc(1)
        memset("dve", onec, 1.0, [Brm])
        gG = arf.alloc(256)
        ld("sp", gG, gla_g[l:l + 1, :].partition_broadcast(128), [], [Brm], part=True)
        bal = arf.alloc(4)
        Bbal = B("bal")
        ld("sp", bal, b_alphaT[l, :, :], [], [Bbal])
        ts("dve", bal, bal, -1.0, None, ALU.mult, None, [Bbal], [Bbal])
        abt = arb.alloc(S)
        Babt = B("abt")
        ld("sp", abt[0:16, :], abT[:, :], [Bsrc], [Babt])
        wal = arb.alloc(512)
        Bwal = B("wal")
        ld("pool", wal[0:16, :], w_alpha[l, :, :], [], [Bwal])
        cum = arf.alloc(S)
        Ee = arf.alloc(S)
        Ei = arf.alloc(S)
        Bcum, BE, BEi = B("cum"), B("E"), B("Ei")
        qraw = arb.alloc(S)
        kraw = arb.alloc(S)
        Bqraw, Bkraw = B("qraw"), B("kraw")
        for hp0 in range(0, NH_B, HP):
            heads = list(range(hp0, hp0 + HP))
            qtl, ktl, khT, lastE, Sst, Sbf = {}, {}, {}, {}, {}, {}
            Bq, Bk, Bkh, BlE, BS, BSb = {}, {}, {}, {}, {}, {}
            for h in heads:
                i = h - hp0
                if hp0 == 0:
                    qtl[i] = arb.alloc(S); ktl[i] = arb.alloc(S); khT[i] = arb.alloc3(NCH, CH)
                    lastE[i] = arf.alloc(NCH); Sst[i] = arf.alloc(256); Sbf[i] = arb.alloc(256)
                    phase_gla.keepers = getattr(phase_gla, "keepers", {})
                    phase_gla.keepers[i] = (qtl[i], ktl[i], khT[i], lastE[i], Sst[i], Sbf[i])
                else:
                    qtl[i], ktl[i], khT[i], lastE[i], Sst[i], Sbf[i] = phase_gla.keepers[i]
                if hp0 == 0:
                    phase_gla.bufs = getattr(phase_gla, "bufs", {})
                    phase_gla.bufs[i] = (B("qt"), B("kt"), B("khT"), B("lastE"), B("S"), B("Sb"))
                Bq[i], Bk[i], Bkh[i], BlE[i], BS[i], BSb[i] = phase_gla.bufs[i]
                ld("sp", qraw, qbT[h * 128:(h + 1) * 128, :], [Bsrc], [Bqraw])
                ld("sp", kraw, kbT[h * 128:(h + 1) * 128, :], [Bsrc], [Bkraw])
                for tb in range(NQB):
                    pp = tb % 2
                    mm(psf[pp], wal[0:16, h * 128:(h + 1) * 128], abt[0:16, tb * 512:(tb + 1) * 512], True, True,
                       [Bwal, Babt], [Bpsf[pp]])
                    act(cum[:, tb * 512:(tb + 1) * 512], psf[pp], AF.Exp, [Bpsf[pp], Bbal], [Bcum], bias=bal[:, h:h + 1], scale=-1.0)
                act(cum, cum, AF.Ln, [Bcum, Brm], [Bcum], bias=onec, scale=1.0)
                P.op("dve", lambda e: e.tensor_tensor_scan(out=cum, data0=rmask, data1=cum, initial=0.0, op0=ALU.mult, op1=ALU.add),
                     [Bcum, Brm], [Bcum])
                act(Ee, cum, AF.Exp, [Bcum], [BE], scale=-1.0 / TAU)
                act(Ei, cum, AF.Exp, [Bcum], [BEi], scale=1.0 / TAU)
                stt("dve", qtl[i], qraw, DK_B ** -0.5, Ee, ALU.mult, ALU.mult, [Bqraw, BE], [Bq[i]])
                tt("pool", ktl[i], kraw, Ei, ALU.mult, [Bkraw, BEi], [Bk[i]])
                cp("dve", lastE[i], Ee.rearrange("p (c t) -> p c t", t=CH)[:, :, CH - 1], [BE], [BlE[i]])
                tt("pool", khT[i], ktl[i].rearrange("p (c t) -> p c t", t=CH),
                   lastE[i].unsqueeze(2).to_broadcast([128, NCH, CH]), ALU.mult, [Bk[i], BlE[i]], [Bkh[i]])
            if hp0 == 0:
                vt = [[arb.alloc3(G, 256) for _ in range(2)] for i in range(HP)]
                gt = [[arb.alloc3(G, 256) for _ in range(2)] for i in range(HP)]
                Bvt = [[B("vt") for _ in range(2)] for i in range(HP)]
                Bgt = [[B("gt") for _ in range(2)] for i in range(HP)]
                attm = [arb.alloc(64) for i in range(HP)]
                Battm = [B("attm") for i in range(HP)]
                khc = [arb.alloc(128) for i in range(HP)]
                Bkhc = [B("khc") for i in range(HP)]
                onf = [arf.alloc(256) for i in range(HP)]
                Bonf = [B("onf") for i in range(HP)]
                obc = [arb.alloc(256) for i in range(HP)]
                Bobc = [B("obc") for i in range(HP)]
                smg = [[arf.alloc(4) for _ in range(2)] for i in range(HP)]
                Bsmg = [[B("smg") for _ in range(2)] for i in range(HP)]
                junk = arf.alloc(256)
                Bjunk = B("junk")
                obst = [[arb.alloc3(2, 256) for _ in range(2)] for i in range(HP)]
                Bobst = [[B("obst") for _ in range(2)] for i in range(HP)]
                phase_gla.loopbufs = (vt, gt, Bvt, Bgt, attm, Battm, khc, Bkhc, onf, Bonf, obc, Bobc, smg, Bsmg, junk, Bjunk, obst, Bobst)
            else:
                (vt, gt, Bvt, Bgt, attm, Battm, khc, Bkhc, onf, Bonf, obc, Bobc, smg, Bsmg, junk, Bjunk, obst, Bobst) = phase_gla.loopbufs

            def load_group(g):
                for h in heads:
                    i = h - hp0
                    t0 = g * G * CH
                    ld("sp", vt[i][g % 2][0:64], vb[t0:t0 + G * CH, h * 256:(h + 1) * 256].rearrange("(c p) f -> p c f", p=CH),
                       [Bsrc], [Bvt[i][g % 2]])
                    ld("sp", gt[i][g % 2][0:64], gb[t0:t0 + G * CH, h * 256:(h + 1) * 256].rearrange("(c p) f -> p c f", p=CH),
                       [Bsrc], [Bgt[i][g % 2]])

            NGRP = NCH // G
            load_group(0)
            for g in range(NGRP):
                if g + 1 < NGRP:
                    load_group(g + 1)
                for ci in range(G):
                    c = g * G + ci
                    for h in heads:
                        i = h - hp0
                        cs = slice(c * CH, (c + 1) * CH)
                        pa = (2 * c + i) % 2
                        po = 2 + i
                        ps_ = 4 + i
                        v_c = vt[i][g % 2][0:64, ci, :]
                        Bv = Bvt[i][g % 2]
                        mm(psf[pa][0:64, 0:64], ktl[i][:, cs], qtl[i][:, cs], True, True, [Bk[i], Bq[i]], [Bpsf[pa]])
                        tt("dve", attm[i][0:64, :], psf[pa][0:64, 0:64], gmask_f, ALU.mult, [Bpsf[pa]], [Battm[i]])
                        tr(psb[0][0:64, i * 128:(i + 1) * 128], khT[i][:, c, :], ident_b, [Bkh[i]], [Bpsb[0]])
                        cp("act", khc[i][0:64, :], psb[0][0:64, i * 128:(i + 1) * 128], [Bpsb[0]], [Bkhc[i]])
                        if c > 0:
                            mm(psf[po][0:64, 0:256], qtl[i][:, cs], Sbf[i], True, False, [Bq[i], BSb[i]], [Bpsf[po]])
                        mm(psf[po][0:64, 0:256], attm[i][0:64, :], v_c, c == 0, True, [Battm[i], Bv], [Bpsf[po]])
                        if c + 1 < NCH:
                            mm(psf[ps_][:, 0:256], khc[i][0:64, :], v_c, True, True, [Bkhc[i], Bv], [Bpsf[ps_]])
                            if c == 0:
                                cp("dve", Sst[i], psf[ps_][:, 0:256], [Bpsf[ps_]], [BS[i]])
                            else:
                                stt("dve", Sst[i], Sst[i], lastE[i][:, c:c + 1], psf[ps_][:, 0:256], ALU.mult, ALU.add,
                                    [BS[i], BlE[i], Bpsf[ps_]], [BS[i]])
                            cp("act", Sbf[i], Sst[i], [BS[i]], [BSb[i]])
                        sm_ = smg[i][c % 2]
                        Bs_ = Bsmg[i][c % 2]
                        act(junk[0:64, :], psf[po][0:64, 0:256], AF.Square, [Bpsf[po]], [Bjunk, Bs_], accum_out=sm_[0:64, 0:1])
                        rsqrt_(sm_[0:64, 1:2], sm_[0:64, 0:1], 1.0 / 256.0, Bs_)
                        stt("dve", onf[i][0:64, :], psf[po][0:64, 0:256], sm_[0:64, 1:2], gG[0:64, :], ALU.mult, ALU.mult,
                            [Bpsf[po], Bs_, Brm], [Bonf[i]])
                        tt("pool", obc[i][0:64, :], onf[i][0:64, :], gt[i][g % 2][0:64, ci, :], ALU.mult, [Bonf[i], Bgt[i][g % 2]], [Bobc[i]])
                        for j in range(2):
                            col = i * 512 + j * 256 + ci * 64
                            tr(psb[1][:, col:col + 64], obc[i][0:64, j * 128:(j + 1) * 128], ident_b[0:64, 0:64], [Bobc[i]], [Bpsb[1]])
                        if ci == G - 1:
                            sg = obst[i][g % 2]
                            Bs2 = Bobst[i][g % 2]
                            cp("act", sg, psb[1][:, i * 512:(i + 1) * 512].rearrange("p (j t) -> p j t", t=256), [Bpsb[1]], [Bs2])
                            t0 = g * G * CH
                            for j in range(2):
                                r0 = h * 256 + j * 128
                                ld("sp", obT[r0:r0 + 128, t0:t0 + G * CH], sg[:, j, :], [Bs2], [BobT], part=True, sem=Bs2)

    RT = {}

    def phase_merge(l, xcur, xmid):
        TB = 256
        NTB = S // TB
        Bsrc = B("mg_src")
        A1s = arf.alloc3(NT, 32); A2s = arf.alloc3(NT, 32); RK = arf.alloc3(NT, 32)
        W12 = arf.alloc3(NT, 2)
        Asum = arf.alloc(32)
        Asb = arb.alloc(32)
        BA1, BA2, BRK, BW12, BAs, BAsb = B("A1s"), B("A2s"), B("RK"), B("W12"), B("Asum"), B("Asb")
        RT.update(A1s=A1s, A2s=A2s, RK=RK, W12=W12, Asum=Asum, Asb=Asb, BA1=BA1, BA2=BA2, BRK=BRK, BW12=BW12, BAs=BAs, BAsb=BAsb)
        arf.push(); arb.push()
        wts = []
        Bwts = []
        for wi, wsrc in enumerate((w_ba, w_bb, w_out)):
            w_ = arb.alloc3(8, 1024)
            Bw_ = B("mgw")
            for kc in range(8):
                ld("pool", w_[:, kc, :], wsrc[l, kc * 128:(kc + 1) * 128, :], [], [Bw_], part=True)
            wts.append(w_)
            Bwts.append(Bw_)
        wba_, wbb_, wout_ = wts
        wr = arb.alloc3(8, 36)
        Bwr = B("wr")
        ld("pool", wr, w_r[l, :, :].rearrange("(kc p) n -> p kc n", p=128), [], [Bwr])
        Bbc = B("bcast")
        bc = {}
        for nm, src in (("g1", adaD[l:l + 1, 2048:3072]), ("sh2", adaD[l:l + 1, 3072:4096]), ("sc2", adaD[l:l + 1, 4096:5120]),
                        ("l1g", ln1_g[l:l + 1, :]), ("l1b", ln1_b[l:l + 1, :])):
            bc[nm] = arf.alloc(D)
            ld("sp", bc[nm], src.partition_broadcast(128), [Bada], [Bbc], part=True)
        brb = arf.alloc(36)
        ld("sp", brb, b_r[l:l + 1, :].partition_broadcast(128), [], [Bbc], part=True)
        oab = [arb.alloc3(8, TB) for _ in range(2)]
        obb = [arb.alloc3(8, TB) for _ in range(2)]
        gtb = [arb.alloc3(16, TB) for _ in range(2)]
        Bin = [B("mg_in") for _ in range(2)]
        mxT = [arb.alloc3(8, TB) for _ in range(2)]
        BmxT = [B("mxT") for _ in range(2)]
        m1 = [arf.alloc(TB) for _ in range(2)]
        Bm1 = [B("m1") for _ in range(2)]
        xt = [arf.alloc(D) for _ in range(2)]
        Bxt = [B("xt") for _ in range(2)]
        rr = [arf.alloc(D) for _ in range(2)]
        Brr = [B("rr") for _ in range(2)]
        tmp = [arf.alloc(16) for _ in range(4)]
        Btmp = [B("tmp") for _ in range(4)]
        u2b = [arb.alloc(D) for _ in range(2)]
        Bu2b = [B("u2b") for _ in range(2)]
        u2T = [arb.alloc3(8, 128) for _ in range(2)]
        Bu2T = [B("u2T") for _ in range(2)]
        rs = [arf.alloc(128) for _ in range(2)]
        Brs = [B("rs") for _ in range(2)]
        a12b = [arb.alloc(32) for _ in range(2)]
        Ba12 = [B("a12b") for _ in range(2)]
        Bxm = B("xmid")
        Bu2d = B("u2d")

        def load_blk(tb):
            i = tb % 2
            ld("sp", oab[i], oaT[:, tb * TB:(tb + 1) * TB].rearrange("(kc p) t -> p kc t", p=128), [Bsrc], [Bin[i]], part=True)
            ld("sp", obb[i], obT[:, tb * TB:(tb + 1) * TB].rearrange("(kc p) t -> p kc t", p=128), [Bsrc], [Bin[i]], part=True)
            ld("sp", gtb[i], gtT[:, tb * TB:(tb + 1) * TB].rearrange("(kc p) t -> p kc t", p=128), [Bsrc], [Bin[i]], part=True)

        load_blk(0)
        for tb in range(NTB):
            i = tb % 2
            if tb + 1 < NTB:
                load_blk(tb + 1)
            for nch in range(8):
                pA, pB = (2 * nch) % 4, (2 * nch + 1) % 4
                for kc in range(8):
                    mm(psf[pA][:, 0:TB], wba_[:, kc, nch * 128:(nch + 1) * 128], oab[i][:, kc, :], kc == 0, kc == 7, [Bwts[0], Bin[i]], [Bpsf[pA]])
                for kc in range(8):
                    mm(psf[pB][:, 0:TB], wbb_[:, kc, nch * 128:(nch + 1) * 128], obb[i][:, kc, :], kc == 0, kc == 7, [Bwts[1], Bin[i]], [Bpsf[pB]])
                mi = nch % 2
                tt("dve", m1[mi], psf[pA][:, 0:TB], gtb[i][:, nch, :], ALU.mult, [Bpsf[pA], Bin[i]], [Bm1[mi]])
                tt("dve", mxT[i][:, nch, :], psf[pB][:, 0:TB], gtb[i][:, 8 + nch, :], ALU.mult, [Bpsf[pB], Bin[i]], [BmxT[i]])
                tt("pool", mxT[i][:, nch, :], mxT[i][:, nch, :], m1[mi], ALU.add, [BmxT[i], Bm1[mi]], [BmxT[i]])
            for tq in range(TB // 128):
                t = tb * (TB // 128) + tq
                j = t % 2
                ld("sp", xt[j], xcur[t * 128:(t + 1) * 128, :], [Bsrc], [Bxt[j]])
                for half in range(2):
                    py = 4 + half
                    for kc in range(8):
                        mm(psf[py], mxT[i][:, kc, tq * 128:(tq + 1) * 128], wout_[:, kc, half * 512:(half + 1) * 512], kc == 0, kc == 7,
                           [BmxT[i], Bwts[2]], [Bpsf[py]])
                    tt("dve", rr[j][:, half * 512:(half + 1) * 512], psf[py], bc["g1"][:, half * 512:(half + 1) * 512], ALU.mult,
                       [Bpsf[py], Bbc], [Brr[j]])
                stt("dve", rr[j], xt[j], ALPHA, rr[j], ALU.mult, ALU.add, [Bxt[j], Brr[j]], [Brr[j]])
                rstd, nmr = ln_stats(rr[j], Brr[j], tmp[2 * j], Btmp[2 * j])
                act(rr[j], rr[j], AF.Identity, [Brr[j], Btmp[2 * j]], [Brr[j]], bias=nmr, scale=rstd)
                tt("pool", rr[j], rr[j], bc["l1g"], ALU.mult, [Brr[j], Bbc], [Brr[j]])
                tt("dve", xt[j], rr[j], bc["l1b"], ALU.add, [Brr[j], Bbc], [Bxt[j]])
                ld("sp", xmid[t * 128:(t + 1) * 128, :], xt[j], [Bxt[j]], [Bxm], part=True, sem=Bxt[j])
                rstd2, nmr2 = ln_stats(xt[j], Bxt[j], tmp[2 * j + 1], Btmp[2 * j + 1])
                act(rr[j], xt[j], AF.Identity, [Bxt[j], Btmp[2 * j + 1]], [Brr[j]], bias=nmr2, scale=rstd2)
                tt("pool", rr[j], rr[j], bc["sc2"], ALU.mult, [Brr[j], Bbc], [Brr[j]])
                tt("dve", u2b[j], rr[j], bc["sh2"], ALU.add, [Brr[j], Bbc], [Bu2b[j]])
                ld("sp", u2d[t * 128:(t + 1) * 128, :], u2b[j], [Bu2b[j]], [Bu2d], part=True, sem=Bu2b[j])
                for kc in range(8):
                    tr(psb[0][:, kc * 128:(kc + 1) * 128], u2b[j][:, kc * 128:(kc + 1) * 128], ident_b, [Bu2b[j]], [Bpsb[0]])
                cp("act", u2T[j], psb[0].rearrange("p (a b) -> p a b", b=128), [Bpsb[0]], [Bu2T[j]])
                RBk = psb[1].bitcast(F32)
                BRBk = Bpsb[1]
                for kc in range(8):
                    mm(RBk[:, 0:36], u2T[j][:, kc, :], wr[:, kc, :], kc == 0, kc == 7, [Bu2T[j], Bwr], [BRBk])
                R_ = rs[j]
                BR = Brs[j]
                lg = R_[:, 0:36]
                tt("dve", lg, RBk[:, 0:36], brb, ALU.add, [BRBk, Bbc], [BR])
                gl = lg[:, 0:4]
                el = lg[:, 4:36].rearrange("p (g e) -> p g e", e=8)
                gmax, ngmax, gsum, gw = R_[:, 36:37], R_[:, 37:38], R_[:, 38:39], R_[:, 39:40]
                ohg = R_[:, 40:44]
                gex = R_[:, 44:48]
                ein = R_[:, 48:56]
                ein2 = R_[:, 56:64]
                mk1 = R_[:, 64:72]
                mk2 = R_[:, 72:80]
                mx1, mx2, dd, ex, den, w1, w2 = (R_[:, 80 + q:81 + q] for q in range(7))
                a12 = R_[:, 90:122]
                rmax(gmax, gl, [BR], [BR])
                ts("dve", ohg, gl, gmax, None, ALU.is_equal, None, [BR], [BR])
                ts("dve", ngmax, gmax, -1.0, None, ALU.mult, None, [BR], [BR])
                act(gex, gl, AF.Exp, [BR], [BR], bias=ngmax, scale=1.0, accum_out=gsum)
                recip(gw, gsum, [BR], [BR])
                ts("dve", ein, el[:, 0, :], ohg[:, 0:1], None, ALU.mult, None, [BR], [BR])
                for g_ in range(1, 4):
                    stt("dve", ein, el[:, g_, :], ohg[:, g_:g_ + 1], ein, ALU.mult, ALU.add, [BR], [BR])
                rmax(mx1, ein, [BR], [BR])
                ts("dve", mk1, ein, mx1, None, ALU.is_equal, None, [BR], [BR])
                stt("dve", ein2, mk1, -1.0e30, ein, ALU.mult, ALU.add, [BR], [BR])
                rmax(mx2, ein2, [BR], [BR])
                ts("dve", mk2, ein2, mx2, None, ALU.is_equal, None, [BR], [BR])
                tt("dve", dd, mx2, mx1, ALU.subtract, [BR], [BR])
                act(ex, dd, AF.Exp, [BR], [BR])
                ts("dve", den, ex, 1.0, None, ALU.add, None, [BR], [BR])
                recip(w1, den, [BR], [BR])
                tt("dve", w2, ex, w1, ALU.mult, [BR], [BR])
                tt("dve", W12[:, t, 0:1], w1, gw, ALU.mult, [BR], [BW12])
                tt("dve", W12[:, t, 1:2], w2, gw, ALU.mult, [BR], [BW12])
                a1v = A1s[:, t, :].rearrange("p (g e) -> p g e", e=8)
                a2v = A2s[:, t, :].rearrange("p (g e) -> p g e", e=8)
                tt("dve", a1v, ohg.unsqueeze(2).to_broadcast([128, 4, 8]), mk1.unsqueeze(1).to_broadcast([128, 4, 8]), ALU.mult, [BR], [BA1])
                tt("dve", a2v, ohg.unsqueeze(2).to_broadcast([128, 4, 8]), mk2.unsqueeze(1).to_broadcast([128, 4, 8]), ALU.mult, [BR], [BA2])
                tt("dve", a12, A1s[:, t, :], A2s[:, t, :], ALU.add, [BA1, BA2], [BR])
                cp("dve", a12b[j], a12, [BR], [Ba12[j]])
                mm(RBk[:, 64:96], ustr_b, a12b[j], True, t == 0, [Ba12[j]], [BRBk])
                if t > 0:
                    mm(RBk[:, 64:96], ones_b, Asb, False, True, [BAsb], [BRBk])
                cp("dve", RK[:, t, :], RBk[:, 64:96], [BRBk], [BRK])
                if t == 0:
                    cp("dve", Asum, a12, [BR], [BAs])
                else:
                    tt("dve", Asum, Asum, a12, ALU.add, [BAs, BR], [BAs])
                cp("dve", Asb, Asum, [BAs], [BAsb])

    c_blkstart = inp("blkstart", [128, NBLK])
    c_iotaG = inp("iotaG", [128, 8])

    def phase_moe(l, xmid, xnext):
        A1s, A2s, RK, W12, Asb = RT["A1s"], RT["A2s"], RT["RK"], RT["W12"], RT["Asb"]
        Bg = B("moe_glob")
        cnt = arf.alloc(32); pad = arf.alloc(32); pend = arf.alloc(32); pst = arf.alloc(32); one32 = arf.alloc(32)
        PR = arf.alloc3(NT, 32)
        D1f = arf.alloc(NT); D2f = arf.alloc(NT)
        D1i = ari.alloc(NT); D2i = ari.alloc(NT)
        bst = arf.alloc(NBLK); ble = arf.alloc(NBLK)
        cmpb = arf.alloc3(NBLK, 32)
        iog = arf.alloc(8)
        ixGf = arf.alloc3(NBLK, 8); ixDf = arf.alloc3(NBLK, 4)
        ixG = ari.alloc3(NBLK, 8); ixD = ari.alloc3(NBLK, 4)
        ld("sp", bst, c_blkstart[:, :], [], [Bg], part=True)
        ld("sp", iog, c_iotaG[:, :], [], [Bg], part=True)
        memset("dve", one32, 1.0, [Bg])
        mm(psf[0][:, 0:32], ones_b, Asb, True, True, [], [Bpsf[0]])
        cp("dve", cnt, psf[0][:, 0:32], [Bpsf[0]], [Bg])
        KC = S // BLK + 1
        cmp2 = cmpb.rearrange("p a b -> p (a b)")[:, 0:32 * KC].rearrange("p (e k) -> p e k", k=KC)
        tt("dve", cmp2, cnt.unsqueeze(2).to_broadcast([128, 32, KC]), bst[:, 0:KC].unsqueeze(1).to_broadcast([128, 32, KC]), ALU.is_gt, [Bg], [Bg])
        P.op("dve", lambda e: e.reduce_sum(out=pad, in_=cmp2, axis=AX.X), [Bg], [Bg])
        ts("dve", pad, pad, float(BLK), None, ALU.mult, None, [Bg], [Bg])
        P.op("dve", lambda e: e.tensor_tensor_scan(out=pend, data0=one32, data1=pad, initial=0.0, op0=ALU.mult, op1=ALU.add), [Bg], [Bg])
        tt("dve", pst, pend, pad, ALU.subtract, [Bg], [Bg])
        tt("dve", PR, RK, pst.unsqueeze(1).to_broadcast([128, NT, 32]), ALU.add, [Bg], [Bg])
        tt("dve", A1s, A1s, PR, ALU.mult, [Bg], [Bg])
        tt("dve", A2s, A2s, PR, ALU.mult, [Bg], [Bg])
        P.op("dve", lambda e: e.reduce_sum(out=D1f, in_=A1s, axis=AX.X), [Bg], [Bg])
        P.op("dve", lambda e: e.reduce_sum(out=D2f, in_=A2s, axis=AX.X), [Bg], [Bg])
        cp("dve", D1i, D1f, [Bg], [Bg])
        cp("dve", D2i, D2f, [Bg], [Bg])
        tt("dve", cmpb, pend.unsqueeze(1).to_broadcast([128, NBLK, 32]), bst.unsqueeze(2).to_broadcast([128, NBLK, 32]), ALU.is_le, [Bg], [Bg])
        P.op("dve", lambda e: e.reduce_sum(out=ble, in_=cmpb, axis=AX.X), [Bg], [Bg])
        ts("dve", ble, ble, float(NE - 1), None, ALU.min, None, [Bg], [Bg])
        ts("dve", bst, ble, 128.0, float(l * NE * 128), ALU.mult, ALU.add, [Bg], [Bg])
        tt("dve", ixGf[:, :, 0], bst, iog[:, 0:1].to_broadcast([128, NBLK]), ALU.add, [Bg], [Bg])
        cp("dve", ixG[:, :, 0], ixGf[:, :, 0], [Bg], [Bg])
        if "dest" in dbg:
            for nm_, ap_, n_ in (("d_cnt", cnt, 32), ("d_pad", pad, 32), ("d_pend", pend, 32), ("d_pst", pst, 32),
                                 ("d_A1", A1s.rearrange("p a b -> p (a b)"), NT * 32), ("d_RK", RK.rearrange("p a b -> p (a b)"), NT * 32),
                                 ("d_PR", PR.rearrange("p a b -> p (a b)"), NT * 32), ("d_W12", W12.rearrange("p a b -> p (a b)"), NT * 2)):
                dt_ = nc.dram_tensor(nm_, [128, n_], F32, kind="ExternalOutput")
                ld("sp", dt_[:, :], ap_, [Bg], [B("dd")])
            dd_ = nc.dram_tensor("dest", [128, 2 * NT + NBLK], F32, kind="ExternalOutput")
            ld("sp", dd_[:, 0:NT], D1f, [Bg], [B("dd")])
            ld("sp", dd_[:, NT:2 * NT], D2f, [Bg], [B("dd")])
            ld("sp", dd_[:, 2 * NT:], ble, [Bg], [B("dd")])

        BXB = B("XB")
        Bu2src = B("u2src")
        ut = [arb.alloc(D) for _ in range(2)]
        But = [B("ut") for _ in range(2)]
        for t in range(NT):
            j = t % 2
            ld("sp", ut[j], u2d[t * 128:(t + 1) * 128, :], [Bu2src], [But[j]])
            for Di in (D1i, D2i):
                P.dma("pool", (lambda src_, off_: (lambda e: e.indirect_dma_start(
                    out=XB[:, :], out_offset=bass.IndirectOffsetOnAxis(ap=off_, axis=0), in_=src_, in_offset=None)))(ut[j], Di[:, t:t + 1]),
                    [But[j], Bg], [BXB], part=True, sem=But[j])
        P.barrier()

        NA = BLK // 128
        xb = [arb.alloc3(NA, D) for _ in range(2)]
        Bxb = [B("xb") for _ in range(2)]
        xbT = [arb.alloc3(8, BLK) for _ in range(2)]
        BxbT = [B("xbT") for _ in range(2)]
        Wg = [arb.alloc3(8, DEXP) for _ in range(2)]
        Wu = [arb.alloc3(8, DEXP) for _ in range(2)]
        Wd = [arb.alloc3(4, D) for _ in range(2)]
        BWg = [B("Wg") for _ in range(2)]
        BWu = [B("Wu") for _ in range(2)]
        BWd = [B("Wd") for _ in range(2)]
        sgf = [arf.alloc(BLK) for _ in range(2)]
        Bsgf = [B("sgf") for _ in range(2)]
        hT = [arb.alloc3(4, BLK) for _ in range(2)]
        BhT = [B("hT") for _ in range(2)]
        yst = [arf.alloc(D) for _ in range(2)]
        Byst = [B("yst") for _ in range(2)]
        BYB = B("YB")

        def gath(dst, src2d, off):
            return lambda e: e.indirect_dma_start(out=dst, out_offset=None, in_=src2d, in_offset=bass.IndirectOffsetOnAxis(ap=off, axis=0))

        def load_block(b):
            i = b % 2
            ld("sp", xb[i], XB[b * BLK:(b + 1) * BLK, :].rearrange("(a p) d -> p a d", p=128), [BXB], [Bxb[i]])
            P.dma("pool", gath(Wg[i].rearrange("p a b -> p (a b)"), w_ge[:, :], ixG[:, b, 0:1]), [Bg], [BWg[i]])
            P.dma("pool", gath(Wu[i].rearrange("p a b -> p (a b)"), w_ue[:, :], ixG[:, b, 0:1]), [Bg], [BWu[i]])
            P.dma("pool", gath(Wd[i].rearrange("p a b -> p (a b)"), w_de[:, :], ixG[:, b, 0:1]), [Bg], [BWd[i]])

        load_block(0)
        ysi = 0
        for b in range(NBLK):
            i = b % 2
            if b + 1 < NBLK:
                load_block(b + 1)
            for a in range(NA):
                for kc in range(8):
                    tr(psb[a % 2][:, kc * 128:(kc + 1) * 128], xb[i][:, a, kc * 128:(kc + 1) * 128], ident_b, [Bxb[i]], [Bpsb[a % 2]])
                cp("act" if a % 2 else "dve", xbT[i][:, :, a * 128:(a + 1) * 128], psb[a % 2].rearrange("p (k s) -> p k s", s=128), [Bpsb[a % 2]], [BxbT[i]])
            for hc in range(4):
                pg, pu = (2 * hc) % 4, (2 * hc + 1) % 4
                for kc in range(8):
                    mm(psf[pg][:, 0:BLK], Wg[i][:, kc, hc * 128:(hc + 1) * 128], xbT[i][:, kc, :], kc == 0, kc == 7, [BWg[i], BxbT[i]], [Bpsf[pg]])
                for kc in range(8):
                    mm(psf[pu][:, 0:BLK], Wu[i][:, kc, hc * 128:(hc + 1) * 128], xbT[i][:, kc, :], kc == 0, kc == 7, [BWu[i], BxbT[i]], [Bpsf[pu]])
                si_ = hc % 2
                act(sgf[si_], psf[pg][:, 0:BLK], AF.Silu, [Bpsf[pg]], [Bsgf[si_]])
                tt("dve", hT[i][:, hc, :], psf[pu][:, 0:BLK], sgf[si_], ALU.mult, [Bpsf[pu], Bsgf[si_]], [BhT[i]])
            for a in range(NA):
                ys = yst[ysi % 2]
                Bys = Byst[ysi % 2]
                ysi += 1
                for half in range(2):
                    py = 4 + half
                    for hc in range(4):
                        mm(psf[py], hT[i][:, hc, a * 128:(a + 1) * 128], Wd[i][:, hc, half * 512:(half + 1) * 512], hc == 0, hc == 3,
                           [BhT[i], BWd[i]], [Bpsf[py]])
                    cp("act" if half else "dve", ys[:, half * 512:(half + 1) * 512], psf[py], [Bpsf[py]], [Bys])
                r0 = b * BLK + a * 128
                ld("sp", YB[r0:r0 + 128, :], ys, [Bys], [BYB], part=True, sem=Bys)
        P.barrier()

        Bbc = B("bc2")
        bc = {}
        for nm, src in (("g2", adaD[l:l + 1, 5120:6144]), ("l2g", ln2_g[l:l + 1, :]), ("l2b", ln2_b[l:l + 1, :])):
            bc[nm] = arf.alloc(D)
            ld("sp", bc[nm], src.partition_broadcast(128), [], [Bbc], part=True)
        y1 = [arf.alloc(D) for _ in range(2)]
        y2 = [arf.alloc(D) for _ in range(2)]
        By1 = [B("y1") for _ in range(2)]
        By2 = [B("y2") for _ in range(2)]
        xt = [arf.alloc(D) for _ in range(2)]
        Bxt = [B("xt2") for _ in range(2)]
        tmp = [arf.alloc(16) for _ in range(2)]
        Btmp = [B("tmp2") for _ in range(2)]
        Bxn = B("xnext")
        Bxs = B("xmid_src")

        def load_t(t):
            j = t % 2
            ld("sp", xt[j], xmid[t * 128:(t + 1) * 128, :], [Bxs], [Bxt[j]])
            P.dma("pool", gath(y1[j], YB[:, :], D1i[:, t:t + 1]), [BYB, Bg], [By1[j]])
            P.dma("pool", gath(y2[j], YB[:, :], D2i[:, t:t + 1]), [BYB, Bg], [By2[j]])

        load_t(0)
        for t in range(NT):
            j = t % 2
            if t + 1 < NT:
                load_t(t + 1)
            ts("dve", y1[j], y1[j], W12[:, t, 0:1], None, ALU.mult, None, [By1[j]], [By1[j]])
            stt("dve", y1[j], y2[j], W12[:, t, 1:2], y1[j], ALU.mult, ALU.add, [By2[j], By1[j]], [By1[j]])
            tt("pool", y1[j], y1[j], bc["g2"], ALU.mult, [By1[j], Bbc], [By1[j]])
            stt("dve", y1[j], xt[j], ALPHA, y1[j], ALU.mult, ALU.add, [Bxt[j], By1[j]], [By1[j]])
            rstd, nmr = ln_stats(y1[j], By1[j], tmp[j], Btmp[j])
            act(y1[j], y1[j], AF.Identity, [By1[j], Btmp[j]], [By1[j]], bias=nmr, scale=rstd)
            tt("pool", y1[j], y1[j], bc["l2g"], ALU.mult, [By1[j], Bbc], [By1[j]])
            tt("dve", xt[j], y1[j], bc["l2b"], ALU.add, [By1[j], Bbc], [Bxt[j]])
            ld("sp", xnext[t * 128:(t + 1) * 128, :], xt[j], [Bxt[j]], [Bxn], part=True, sem=Bxt[j])
        arf.pop(); arb.pop()

    phase_ada()
    phase_reset()
    if phases == "all":
        xcur = x_in
        for l in range(L):
            phase_proj(l, xcur)
            phase_reset()
            phase_attn(l)
            phase_reset()
            phase_gla(l)
            phase_reset()
            phase_merge(l, xcur, xB)
            phase_reset()
            phase_moe(l, xB, out if l == L - 1 else xA)
            phase_reset()
            xcur = xA
    else:
        if "proj" in phases:
            phase_proj(0, x_in)
            phase_reset()
        if "attn" in phases:
            phase_attn(0)
            phase_reset()
        if "gla" in phases:
            phase_gla(0)
            phase_reset()
        if "merge" in phases:
            phase_merge(0, x_in, xB)
            phase_reset()
        if "moe" in phases:
            phase_moe(0, xB, out)
            phase_reset()
    P.emit(nc, st)
    st.close()
    nc._prog = P
    return nc


def make_in_map(inp, b, S):
    f = lambda a: np.ascontiguousarray(np.asarray(a, dtype=np.float32))
    m = {}
    m["x"] = f(inp["x"][b, :S])
    m["cT"] = f(np.asarray(inp["c"][b]).reshape(8, 128).T)
    for k_ in ("w_ada", "b_ada", "w_in", "w_alpha", "diff_norm_g", "gla_norm_g", "w_branch_a", "w_branch_b",
               "w_out", "ln1_g", "ln1_b", "ln2_g", "ln2_b"):
        m[k_] = f(inp[k_])
    m["b_gatesT"] = f(np.asarray(inp["b_gates"]).reshape(DEPTH, 16, 128).transpose(0, 2, 1))
    m["b_alphaT"] = f(np.asarray(inp["b_alpha"]).reshape(DEPTH, 4, 128).transpose(0, 2, 1))
    m["lam4"] = f(np.stack([np.asarray(inp[k_]) for k_ in ("lambda_q1", "lambda_k1", "lambda_q2", "lambda_k2")], axis=1))
    m["w_router"] = f(np.concatenate([np.asarray(inp["w_router_g"]), np.asarray(inp["w_router_e"])], axis=2))
    m["b_router"] = f(np.concatenate([np.asarray(inp["b_router_g"]), np.asarray(inp["b_router_e"])], axis=1))
    m["w_gate_e"] = f(np.asarray(inp["w_gate_e"]).reshape(DEPTH, NE, 8, 128, DEXP).transpose(0, 1, 3, 2, 4)).reshape(DEPTH * NE * 128, 8 * DEXP)
    m["w_up_e"] = f(np.asarray(inp["w_up_e"]).reshape(DEPTH, NE, 8, 128, DEXP).transpose(0, 1, 3, 2, 4)).reshape(DEPTH * NE * 128, 8 * DEXP)
    m["w_down_e"] = f(np.asarray(inp["w_down_e"]).reshape(DEPTH, NE, 4, 128, D).transpose(0, 1, 3, 2, 4)).reshape(DEPTH * NE * 128, 4 * D)
    m.update(host_consts(S))
    m["iota_p"] = np.arange(128, dtype=np.float32).reshape(128, 1)
    nslot = ((2 * S + NE * (BLK - 1)) // BLK + 1) * BLK
    nblk = nslot // BLK
    m["blkstart"] = np.broadcast_to((np.arange(nblk, dtype=np.float32) * BLK)[None, :], (128, nblk)).copy()
    m["iotaG"] = (np.arange(8, dtype=np.float32)[None, :] * 128 + np.arange(128, dtype=np.float32)[:, None]).copy()
    return m


_NC_CACHE = {}


def kernel(**inputs):
    S = 4096
    nb = 8
    if "nc" not in _NC_CACHE:
        _NC_CACHE["nc"] = build(S, DEPTH)
    nc = _NC_CACHE["nc"]
    inp = {k_: np.asarray(v) for k_, v in inputs.items()}
    shared = make_in_map(inp, 0, S)
    in_maps = []
    for b in range(nb):
        m = dict(shared)
        m["x"] = np.ascontiguousarray(inp["x"][b], dtype=np.float32)
        m["cT"] = np.ascontiguousarray(inp["c"][b].reshape(8, 128).T, dtype=np.float32)
        in_maps.append(m)
    res = run_bass_kernel_spmd(nc, in_maps, core_ids=list(range(nb)))
    return np.stack([np.asarray(r["out"], dtype=np.float32) for r in res.results], axis=0)
```

```python
import math
from contextlib import ExitStack
import numpy as np
import concourse.bass as bass
import concourse.mybir as mybir
from concourse.bass_utils import run_bass_kernel_spmd

F32 = mybir.dt.float32
BF16 = mybir.dt.bfloat16
I32 = mybir.dt.int32
AF = mybir.ActivationFunctionType
ALU = mybir.AluOpType
AX = mybir.AxisListType

D = 1024
DEPTH = 4
NH_A = 8
NH_B = 4
DK_B = 128
DV_B = 256
RANK = 16
TAU = 16.0
CH = 64
NG = 4
EPG = 8
NE = 32
DEXP = 512
EPS = 1e-5
ALPHA = (2.0 * DEPTH) ** 0.25
D_IN = 8208
C_QA, C_KA, C_VA, C_QB, C_KB, C_VB, C_GB, C_AB, C_GT = 0, 1024, 2048, 3072, 3584, 4096, 5120, 6144, 6160
BLK = 256

ENGINES = ("pe", "act", "dve", "pool", "sp")
SEM_ROT = 30000


class Buf:
    __slots__ = ("name", "lw", "readers", "dcount")

    def __init__(self, name):
        self.name = name
        self.lw = None
        self.readers = []
        self.dcount = 0


class Ins:
    __slots__ = ("eng", "fn", "deps", "is_dma", "sig", "tok", "part", "dbuf", "barrier")

    def __init__(self, eng, fn, is_dma=False, part=False):
        self.eng = eng
        self.fn = fn
        self.deps = []
        self.is_dma = is_dma
        self.sig = False
        self.tok = None
        self.part = part
        self.dbuf = None
        self.barrier = None


class Prog:
    def __init__(self):
        self.ins = []
        self.bufs = []
        self.last_on = {e: None for e in ENGINES}

    def buf(self, name="b"):
        b = Buf(name)
        self.bufs.append(b)
        return b

    def _deps(self, I, reads, writes):
        deps = []
        for b in reads:
            if b.lw is not None:
                deps.append((b.lw, "raw"))
        for b in writes:
            if b.lw is not None:
                if not (I.is_dma and I.part and b.lw.is_dma and b.lw.part and not b.readers):
                    deps.append((b.lw, "waw"))
            for r in b.readers:
                deps.append((r, "war"))
        out = []
        seen = set()
        for d, kind in deps:
            if d is I or id(d) in seen:
                continue
            if (not d.is_dma) and (not I.is_dma) and d.eng == I.eng:
                if I.eng == "pe":
                    continue
                if kind == "war":
                    continue
            seen.add(id(d))
            out.append(d)
        for d in out:
            d.sig = True
        I.deps = out
        for b in reads:
            b.readers.append(I)
        for b in writes:
            b.lw = I
            b.readers = []

    def op(self, eng, fn, reads=(), writes=()):
        I = Ins(eng, fn)
        self._deps(I, list(reads), list(writes))
        self.ins.append(I)
        self.last_on[eng] = I
        return I

    def dma(self, eng, fn, reads=(), writes=(), part=False, sem=None):
        writes = list(writes)
        assert len(writes) == 1
        I = Ins(eng, fn, is_dma=True, part=part)
        I.dbuf = sem if sem is not None else writes[0]
        self._deps(I, list(reads), writes)
        self.ins.append(I)
        return I

    def barrier(self):
        lasts = [self.last_on[e] for e in ENGINES if self.last_on[e] is not None]
        for d in lasts:
            d.sig = True
        for e in ENGINES:
            I = Ins(e, None)
            I.barrier = (list(lasts), None)
            self.ins.append(I)
        for b in self.bufs:
            b.lw = None
            b.readers = []

    def emit(self, nc, stack):
        esems = {e: [] for e in ENGINES}
        ecount = {e: 0 for e in ENGINES}
        nsem = [0]
        recs = []
        free = []
        brec = {}
        last_use = {}
        for idx, I in enumerate(self.ins):
            if I.is_dma:
                last_use[id(I.dbuf)] = idx

        def new_sem(name):
            nsem[0] += 1
            return stack.enter_context(nc.semaphore(name))

        for idx, I in enumerate(self.ins):
            if I.barrier is not None:
                I.barrier = (I.barrier[0], [(r[0], r[1]) for r in recs if r[1] > 0])
                for bid in list(brec.keys()):
                    if last_use.get(bid, -1) < idx:
                        free.append(brec.pop(bid))
                continue
            if I.is_dma:
                b = I.dbuf
                if id(b) not in brec:
                    if free:
                        brec[id(b)] = free.pop()
                    else:
                        r = [new_sem(f"d{nsem[0]}"), 0]
                        recs.append(r)
                        brec[id(b)] = r
                r = brec[id(b)]
                r[1] += 16
                I.tok = (r[0], r[1])
            elif I.sig:
                e = I.eng
                if not esems[e] or ecount[e] >= SEM_ROT:
                    esems[e].append(new_sem(f"e_{e}{len(esems[e])}"))
                    ecount[e] = 0
                ecount[e] += 1
                I.tok = (esems[e][-1], ecount[e])
        self.n_sems = nsem[0]
        streams = {e: [I for I in self.ins if I.eng == e] for e in ENGINES}

        def run_stream(e, eng):
            waited = {}

            def wait(tok):
                sem, val = tok
                k = id(sem)
                if waited.get(k, 0) >= val:
                    return
                waited[k] = val
                eng.wait_ge(sem, val)

            for I in streams[e]:
                if I.barrier is not None:
                    lasts, dcounts = I.barrier
                    for d in lasts:
                        if d.tok is not None and not (d.eng == e and e == "pe"):
                            wait(d.tok)
                    for hs, cnt in dcounts:
                        wait((hs, cnt))
                    continue
                for d in I.deps:
                    wait(d.tok)
                r = I.fn(eng)
                if I.tok is not None:
                    r.then_inc(I.tok[0], 16 if I.is_dma else 1)

        with nc.Block() as block:
            @block.tensor
            def _(eng):
                run_stream("pe", eng)

            @block.scalar
            def _(eng):
                run_stream("act", eng)

            @block.vector
            def _(eng):
                run_stream("dve", eng)

            @block.gpsimd
            def _(eng):
                run_stream("pool", eng)

            @block.sync
            def _(eng):
                run_stream("sp", eng)


class Arena:
    def __init__(self, ap2d, n):
        self.ap = ap2d
        self.n = n
        self.off = 0
        self.mark = 0

    def reset(self):
        self.off = self.mark

    def keep(self):
        self.mark = self.off

    def push(self):
        self.stack = getattr(self, "stack", [])
        self.stack.append(self.mark)
        self.mark = self.off

    def pop(self):
        self.mark = self.stack.pop()

    def alloc(self, n, parts=128):
        assert self.off + n <= self.n, f"arena overflow {self.off}+{n}>{self.n}"
        a = self.ap[0:parts, self.off:self.off + n]
        self.off += n
        return a

    def alloc3(self, a, b, parts=128):
        return self.alloc(a * b, parts).rearrange("p (a b) -> p a b", b=b)


class K:
    pass


def slopes():
    return [2.0 ** (-8.0 * (i + 1) / NH_A) for i in range(NH_A)]


def host_consts(S):
    c = {}
    c["ident"] = np.eye(128, dtype=np.float32)
    ki = np.arange(128)[:, None]
    qi = np.arange(128)[None, :]
    c["tri"] = (qi >= ki).astype(np.float32)
    c["ustrict"] = (ki < qi).astype(np.float32)
    s64 = np.arange(64)[:, None]
    t64 = np.arange(64)[None, :]
    c["gmask"] = (s64 <= t64).astype(np.float32)
    qpos = np.arange(S) % 512
    c["qaug"] = np.stack([qpos // 16, qpos % 16, np.ones(S)]).astype(np.float32)
    kpos = np.arange(S) % 128
    sl = np.array(slopes())
    c["kaug"] = np.stack([np.stack([np.full(S, -128.0 * s), np.full(S, -8.0 * s), 8.0 * s * kpos])
                          for s in sl]).astype(np.float32)
    dl = np.arange(-3, 33)
    c["biascol"] = np.broadcast_to((-sl[:, None] * 128.0 * dl[None, :]).reshape(1, -1), (128, 8 * 36)).astype(np.float32).copy()
    return c


def build(S, L=DEPTH, dbg=(), phases="all"):
    nc = bass.Bass("TRN2", target_bir_lowering=False)
    NT = S // 128
    NQB = S // 512
    NCH = S // CH
    NSLOT = ((2 * S + NE * (BLK - 1)) // BLK + 1) * BLK
    NBLK = NSLOT // BLK
    P = Prog()
    st = ExitStack()

    def inp(name, shape, dt=F32):
        return nc.dram_tensor(name, list(shape), dt, kind="ExternalInput")

    def scratch(name, shape, dt):
        kind = "ExternalOutput" if name in dbg else "Internal"
        return nc.dram_tensor(name, list(shape), dt, kind=kind)

    x_in = inp("x", [S, D])
    cT = inp("cT", [128, 8])
    w_ada = inp("w_ada", [DEPTH, D, 6 * D])
    b_ada = inp("b_ada", [DEPTH, 6 * D])
    w_in = inp("w_in", [DEPTH, D, D_IN])
    b_gatesT = inp("b_gatesT", [DEPTH, 128, 16])
    w_alpha = inp("w_alpha", [DEPTH, RANK, 512])
    b_alphaT = inp("b_alphaT", [DEPTH, 128, 4])
    lam_in = inp("lam4", [DEPTH, 4, 64])
    diff_g = inp("diff_norm_g", [DEPTH, 128])
    gla_g = inp("gla_norm_g", [DEPTH, 256])
    w_ba = inp("w_branch_a", [DEPTH, D, D])
    w_bb = inp("w_branch_b", [DEPTH, D, D])
    w_out = inp("w_out", [DEPTH, D, D])
    ln1_g = inp("ln1_g", [DEPTH, D])
    ln1_b = inp("ln1_b", [DEPTH, D])
    w_r = inp("w_router", [DEPTH, D, 36])
    b_r = inp("b_router", [DEPTH, 36])
    w_ge = inp("w_gate_e", [DEPTH * NE * 128, 8 * DEXP])
    w_ue = inp("w_up_e", [DEPTH * NE * 128, 8 * DEXP])
    w_de = inp("w_down_e", [DEPTH * NE * 128, 4 * D])
    ln2_g = inp("ln2_g", [DEPTH, D])
    ln2_b = inp("ln2_b", [DEPTH, D])
    c_ident = inp("ident", [128, 128])
    c_tri = inp("tri", [128, 128])
    c_ustrict = inp("ustrict", [128, 128])
    c_gmask = inp("gmask", [64, 64])
    c_qaug = inp("qaug", [3, S])
    c_kaug = inp("kaug", [8, 3, S])
    c_biascol = inp("biascol", [128, 8 * 36])
    c_iota = inp("iota_p", [128, 1])
    out = nc.dram_tensor("out", [S, D], F32, kind="ExternalOutput")

    xA = scratch("xA", [S, D], F32)
    xB = scratch("xB", [S, D], F32)
    adaD = scratch("adaD", [DEPTH, 6 * D], F32)
    qaT = scratch("qaT", [1024, S], BF16)
    kaT = scratch("kaT", [1024, S], BF16)
    qbT = scratch("qbT", [512, S], BF16)
    kbT = scratch("kbT", [512, S], BF16)
    abT = scratch("abT", [16, S], BF16)
    gtT = scratch("gtT", [2048, S], BF16)
    va = scratch("va", [S, 1024], BF16)
    vb = scratch("vb", [S, 1024], BF16)
    gb = scratch("gb", [S, 1024], BF16)
    oaT = scratch("oaT", [1024, S], BF16)
    obT = scratch("obT", [1024, S], BF16)
    u2d = scratch("u2d", [S, D], BF16)
    XB = scratch("XB", [NSLOT, D], BF16)
    YB = scratch("YB", [NSLOT, D], F32)

    NF, NBF, NI = 22016, 57 * 1024, 1024
    arf = Arena(st.enter_context(nc.sbuf_tensor("arena_f", [128, NF], F32))[:, :], NF)
    arb = Arena(st.enter_context(nc.sbuf_tensor("arena_b", [128, NBF], BF16))[:, :], NBF)
    ari = Arena(st.enter_context(nc.sbuf_tensor("arena_i", [128, NI], I32))[:, :], NI)
    psf_t = st.enter_context(nc.psum_tensor("psf", [128, 6, 512], F32))
    psb_t = st.enter_context(nc.psum_tensor("psb", [128, 2, 1024], BF16))
    psf = [psf_t[:, i, :] for i in range(6)]
    psb = [psb_t[:, i, :] for i in range(2)]
    Bpsf = [P.buf(f"psf{i}") for i in range(6)]
    Bpsb = [P.buf(f"psb{i}") for i in range(2)]

    def B(name="b"):
        return P.buf(name)

    def mm(o, lhsT, rhs, start, stop, reads, writes):
        P.op("pe", lambda e: e.matmul(o, lhsT=lhsT, rhs=rhs, start=start, stop=stop), reads, writes)

    def tr(o, in_, ident, reads, writes):
        P.op("pe", lambda e: e.transpose(o, in_, ident), reads, writes)

    def act(o, in_, func, reads, writes, bias=None, scale=None, accum_out=None):
        kw = {}
        if bias is not None:
            kw["bias"] = bias
        if scale is not None:
            kw["scale"] = scale
        if accum_out is not None:
            kw["accum_out"] = accum_out
        P.op("act", lambda e: e.activation(out=o, in_=in_, func=func, **kw), reads, writes)

    def tt(eng, o, in0, in1, op, reads, writes):
        P.op(eng, lambda e: e.tensor_tensor(out=o, in0=in0, in1=in1, op=op), reads, writes)

    def ts(eng, o, in0, s1, s2, op0, op1, reads, writes, accum_out=None):
        kw = {}
        if accum_out is not None:
            kw["accum_out"] = accum_out
        if s2 is None:
            P.op(eng, lambda e: e.tensor_scalar(out=o, in0=in0, scalar1=s1, scalar2=None, op0=op0, **kw), reads, writes)
        else:
            P.op(eng, lambda e: e.tensor_scalar(out=o, in0=in0, scalar1=s1, scalar2=s2, op0=op0, op1=op1, **kw), reads, writes)

    def stt(eng, o, in0, scalar, in1, op0, op1, reads, writes):
        P.op(eng, lambda e: e.scalar_tensor_tensor(out=o, in0=in0, scalar=scalar, in1=in1, op0=op0, op1=op1), reads, writes)

    def cp(eng, o, in_, reads, writes):
        if eng == "act":
            P.op("act", lambda e: e.activation(out=o, in_=in_, func=AF.Copy), reads, writes)
        else:
            P.op(eng, lambda e: e.tensor_copy(out=o, in_=in_), reads, writes)

    def ld(q, o, in_, reads, writes, part=False, sem=None):
        P.dma(q, lambda e: e.dma_start(out=o, in_=in_), reads, writes, part=part, sem=sem)

    def rmax(o, in_, reads, writes):
        P.op("dve", lambda e: e.reduce_max(out=o, in_=in_, axis=AX.X), reads, writes)

    def rsum(o, in_, reads, writes):
        P.op("dve", lambda e: e.reduce_sum(out=o, in_=in_, axis=AX.X), reads, writes)

    def recip(o, in_, reads, writes):
        P.op("dve", lambda e: e.reciprocal(out=o, in_=in_), reads, writes)

    def memset(eng, o, val, writes):
        P.op(eng, lambda e: e.memset(o, val), (), writes)

    epsc = None

    def rsqrt_(o, in_, scale, Bt):
        act(o, in_, AF.Sqrt, [Bt, Bc2[7]], [Bt], bias=epsc[0:o.shape[0], :], scale=scale)
        P.op("dve", lambda e: e.reciprocal(out=o, in_=o), [Bt], [Bt])

    def ln_stats(xt, Bx, tmpf, Bt, tag=""):
        stats = tmpf[:, 0:12].rearrange("p (a b) -> p a b", b=6)
        mv = tmpf[:, 12:14]
        for hh in range(2):
            P.op("dve", (lambda h2: (lambda e: e.bn_stats(out=stats[:, h2, :], in_=xt[:, h2 * 512:(h2 + 1) * 512])))(hh), [Bx], [Bt])
        P.op("dve", lambda e: e.bn_aggr(out=mv, in_=stats), [Bt], [Bt])
        rstd = tmpf[:, 14:15]
        nmr = tmpf[:, 15:16]
        rsqrt_(rstd, mv[:, 1:2], 1.0, Bt)
        stt("dve", nmr, mv[:, 0:1], -1.0, rstd, ALU.mult, ALU.mult, [Bt], [Bt])
        return rstd, nmr

    ident_b = arb.alloc(128)
    ident_f = arf.alloc(128)
    tri_b = arb.alloc(128)
    ustr_b = arb.alloc(128)
    ones_b = arb.alloc(128)
    gmask_f = arf.alloc(64, parts=64)
    biascol = arf.alloc(8 * 36)
    iota_p = arf.alloc(1)
    Bconst = B("const")
    Bc2 = [B("c2_%d" % i) for i in range(8)]
    ld("pool", ident_b, c_ident[:, :], [], [Bc2[0]])
    ld("sp", ident_f, c_ident[:, :], [], [Bc2[1]])
    ld("pool", tri_b, c_tri[:, :], [], [Bc2[2]])
    ld("pool", ustr_b, c_ustrict[:, :], [], [Bc2[3]])
    ld("sp", gmask_f, c_gmask[:, :], [], [Bc2[4]])
    ld("sp", biascol, c_biascol[:, :], [], [Bc2[5]])
    ld("sp", iota_p, c_iota[:, :], [], [Bc2[6]])
    memset("dve", ones_b, 1.0, [Bc2[7]])
    epsc = arf.alloc(1)
    memset("dve", epsc, EPS, [Bc2[7]])
    arf.keep(); arb.keep(); ari.keep()
    P.barrier()
    CONST = Bc2

    def phase_reset():
        P.barrier()
        arf.reset(); arb.reset(); ari.reset()

    def phase_ada():
        ct = arf.alloc(8)
        condT = arf.alloc(8)
        row = arf.alloc(6 * D, parts=1)
        brow = arf.alloc(6 * D, parts=1)
        wt = [arf.alloc3(8, 512) for _ in range(2)]
        Bct, Brow, Bbrow = B(), B(), B()
        Bwt = [B(), B()]
        ld("sp", ct, cT[:, :], [], [Bct])
        act(condT, ct, AF.Silu, [Bct], [Bct])
        for l in range(L):
            ld("sp", brow, b_ada[l:l + 1, :], [], [Bbrow])
            for nb in range(12):
                w = wt[nb % 2]
                Bw = Bwt[nb % 2]
                ld("sp", w, w_ada[l, :, nb * 512:(nb + 1) * 512].rearrange("(kc p) n -> p kc n", p=128), [], [Bw])
                for kc in range(8):
                    mm(psf[nb % 2][0:1, :], condT[:, kc:kc + 1], w[:, kc, :], kc == 0, kc == 7, [Bct, Bw], [Bpsf[nb % 2]])
                tt("dve", row[:, nb * 512:(nb + 1) * 512], psf[nb % 2][0:1, :], brow[:, nb * 512:(nb + 1) * 512], ALU.add,
                   [Bpsf[nb % 2], Bbrow], [Brow])
            for c0 in (1024, 4096):
                ts("dve", row[:, c0:c0 + 1024], row[:, c0:c0 + 1024], 1.0, None, ALU.add, None, [Brow], [Brow])
            ld("sp", adaD[l:l + 1, :], row, [Brow], [Bada], part=True, sem=Brow)

    Bada = B("adaD")

    def phase_proj(l, xsrc):
        uT = arb.alloc3(8, S)
        BuTs = [B("uT") for _ in range(NQB)]
        sc = arf.alloc(D)
        sh = arf.alloc(D)
        Bmod = B("mod")
        ld("sp", sc, adaD[l:l + 1, 1024:2048].partition_broadcast(128), [Bada], [Bmod], part=True)
        ld("sp", sh, adaD[l:l + 1, 0:1024].partition_broadcast(128), [Bada], [Bmod], part=True)
        xt = [arf.alloc(D) for _ in range(2)]
        Bxt = [B(), B()]
        xn = [arf.alloc(D) for _ in range(2)]
        Bxn = [B(), B()]
        ub = [arb.alloc(D) for _ in range(2)]
        Bub = [B(), B()]
        tmp = [arf.alloc(16) for _ in range(2)]
        Btmp = [B(), B()]
        Bx = B("xsrc")
        for t in range(NT):
            i = t % 2
            ld("sp", xt[i], xsrc[t * 128:(t + 1) * 128, :], [Bx], [Bxt[i]])
            rstd, nmr = ln_stats(xt[i], Bxt[i], tmp[i], Btmp[i])
            act(xn[i], xt[i], AF.Identity, [Bxt[i], Btmp[i]], [Bxn[i]], bias=nmr, scale=rstd)
            tt("pool", xn[i], xn[i], sc, ALU.mult, [Bxn[i], Bmod], [Bxn[i]])
            tt("dve", ub[i], xn[i], sh, ALU.add, [Bxn[i], Bmod], [Bub[i]])
            pb = psb[t % 2]
            for kc in range(8):
                tr(pb[:, kc * 128:(kc + 1) * 128], ub[i][:, kc * 128:(kc + 1) * 128], ident_b, [Bub[i]], [Bpsb[t % 2]])
            cp("act", uT[:, :, t * 128:(t + 1) * 128], pb.rearrange("p (a b) -> p a b", b=128), [Bpsb[t % 2]], [BuTs[t // 4]])

        wblk = [arb.alloc3(8, 512) for _ in range(2)]
        Bw = [B(), B()]
        stg = [arb.alloc(S) for _ in range(2)]
        Bstg = [B(), B()]
        stt_ = [arb.alloc3(4, 512) for _ in range(2)]
        Bstt = [B(), B()]
        bgT = arf.alloc(16)
        Bbg = B()
        ld("sp", bgT, b_gatesT[l, :, :], [], [Bbg])
        wab = arb.alloc3(8, 16)
        Bwab = B()
        blocks = []
        for j in range(2):
            blocks.append((C_QA + 512 * j, "fm", qaT, 512 * j, None))
        for j in range(2):
            blocks.append((C_KA + 512 * j, "fm", kaT, 512 * j, None))
        blocks.append((C_QB, "fm", qbT, 0, None))
        blocks.append((C_KB, "fm", kbT, 0, None))
        for j in range(4):
            blocks.append((C_GT + 512 * j, "fm", gtT, 512 * j, "sig"))
        for j in range(2):
            blocks.append((C_VA + 512 * j, "tm", va, 512 * j, None))
        for j in range(2):
            blocks.append((C_VB + 512 * j, "tm", vb, 512 * j, None))
        for j in range(2):
            blocks.append((C_GB + 512 * j, "tm", gb, 512 * j, "silu"))
        Bdst = {id(t_): B() for t_ in (qaT, kaT, qbT, kbT, gtT, va, vb, gb, abT)}
        pi = 0
        si = 0
        for bi, (c0, kind, dst, d0, fn) in enumerate(blocks):
            w = wblk[bi % 2]
            ld("pool", w, w_in[l, :, c0:c0 + 512].rearrange("(kc p) n -> p kc n", p=128), [], [Bw[bi % 2]])
            if kind == "fm":
                for nch in range(4):
                    sg = stg[si % 2]
                    Bs = Bstg[si % 2]
                    si += 1
                    for tb in range(NQB):
                        pp = pi % 4
                        pi += 1
                        for kc in range(8):
                            mm(psf[pp], w[:, kc, nch * 128:(nch + 1) * 128], uT[:, kc, tb * 512:(tb + 1) * 512],
                               kc == 0, kc == 7, [Bw[bi % 2], BuTs[tb]], [Bpsf[pp]])
                        if fn == "sig":
                            gi = (d0 + nch * 128) // 128
                            act(sg[:, tb * 512:(tb + 1) * 512], psf[pp], AF.Sigmoid, [Bpsf[pp], Bbg], [Bs], bias=bgT[:, gi:gi + 1])
                        elif tb % 2 == 0:
                            cp("dve", sg[:, tb * 512:(tb + 1) * 512], psf[pp], [Bpsf[pp]], [Bs])
                        else:
                            cp("act", sg[:, tb * 512:(tb + 1) * 512], psf[pp], [Bpsf[pp]], [Bs])
                    r0 = d0 + nch * 128
                    ld("sp", dst[r0:r0 + 128, :], sg, [Bs], [Bdst[id(dst)]], part=True, sem=Bs)
            else:
                for t4 in range(NT // 4):
                    sg = stt_[si % 2]
                    Bs = Bstt[si % 2]
                    si += 1
                    for tq in range(4):
                        t = t4 * 4 + tq
                        pp = pi % 4
                        pi += 1
                        for kc in range(8):
                            mm(psf[pp], uT[:, kc, t * 128:(t + 1) * 128], w[:, kc, :], kc == 0, kc == 7,
                               [Bw[bi % 2], BuTs[t // 4]], [Bpsf[pp]])
                        if fn == "silu":
                            act(sg[:, tq, :], psf[pp], AF.Silu, [Bpsf[pp]], [Bs])
                        elif tq % 2 == 0:
                            cp("dve", sg[:, tq, :], psf[pp], [Bpsf[pp]], [Bs])
                        else:
                            cp("act", sg[:, tq, :], psf[pp], [Bpsf[pp]], [Bs])
                    ld("sp", dst[t4 * 512:(t4 + 1) * 512, d0:d0 + 512].rearrange("(a p) n -> p a n", p=128), sg, [Bs],
                       [Bdst[id(dst)]], part=True, sem=Bs)
        ld("pool", wab, w_in[l, :, C_AB:C_AB + 16].rearrange("(kc p) n -> p kc n", p=128), [], [Bwab])
        sg = stg[si % 2]
        Bs = Bstg[si % 2]
        for tb in range(NQB):
            pp = pi % 4
            pi += 1
            for kc in range(8):
                mm(psf[pp][0:16, :], wab[:, kc, :], uT[:, kc, tb * 512:(tb + 1) * 512], kc == 0, kc == 7, [Bwab, BuTs[tb]], [Bpsf[pp]])
            cp("dve", sg[0:16, tb * 512:(tb + 1) * 512], psf[pp][0:16, :], [Bpsf[pp]], [Bs])
        ld("sp", abT[:, :], sg[0:16, :], [Bs], [Bdst[id(abT)]], sem=Bs)

    BoaT = B("oaT")

    def phase_attn(l):
        lam_init = 0.8 - 0.6 * math.exp(-0.3 * l)
        lv = arf.alloc(256)
        Blam = B("lam")
        ld("sp", lv, lam_in[l:l + 1, :, :].rearrange("a b c -> a (b c)").partition_broadcast(128), [], [Blam])
        lv4 = lv.rearrange("p (a b c) -> p a b c", a=2, b=2)
        pr = arf.alloc3(2, 64)
        s12 = arf.alloc(2)
        e12 = arf.alloc(2)
        neglam = arf.alloc(1)
        tt("dve", pr, lv4[:, :, 0, :], lv4[:, :, 1, :], ALU.mult, [Blam], [Blam])
        P.op("dve", lambda e: e.reduce_sum(out=s12, in_=pr, axis=AX.X), [Blam], [Blam])
        act(e12, s12, AF.Exp, [Blam], [Blam])
        tt("dve", neglam, e12[:, 0:1], e12[:, 1:2], ALU.subtract, [Blam], [Blam])
        ts("dve", neglam, neglam, lam_init, -1.0, ALU.add, ALU.mult, [Blam], [Blam])
        gA = arf.alloc(128)
        BgA = B("gA")
        ld("sp", gA, diff_g[l:l + 1, :].partition_broadcast(128), [], [BgA])
        ts("dve", gA, gA, 1.0 - lam_init, None, ALU.mult, None, [BgA], [BgA])

        QT = [[arb.alloc(S) for c in range(2)] for p in range(2)]
        KT = [[arb.alloc(S) for c in range(2)] for p in range(2)]
        VA = [arb.alloc3(NT, 129) for p in range(2)]
        BQ = [[B("Q") for c in range(2)] for p in range(2)]
        BK = [[B("K") for c in range(2)] for p in range(2)]
        BV = [B("V") for p in range(2)]
        for p in range(2):
            memset("pool", VA[p][:, :, 128:129], 1.0, [BV[p]])
        PT = [arb.alloc(512) for _ in range(3)]
        BPT = [B("PT") for _ in range(3)]
        o0 = [arf.alloc(128) for _ in range(4)]
        Bo0 = [B("o0") for _ in range(4)]
        sm = [arf.alloc(8) for _ in range(8)]
        Bsm = [B("sm") for _ in range(8)]
        of = [arf.alloc(128) for _ in range(2)]
        Bof = [B("of") for _ in range(2)]
        junk = arf.alloc(128)
        Bjunk = B("junk")
        obf = [arb.alloc(128) for _ in range(2)]
        Bobf = [B("obf") for _ in range(2)]
        ostg = [arb.alloc(512) for _ in range(2)]
        Bostg = [B("ostg") for _ in range(2)]
        Bsrc = B("attn_src")

        def load_head(h):
            p = h % 2
            for c in range(2):
                r0 = h * 128 + c * 64
                ld("sp", QT[p][c][0:64, :], qaT[r0:r0 + 64, :], [Bsrc], [BQ[p][c]], part=True)
                ld("pool", QT[p][c][64:67, :], c_qaug[:, :], [], [BQ[p][c]], part=True)
                ld("sp", KT[p][c][0:64, :], kaT[r0:r0 + 64, :], [Bsrc], [BK[p][c]], part=True)
                ld("pool", KT[p][c][64:67, :], c_kaug[h, :, :], [], [BK[p][c]], part=True)
            ld("sp", VA[p][:, :, 0:128], va[:, h * 128:(h + 1) * 128].rearrange("(t p) d -> p t d", p=128), [Bsrc], [BV[p]])

        load_head(0)
        sl_ = slopes()
        jobs = []
        for h in range(NH_A):
            dmax = int((60.0 / sl_[h] + 127.0) // 128.0)
            for qb in range(NQB):
                for c in range(2):
                    k0 = max(0, 4 * qb - dmax)
                    for kt in range(k0, 4 * (qb + 1)):
                        jobs.append((h, qb, c, kt, k0))
        state = {"pti": 0, "si": 0}
        Sbank = [psf[0], psf[1], psb[1].bitcast(F32)]
        BSbank = [Bpsf[0], Bpsf[1], Bpsb[1]]
        PT4 = PT + [arb.alloc(512)]
        BPT4 = BPT + [B("PT")]
        of4 = [arf.alloc(128) for _ in range(4)]
        Bof4 = [B("of4") for _ in range(4)]
        ssq = [arf.alloc(8) for _ in range(2)]
        Bssq = [B("ssq") for _ in range(2)]
        LA = 2

        def issue_S(job):
            h, qb, c, kt, k0 = job
            p = h % 2
            j = kt - 4 * qb
            col0 = 128 * max(j, 0)
            sb_ = state["si"] % 3
            state["si"] += 1
            mm(Sbank[sb_][:, col0:512], KT[p][c][0:67, kt * 128:(kt + 1) * 128],
               QT[p][c][0:67, qb * 512 + col0:(qb + 1) * 512], True, True, [BK[p][c], BQ[p][c]], [BSbank[sb_]])
            pt = PT4[state["pti"] % 4]
            Bp = BPT4[state["pti"] % 4]
            state["pti"] += 1
            bi_ = h * 36 + (4 * qb - kt + 3)
            act(pt[:, col0:512], Sbank[sb_][:, col0:512], AF.Exp, [BSbank[sb_]], [Bp],
                bias=biascol[:, bi_:bi_ + 1], scale=0.125)
            if j >= 0:
                tt("pool", pt[:, col0:col0 + 128], pt[:, col0:col0 + 128], tri_b, ALU.mult, [Bp], [Bp])
            return pt, Bp

        loaded = {0}
        pend = [issue_S(jobs[i_]) for i_ in range(min(LA, len(jobs)))]
        gi = 0
        for ji, job in enumerate(jobs):
            h, qb, c, kt, k0 = job
            p = h % 2
            if h + 1 < NH_A and (h + 1) not in loaded:
                load_head(h + 1)
                loaded.add(h + 1)
            pt, Bp = pend.pop(0)
            if ji + LA < len(jobs):
                pend.append(issue_S(jobs[ji + LA]))
            j = kt - 4 * qb
            Ob = [2 + 2 * c, 3 + 2 * c]
            O = [psf[Ob[qt // 2]][:, (qt % 2) * 256:(qt % 2) * 256 + 129] for qt in range(4)]
            for qt in range(max(j, 0), 4):
                mm(O[qt], pt[:, qt * 128:(qt + 1) * 128], VA[p][:, kt, :], kt == k0 and qt % 2 == 0, kt == 4 * qb + qt,
                   [Bp, BV[p]], [Bpsf[Ob[qt // 2]]])
            if kt != 4 * qb + 3:
                continue
            g_ = gi % 2
            sq = ssq[g_]
            Bq_ = Bssq[g_]
            if c == 0:
                for qt in range(4):
                    Bo = Bpsf[Ob[qt // 2]]
                    recip(sq[:, 4 + qt:5 + qt], O[qt][:, 128:129], [Bo], [Bq_])
                    ts("dve", o0[qt], O[qt][:, 0:128], sq[:, 4 + qt:5 + qt], None, ALU.mult, None, [Bo, Bq_], [Bo0[qt]])
                continue
            gi += 1
            for qt in range(4):
                Bo = Bpsf[Ob[qt // 2]]
                recip(sq[:, 4 + qt:5 + qt], O[qt][:, 128:129], [Bo], [Bq_])
                tt("dve", sq[:, 4 + qt:5 + qt], sq[:, 4 + qt:5 + qt], neglam, ALU.mult, [Bq_, Blam], [Bq_])
                stt("dve", of4[qt], O[qt][:, 0:128], sq[:, 4 + qt:5 + qt], o0[qt], ALU.mult, ALU.add, [Bo, Bq_, Bo0[qt]], [Bof4[qt]])
                tt("pool", junk, of4[qt], of4[qt], ALU.mult, [Bof4[qt]], [Bjunk])
                rsum(sq[:, qt:qt + 1], junk, [Bjunk], [Bq_])
            act(sq[:, 0:4], sq[:, 0:4], AF.Ln, [Bq_, Bc2[7]], [Bq_], bias=epsc, scale=1.0 / 128.0)
            act(sq[:, 0:4], sq[:, 0:4], AF.Exp, [Bq_], [Bq_], scale=-0.5)
            for qt in range(4):
                e_ = qt % 2
                stt("dve", obf[e_], of4[qt], sq[:, qt:qt + 1], gA, ALU.mult, ALU.mult, [Bof4[qt], Bq_, BgA], [Bobf[e_]])
                tr(psb[0][:, qt * 128:(qt + 1) * 128], obf[e_], ident_b, [Bobf[e_]], [Bpsb[0]])
            tb_ = gi % 2
            sg = ostg[tb_]
            cp("dve", sg, psb[0][:, 0:512], [Bpsb[0]], [Bostg[tb_]])
            ld("sp", oaT[h * 128:(h + 1) * 128, qb * 512:(qb + 1) * 512], sg, [Bostg[tb_]], [BoaT],
               part=True, sem=Bostg[tb_])

    BobT = B("obT")

    def phase_gla(l):
        HP = NH_B
        G = 2
        NGRP = NCH // G
        Bsrc = B("gla_src")
        rmask = arf.alloc(S)
        Brm = B("rmask")
        memset("pool", rmask, 1.0, [Brm])
        memset("pool", rmask.rearrange("p (c t) -> p c t", t=CH)[:, :, 0:1], 0.0, [Brm])
        onec = arf.alloc(1)
        memset("dve", onec, 1.0, [Brm])
        gG = arf.alloc(256)
        ld("sp", gG, gla_g[l:l + 1, :].partition_broadcast(128), [], [Brm], part=True)
        bal = arf.alloc(4)
        Bbal = B("bal")
        ld("sp", bal, b_alphaT[l, :, :], [], [Bbal])
        ts("dve", bal, bal, -1.0, None, ALU.mult, None, [Bbal], [Bbal])
        abt = arb.alloc(S)
        Babt = B("abt")
        ld("sp", abt[0:16, :], abT[:, :], [Bsrc], [Babt])
        wal = arb.alloc(512)
        Bwal = B("wal")
        ld("pool", wal[0:16, :], w_alpha[l, :, :], [], [Bwal])
        cum = arf.alloc(S)
        Ee = arf.alloc(S)
        Ei = arf.alloc(S)
        Bcum, BE, BEi = B("cum"), B("E"), B("Ei")
        qtl = [arb.alloc(S) for _ in range(HP)]
        ktl = [arb.alloc(S) for _ in range(HP)]
        lastE = [arf.alloc(NCH) for _ in range(HP)]
        Sst = [arf.alloc(256) for _ in range(HP)]
        Sbf = [arb.alloc(256) for _ in range(HP)]
        Bq = [B("qt") for _ in range(HP)]
        Bk = [B("kt") for _ in range(HP)]
        BlE = [B("lastE") for _ in range(HP)]
        BS = [B("S") for _ in range(HP)]
        BSb = [B("Sb") for _ in range(HP)]
        for i in range(HP):
            h = i
            ld("sp", qtl[i], qbT[h * 128:(h + 1) * 128, :], [Bsrc], [Bq[i]])
            ld("sp", ktl[i], kbT[h * 128:(h + 1) * 128, :], [Bsrc], [Bk[i]])
        for i in range(HP):
            h = i
            for tb in range(NQB):
                pp = tb % 2
                mm(psf[pp], wal[0:16, h * 128:(h + 1) * 128], abt[0:16, tb * 512:(tb + 1) * 512], True, True,
                   [Bwal, Babt], [Bpsf[pp]])
                act(cum[:, tb * 512:(tb + 1) * 512], psf[pp], AF.Exp, [Bpsf[pp], Bbal], [Bcum], bias=bal[:, h:h + 1], scale=-1.0)
            act(cum, cum, AF.Ln, [Bcum, Brm], [Bcum], bias=onec, scale=1.0)
            P.op("dve", lambda e: e.tensor_tensor_scan(out=cum, data0=rmask, data1=cum, initial=0.0, op0=ALU.mult, op1=ALU.add),
                 [Bcum, Brm], [Bcum])
            act(Ee, cum, AF.Exp, [Bcum], [BE], scale=-1.0 / TAU)
            act(Ei, cum, AF.Exp, [Bcum], [BEi], scale=1.0 / TAU)
            stt("dve", qtl[i], qtl[i], DK_B ** -0.5, Ee, ALU.mult, ALU.mult, [Bq[i], BE], [Bq[i]])
            tt("pool", ktl[i], ktl[i], Ei, ALU.mult, [Bk[i], BEi], [Bk[i]])
            cp("dve", lastE[i], Ee.rearrange("p (c t) -> p c t", t=CH)[:, :, CH - 1], [BE], [BlE[i]])
        vt = [[arb.alloc3(G, 256) for _ in range(2)] for i in range(HP)]
        gt = [[arb.alloc3(G, 256) for _ in range(2)] for i in range(HP)]
        Bvt = [[B("vt") for _ in range(2)] for i in range(HP)]
        Bgt = [[B("gt") for _ in range(2)] for i in range(HP)]
        attm = [arb.alloc(64) for i in range(HP)]
        Battm = [B("attm") for i in range(HP)]
        khc = [arb.alloc(128) for i in range(HP)]
        Bkhc = [B("khc") for i in range(HP)]
        onf = [arf.alloc(256) for i in range(HP)]
        Bonf = [B("onf") for i in range(HP)]
        obc = [arb.alloc(256) for i in range(HP)]
        Bobc = [B("obc") for i in range(HP)]
        smg = [[arf.alloc(4) for _ in range(2)] for i in range(HP)]
        Bsmg = [[B("smg") for _ in range(2)] for i in range(HP)]
        junk = [arf.alloc(256) for _ in range(2)]
        Bjunk = [B("junk") for _ in range(2)]
        obst = [[arb.alloc3(2, G * CH) for _ in range(2)] for i in range(HP)]
        Bobst = [[B("obst") for _ in range(2)] for i in range(HP)]

        def load_group(g):
            for i in range(HP):
                h = i
                t0 = g * G * CH
                ld("sp", vt[i][g % 2][0:64], vb[t0:t0 + G * CH, h * 256:(h + 1) * 256].rearrange("(c p) f -> p c f", p=CH),
                   [Bsrc], [Bvt[i][g % 2]])
                ld("sp", gt[i][g % 2][0:64], gb[t0:t0 + G * CH, h * 256:(h + 1) * 256].rearrange("(c p) f -> p c f", p=CH),
                   [Bsrc], [Bgt[i][g % 2]])

        load_group(0)
        for g in range(NGRP):
            if g + 1 < NGRP:
                load_group(g + 1)
            for ci in range(G):
                c = g * G + ci
                for i in range(HP):
                    h = i
                    cs = slice(c * CH, (c + 1) * CH)
                    pa = c % 2
                    po = 2 + i // 2
                    ps_ = 4 + i // 2
                    oc = slice((i % 2) * 256, (i % 2) * 256 + 256)
                    v_c = vt[i][g % 2][0:64, ci, :]
                    Bv = Bvt[i][g % 2]
                    mm(psf[pa][0:64, i * 64:(i + 1) * 64], ktl[i][:, cs], qtl[i][:, cs], True, True, [Bk[i], Bq[i]], [Bpsf[pa]])
                    tt("dve", attm[i][0:64, :], psf[pa][0:64, i * 64:(i + 1) * 64], gmask_f, ALU.mult, [Bpsf[pa]], [Battm[i]])
                    tr(psb[0][0:64, i * 128:(i + 1) * 128], ktl[i][:, cs], ident_b, [Bk[i]], [Bpsb[0]])
                    cp("act", khc[i][0:64, :], psb[0][0:64, i * 128:(i + 1) * 128], [Bpsb[0]], [Bkhc[i]])
                    if c > 0:
                        mm(psf[po][0:64, oc], qtl[i][:, cs], Sbf[i], True, False, [Bq[i], BSb[i]], [Bpsf[po]])
                    mm(psf[po][0:64, oc], attm[i][0:64, :], v_c, c == 0, True, [Battm[i], Bv], [Bpsf[po]])
                    if c + 1 < NCH:
                        mm(psf[ps_][:, oc], khc[i][0:64, :], v_c, True, True, [Bkhc[i], Bv], [Bpsf[ps_]])
                        if c == 0:
                            ts("dve", Sst[i], psf[ps_][:, oc], lastE[i][:, c:c + 1], None, ALU.mult, None, [Bpsf[ps_], BlE[i]], [BS[i]])
                        else:
                            tt("dve", Sst[i], Sst[i], psf[ps_][:, oc], ALU.add, [BS[i], Bpsf[ps_]], [BS[i]])
                            ts("dve", Sst[i], Sst[i], lastE[i][:, c:c + 1], None, ALU.mult, None, [BS[i], BlE[i]], [BS[i]])
                        cp("pool", Sbf[i], Sst[i], [BS[i]], [BSb[i]])
                    sm_ = smg[i][c % 2]
                    Bs_ = Bsmg[i][c % 2]
                    jk = junk[i % 2]
                    act(jk[0:64, :], psf[po][0:64, oc], AF.Square, [Bpsf[po]], [Bjunk[i % 2], Bs_], accum_out=sm_[0:64, 0:1])
                    act(sm_[0:64, 1:2], sm_[0:64, 0:1], AF.Ln, [Bs_, Bc2[7]], [Bs_], bias=epsc[0:64, :], scale=1.0 / 256.0)
                    act(sm_[0:64, 1:2], sm_[0:64, 1:2], AF.Exp, [Bs_], [Bs_], scale=-0.5)
                    stt("dve", onf[i][0:64, :], psf[po][0:64, oc], sm_[0:64, 1:2], gG[0:64, :], ALU.mult, ALU.mult,
                        [Bpsf[po], Bs_, Brm], [Bonf[i]])
                    tt("pool", obc[i][0:64, :], onf[i][0:64, :], gt[i][g % 2][0:64, ci, :], ALU.mult, [Bonf[i], Bgt[i][g % 2]], [Bobc[i]])
                    for j in range(2):
                        col = i * 256 + j * (G * CH) + ci * CH
                        tr(psb[1][:, col:col + CH], obc[i][0:64, j * 128:(j + 1) * 128], ident_b[0:64, 0:64], [Bobc[i]], [Bpsb[1]])
                    if ci == G - 1:
                        sg = obst[i][g % 2]
                        Bs2 = Bobst[i][g % 2]
                        cp("act", sg, psb[1][:, i * 256:(i + 1) * 256].rearrange("p (j t) -> p j t", t=G * CH), [Bpsb[1]], [Bs2])
                        t0 = g * G * CH
                        for j in range(2):
                            r0 = h * 256 + j * 128
                            ld("sp", obT[r0:r0 + 128, t0:t0 + G * CH], sg[:, j, :], [Bs2], [BobT], part=True, sem=Bs2)

    RT = {}

    def phase_merge(l, xcur, xmid):
        TB = 256
        NTB = S // TB
        Bsrc = B("mg_src")
        A1s = arf.alloc3(NT, 32); A2s = arf.alloc3(NT, 32); RK = arf.alloc3(NT, 32)
        W12 = arf.alloc3(NT, 2)
        Asum = arf.alloc(32)
        Asb = arb.alloc(32)
        BA1, BA2, BRK, BW12, BAs, BAsb = B("A1s"), B("A2s"), B("RK"), B("W12"), B("Asum"), B("Asb")
        RT.update(A1s=A1s, A2s=A2s, RK=RK, W12=W12, Asum=Asum, Asb=Asb, BA1=BA1, BA2=BA2, BRK=BRK, BW12=BW12, BAs=BAs, BAsb=BAsb)
        arf.push(); arb.push()
        wts = []
        Bwts = []
        for wi, wsrc in enumerate((w_ba, w_bb, w_out)):
            w_ = arb.alloc3(8, 1024)
            Bw_ = B("mgw")
            for kc in range(8):
                ld("pool", w_[:, kc, :], wsrc[l, kc * 128:(kc + 1) * 128, :], [], [Bw_], part=True)
            wts.append(w_)
            Bwts.append(Bw_)
        wba_, wbb_, wout_ = wts
        wr = arb.alloc3(8, 36)
        Bwr = B("wr")
        ld("pool", wr, w_r[l, :, :].rearrange("(kc p) n -> p kc n", p=128), [], [Bwr])
        Bbc = B("bcast")
        bc = {}
        for nm, src in (("g1", adaD[l:l + 1, 2048:3072]), ("sh2", adaD[l:l + 1, 3072:4096]), ("sc2", adaD[l:l + 1, 4096:5120]),
                        ("l1g", ln1_g[l:l + 1, :]), ("l1b", ln1_b[l:l + 1, :])):
            bc[nm] = arf.alloc(D)
            ld("sp", bc[nm], src.partition_broadcast(128), [Bada], [Bbc], part=True)
        brb = arf.alloc(36)
        ld("sp", brb, b_r[l:l + 1, :].partition_broadcast(128), [], [Bbc], part=True)
        oab = [arb.alloc3(8, TB) for _ in range(2)]
        obb = [arb.alloc3(8, TB) for _ in range(2)]
        gtb = [arb.alloc3(16, TB) for _ in range(2)]
        Bin = [B("mg_in") for _ in range(2)]
        mxT = [arb.alloc3(8, TB) for _ in range(2)]
        BmxT = [B("mxT") for _ in range(2)]
        m1 = [arf.alloc(TB) for _ in range(2)]
        Bm1 = [B("m1") for _ in range(2)]
        xt = [arf.alloc(D) for _ in range(2)]
        Bxt = [B("xt") for _ in range(2)]
        rr = [arf.alloc(D) for _ in range(2)]
        Brr = [B("rr") for _ in range(2)]
        tmp = [arf.alloc(16) for _ in range(4)]
        Btmp = [B("tmp") for _ in range(4)]
        u2b = [arb.alloc(D) for _ in range(2)]
        Bu2b = [B("u2b") for _ in range(2)]
        u2T = [arb.alloc3(8, 128) for _ in range(2)]
        Bu2T = [B("u2T") for _ in range(2)]
        rs = [arf.alloc(128) for _ in range(2)]
        Brs = [B("rs") for _ in range(2)]
        a12b = [arb.alloc(32) for _ in range(2)]
        Ba12 = [B("a12b") for _ in range(2)]
        Bxm = B("xmid")
        Bu2d = B("u2d")

        def load_blk(tb):
            i = tb % 2
            ld("sp", oab[i], oaT[:, tb * TB:(tb + 1) * TB].rearrange("(kc p) t -> p kc t", p=128), [Bsrc], [Bin[i]], part=True)
            ld("sp", obb[i], obT[:, tb * TB:(tb + 1) * TB].rearrange("(kc p) t -> p kc t", p=128), [Bsrc], [Bin[i]], part=True)
            ld("sp", gtb[i], gtT[:, tb * TB:(tb + 1) * TB].rearrange("(kc p) t -> p kc t", p=128), [Bsrc], [Bin[i]], part=True)

        load_blk(0)
        for tb in range(NTB):
            i = tb % 2
            if tb + 1 < NTB:
                load_blk(tb + 1)
            for nch in range(8):
                pA, pB = (2 * nch) % 4, (2 * nch + 1) % 4
                for kc in range(8):
                    mm(psf[pA][:, 0:TB], wba_[:, kc, nch * 128:(nch + 1) * 128], oab[i][:, kc, :], kc == 0, kc == 7, [Bwts[0], Bin[i]], [Bpsf[pA]])
                for kc in range(8):
                    mm(psf[pB][:, 0:TB], wbb_[:, kc, nch * 128:(nch + 1) * 128], obb[i][:, kc, :], kc == 0, kc == 7, [Bwts[1], Bin[i]], [Bpsf[pB]])
                mi = nch % 2
                tt("dve", m1[mi], psf[pA][:, 0:TB], gtb[i][:, nch, :], ALU.mult, [Bpsf[pA], Bin[i]], [Bm1[mi]])
                tt("dve", mxT[i][:, nch, :], psf[pB][:, 0:TB], gtb[i][:, 8 + nch, :], ALU.mult, [Bpsf[pB], Bin[i]], [BmxT[i]])
                tt("pool", mxT[i][:, nch, :], mxT[i][:, nch, :], m1[mi], ALU.add, [BmxT[i], Bm1[mi]], [BmxT[i]])
            for tq in range(TB // 128):
                t = tb * (TB // 128) + tq
                j = t % 2
                ld("sp", xt[j], xcur[t * 128:(t + 1) * 128, :], [Bsrc], [Bxt[j]])
                for half in range(2):
                    py = 4 + half
                    for kc in range(8):
                        mm(psf[py], mxT[i][:, kc, tq * 128:(tq + 1) * 128], wout_[:, kc, half * 512:(half + 1) * 512], kc == 0, kc == 7,
                           [BmxT[i], Bwts[2]], [Bpsf[py]])
                    tt("dve", rr[j][:, half * 512:(half + 1) * 512], psf[py], bc["g1"][:, half * 512:(half + 1) * 512], ALU.mult,
                       [Bpsf[py], Bbc], [Brr[j]])
                stt("dve", rr[j], xt[j], ALPHA, rr[j], ALU.mult, ALU.add, [Bxt[j], Brr[j]], [Brr[j]])
                rstd, nmr = ln_stats(rr[j], Brr[j], tmp[2 * j], Btmp[2 * j])
                act(rr[j], rr[j], AF.Identity, [Brr[j], Btmp[2 * j]], [Brr[j]], bias=nmr, scale=rstd)
                tt("pool", rr[j], rr[j], bc["l1g"], ALU.mult, [Brr[j], Bbc], [Brr[j]])
                tt("dve", xt[j], rr[j], bc["l1b"], ALU.add, [Brr[j], Bbc], [Bxt[j]])
                ld("sp", xmid[t * 128:(t + 1) * 128, :], xt[j], [Bxt[j]], [Bxm], part=True, sem=Bxt[j])
                rstd2, nmr2 = ln_stats(xt[j], Bxt[j], tmp[2 * j + 1], Btmp[2 * j + 1])
                act(rr[j], xt[j], AF.Identity, [Bxt[j], Btmp[2 * j + 1]], [Brr[j]], bias=nmr2, scale=rstd2)
                tt("pool", rr[j], rr[j], bc["sc2"], ALU.mult, [Brr[j], Bbc], [Brr[j]])
                tt("dve", u2b[j], rr[j], bc["sh2"], ALU.add, [Brr[j], Bbc], [Bu2b[j]])
                ld("sp", u2d[t * 128:(t + 1) * 128, :], u2b[j], [Bu2b[j]], [Bu2d], part=True, sem=Bu2b[j])
                for kc in range(8):
                    tr(psb[0][:, kc * 128:(kc + 1) * 128], u2b[j][:, kc * 128:(kc + 1) * 128], ident_b, [Bu2b[j]], [Bpsb[0]])
                cp("act", u2T[j], psb[0].rearrange("p (a b) -> p a b", b=128), [Bpsb[0]], [Bu2T[j]])
                RBk = psb[1].bitcast(F32)
                BRBk = Bpsb[1]
                for kc in range(8):
                    mm(RBk[:, 0:36], u2T[j][:, kc, :], wr[:, kc, :], kc == 0, kc == 7, [Bu2T[j], Bwr], [BRBk])
                R_ = rs[j]
                BR = Brs[j]
                lg = R_[:, 0:36]
                tt("dve", lg, RBk[:, 0:36], brb, ALU.add, [BRBk, Bbc], [BR])
                gl = lg[:, 0:4]
                el = lg[:, 4:36].rearrange("p (g e) -> p g e", e=8)
                gmax, ngmax, gsum, gw = R_[:, 36:37], R_[:, 37:38], R_[:, 38:39], R_[:, 39:40]
                ohg = R_[:, 40:44]
                gex = R_[:, 44:48]
                ein = R_[:, 48:56]
                ein2 = R_[:, 56:64]
                mk1 = R_[:, 64:72]
                mk2 = R_[:, 72:80]
                mx1, mx2, dd, ex, den, w1, w2 = (R_[:, 80 + q:81 + q] for q in range(7))
                a12 = R_[:, 90:122]
                rmax(gmax, gl, [BR], [BR])
                ts("dve", ohg, gl, gmax, None, ALU.is_equal, None, [BR], [BR])
                ts("dve", ngmax, gmax, -1.0, None, ALU.mult, None, [BR], [BR])
                act(gex, gl, AF.Exp, [BR], [BR], bias=ngmax, scale=1.0, accum_out=gsum)
                recip(gw, gsum, [BR], [BR])
                ts("dve", ein, el[:, 0, :], ohg[:, 0:1], None, ALU.mult, None, [BR], [BR])
                for g_ in range(1, 4):
                    stt("dve", ein, el[:, g_, :], ohg[:, g_:g_ + 1], ein, ALU.mult, ALU.add, [BR], [BR])
                rmax(mx1, ein, [BR], [BR])
                ts("dve", mk1, ein, mx1, None, ALU.is_equal, None, [BR], [BR])
                stt("dve", ein2, mk1, -1.0e30, ein, ALU.mult, ALU.add, [BR], [BR])
                rmax(mx2, ein2, [BR], [BR])
                ts("dve", mk2, ein2, mx2, None, ALU.is_equal, None, [BR], [BR])
                tt("dve", dd, mx2, mx1, ALU.subtract, [BR], [BR])
                act(ex, dd, AF.Exp, [BR], [BR])
                ts("dve", den, ex, 1.0, None, ALU.add, None, [BR], [BR])
                recip(w1, den, [BR], [BR])
                tt("dve", w2, ex, w1, ALU.mult, [BR], [BR])
                tt("dve", W12[:, t, 0:1], w1, gw, ALU.mult, [BR], [BW12])
                tt("dve", W12[:, t, 1:2], w2, gw, ALU.mult, [BR], [BW12])
                a1v = A1s[:, t, :].rearrange("p (g e) -> p g e", e=8)
                a2v = A2s[:, t, :].rearrange("p (g e) -> p g e", e=8)
                tt("dve", a1v, ohg.unsqueeze(2).to_broadcast([128, 4, 8]), mk1.unsqueeze(1).to_broadcast([128, 4, 8]), ALU.mult, [BR], [BA1])
                tt("dve", a2v, ohg.unsqueeze(2).to_broadcast([128, 4, 8]), mk2.unsqueeze(1).to_broadcast([128, 4, 8]), ALU.mult, [BR], [BA2])
                tt("dve", a12, A1s[:, t, :], A2s[:, t, :], ALU.add, [BA1, BA2], [BR])
                cp("dve", a12b[j], a12, [BR], [Ba12[j]])
                mm(RBk[:, 64:96], ustr_b, a12b[j], True, t == 0, [Ba12[j]], [BRBk])
                if t > 0:
                    mm(RBk[:, 64:96], ones_b, Asb, False, True, [BAsb], [BRBk])
                cp("dve", RK[:, t, :], RBk[:, 64:96], [BRBk], [BRK])
                if t == 0:
                    cp("dve", Asum, a12, [BR], [BAs])
                else:
                    tt("dve", Asum, Asum, a12, ALU.add, [BAs, BR], [BAs])
                cp("dve", Asb, Asum, [BAs], [BAsb])

    c_blkstart = inp("blkstart", [128, NBLK])
    c_iotaG = inp("iotaG", [128, 8])

    def phase_moe(l, xmid, xnext):
        A1s, A2s, RK, W12, Asb = RT["A1s"], RT["A2s"], RT["RK"], RT["W12"], RT["Asb"]
        Bg = B("moe_glob")
        cnt = arf.alloc(32); pad = arf.alloc(32); pend = arf.alloc(32); pst = arf.alloc(32); one32 = arf.alloc(32)
        PR = arf.alloc3(NT, 32)
        D1f = arf.alloc(NT); D2f = arf.alloc(NT)
        D1i = ari.alloc(NT); D2i = ari.alloc(NT)
        bst = arf.alloc(NBLK); ble = arf.alloc(NBLK)
        cmpb = arf.alloc3(NBLK, 32)
        iog = arf.alloc(8)
        ixGf = arf.alloc3(NBLK, 8); ixDf = arf.alloc3(NBLK, 4)
        ixG = ari.alloc3(NBLK, 8); ixD = ari.alloc3(NBLK, 4)
        ld("sp", bst, c_blkstart[:, :], [], [Bg], part=True)
        ld("sp", iog, c_iotaG[:, :], [], [Bg], part=True)
        memset("dve", one32, 1.0, [Bg])
        mm(psf[0][:, 0:32], ones_b, Asb, True, True, [], [Bpsf[0]])
        cp("dve", cnt, psf[0][:, 0:32], [Bpsf[0]], [Bg])
        KC = S // BLK + 1
        cmp2 = cmpb.rearrange("p a b -> p (a b)")[:, 0:32 * KC].rearrange("p (e k) -> p e k", k=KC)
        tt("dve", cmp2, cnt.unsqueeze(2).to_broadcast([128, 32, KC]), bst[:, 0:KC].unsqueeze(1).to_broadcast([128, 32, KC]), ALU.is_gt, [Bg], [Bg])
        P.op("dve", lambda e: e.reduce_sum(out=pad, in_=cmp2, axis=AX.X), [Bg], [Bg])
        ts("dve", pad, pad, float(BLK), None, ALU.mult, None, [Bg], [Bg])
        P.op("dve", lambda e: e.tensor_tensor_scan(out=pend, data0=one32, data1=pad, initial=0.0, op0=ALU.mult, op1=ALU.add), [Bg], [Bg])
        tt("dve", pst, pend, pad, ALU.subtract, [Bg], [Bg])
        tt("dve", PR, RK, pst.unsqueeze(1).to_broadcast([128, NT, 32]), ALU.add, [Bg], [Bg])
        tt("dve", A1s, A1s, PR, ALU.mult, [Bg], [Bg])
        tt("dve", A2s, A2s, PR, ALU.mult, [Bg], [Bg])
        P.op("dve", lambda e: e.reduce_sum(out=D1f, in_=A1s, axis=AX.X), [Bg], [Bg])
        P.op("dve", lambda e: e.reduce_sum(out=D2f, in_=A2s, axis=AX.X), [Bg], [Bg])
        cp("dve", D1i, D1f, [Bg], [Bg])
        cp("dve", D2i, D2f, [Bg], [Bg])
        tt("dve", cmpb, pend.unsqueeze(1).to_broadcast([128, NBLK, 32]), bst.unsqueeze(2).to_broadcast([128, NBLK, 32]), ALU.is_le, [Bg], [Bg])
        P.op("dve", lambda e: e.reduce_sum(out=ble, in_=cmpb, axis=AX.X), [Bg], [Bg])
        ts("dve", ble, ble, float(NE - 1), None, ALU.min, None, [Bg], [Bg])
        ts("dve", bst, ble, 128.0, float(l * NE * 128), ALU.mult, ALU.add, [Bg], [Bg])
        tt("dve", ixGf[:, :, 0], bst, iog[:, 0:1].to_broadcast([128, NBLK]), ALU.add, [Bg], [Bg])
        cp("dve", ixG[:, :, 0], ixGf[:, :, 0], [Bg], [Bg])
        if "dest" in dbg:
            for nm_, ap_, n_ in (("d_cnt", cnt, 32), ("d_pad", pad, 32), ("d_pend", pend, 32), ("d_pst", pst, 32),
                                 ("d_A1", A1s.rearrange("p a b -> p (a b)"), NT * 32), ("d_RK", RK.rearrange("p a b -> p (a b)"), NT * 32),
                                 ("d_PR", PR.rearrange("p a b -> p (a b)"), NT * 32), ("d_W12", W12.rearrange("p a b -> p (a b)"), NT * 2)):
                dt_ = nc.dram_tensor(nm_, [128, n_], F32, kind="ExternalOutput")
                ld("sp", dt_[:, :], ap_, [Bg], [B("dd")])
            dd_ = nc.dram_tensor("dest", [128, 2 * NT + NBLK], F32, kind="ExternalOutput")
            ld("sp", dd_[:, 0:NT], D1f, [Bg], [B("dd")])
            ld("sp", dd_[:, NT:2 * NT], D2f, [Bg], [B("dd")])
            ld("sp", dd_[:, 2 * NT:], ble, [Bg], [B("dd")])

        BXB = B("XB")
        Bu2src = B("u2src")
        ut = [arb.alloc(D) for _ in range(2)]
        But = [B("ut") for _ in range(2)]
        for t in range(NT):
            j = t % 2
            ld("sp", ut[j], u2d[t * 128:(t + 1) * 128, :], [Bu2src], [But[j]])
            for Di in (D1i, D2i):
                P.dma("pool", (lambda src_, off_: (lambda e: e.indirect_dma_start(
                    out=XB[:, :], out_offset=bass.IndirectOffsetOnAxis(ap=off_, axis=0), in_=src_, in_offset=None)))(ut[j], Di[:, t:t + 1]),
                    [But[j], Bg], [BXB], part=True, sem=But[j])
        P.barrier()

        NA = BLK // 128
        xb = [arb.alloc3(NA, D) for _ in range(2)]
        Bxb = [B("xb") for _ in range(2)]
        xbT = [arb.alloc3(8, BLK) for _ in range(2)]
        BxbT = [B("xbT") for _ in range(2)]
        NWB = 2
        Wg = [arb.alloc3(8, DEXP) for _ in range(NWB)]
        Wu = [arb.alloc3(8, DEXP) for _ in range(NWB)]
        Wd = [arb.alloc3(4, D) for _ in range(NWB)]
        BWg = [B("Wg") for _ in range(NWB)]
        BWu = [B("Wu") for _ in range(NWB)]
        BWd = [B("Wd") for _ in range(NWB)]
        sgf = [arf.alloc(BLK) for _ in range(2)]
        Bsgf = [B("sgf") for _ in range(2)]
        hT = [arb.alloc3(4, BLK) for _ in range(2)]
        BhT = [B("hT") for _ in range(2)]
        yst = [arf.alloc(D) for _ in range(2)]
        Byst = [B("yst") for _ in range(2)]
        BYB = B("YB")

        def gath(dst, src2d, off):
            return lambda e: e.indirect_dma_start(out=dst, out_offset=None, in_=src2d, in_offset=bass.IndirectOffsetOnAxis(ap=off, axis=0))

        def load_x(b):
            i = b % 2
            ld("sp", xb[i], XB[b * BLK:(b + 1) * BLK, :].rearrange("(a p) d -> p a d", p=128), [BXB], [Bxb[i]])

        def load_w(b):
            w_ = b % NWB
            P.dma("pool", gath(Wg[w_].rearrange("p a b -> p (a b)"), w_ge[:, :], ixG[:, b, 0:1]), [Bg], [BWg[w_]])
            P.dma("pool", gath(Wu[w_].rearrange("p a b -> p (a b)"), w_ue[:, :], ixG[:, b, 0:1]), [Bg], [BWu[w_]])
            P.dma("pool", gath(Wd[w_].rearrange("p a b -> p (a b)"), w_de[:, :], ixG[:, b, 0:1]), [Bg], [BWd[w_]])

        load_x(0)
        for b0 in range(min(NWB - 1, NBLK)):
            load_w(b0)
        ysi = 0
        for b in range(NBLK):
            i = b % 2
            wi = b % NWB
            if b + 1 < NBLK:
                load_x(b + 1)
            if b + NWB - 1 < NBLK:
                load_w(b + NWB - 1)
            for a in range(NA):
                for kc in range(8):
                    tr(psb[a % 2][:, kc * 128:(kc + 1) * 128], xb[i][:, a, kc * 128:(kc + 1) * 128], ident_b, [Bxb[i]], [Bpsb[a % 2]])
                cp("act" if a % 2 else "dve", xbT[i][:, :, a * 128:(a + 1) * 128], psb[a % 2].rearrange("p (k s) -> p k s", s=128), [Bpsb[a % 2]], [BxbT[i]])
            for hc in range(4):
                pg, pu = (2 * hc) % 4, (2 * hc + 1) % 4
                for kc in range(8):
                    mm(psf[pg][:, 0:BLK], Wg[wi][:, kc, hc * 128:(hc + 1) * 128], xbT[i][:, kc, :], kc == 0, kc == 7, [BWg[wi], BxbT[i]], [Bpsf[pg]])
                for kc in range(8):
                    mm(psf[pu][:, 0:BLK], Wu[wi][:, kc, hc * 128:(hc + 1) * 128], xbT[i][:, kc, :], kc == 0, kc == 7, [BWu[wi], BxbT[i]], [Bpsf[pu]])
                si_ = hc % 2
                act(sgf[si_], psf[pg][:, 0:BLK], AF.Silu, [Bpsf[pg]], [Bsgf[si_]])
                tt("dve", hT[i][:, hc, :], psf[pu][:, 0:BLK], sgf[si_], ALU.mult, [Bpsf[pu], Bsgf[si_]], [BhT[i]])
            for a in range(NA):
                ys = yst[ysi % 2]
                Bys = Byst[ysi % 2]
                ysi += 1
                for half in range(2):
                    py = 4 + half
                    for hc in range(4):
                        mm(psf[py], hT[i][:, hc, a * 128:(a + 1) * 128], Wd[wi][:, hc, half * 512:(half + 1) * 512], hc == 0, hc == 3,
                           [BhT[i], BWd[wi]], [Bpsf[py]])
                    cp("act" if half else "dve", ys[:, half * 512:(half + 1) * 512], psf[py], [Bpsf[py]], [Bys])
                r0 = b * BLK + a * 128
                ld("sp", YB[r0:r0 + 128, :], ys, [Bys], [BYB], part=True, sem=Bys)
        P.barrier()

        Bbc = B("bc2")
        bc = {}
        for nm, src in (("g2", adaD[l:l + 1, 5120:6144]), ("l2g", ln2_g[l:l + 1, :]), ("l2b", ln2_b[l:l + 1, :])):
            bc[nm] = arf.alloc(D)
            ld("sp", bc[nm], src.partition_broadcast(128), [], [Bbc], part=True)
        y1 = [arf.alloc(D) for _ in range(2)]
        y2 = [arf.alloc(D) for _ in range(2)]
        By1 = [B("y1") for _ in range(2)]
        By2 = [B("y2") for _ in range(2)]
        xt = [arf.alloc(D) for _ in range(2)]
        Bxt = [B("xt2") for _ in range(2)]
        tmp = [arf.alloc(16) for _ in range(2)]
        Btmp = [B("tmp2") for _ in range(2)]
        Bxn = B("xnext")
        Bxs = B("xmid_src")

        def load_t(t):
            j = t % 2
            ld("sp", xt[j], xmid[t * 128:(t + 1) * 128, :], [Bxs], [Bxt[j]])
            P.dma("pool", gath(y1[j], YB[:, :], D1i[:, t:t + 1]), [BYB, Bg], [By1[j]])
            P.dma("pool", gath(y2[j], YB[:, :], D2i[:, t:t + 1]), [BYB, Bg], [By2[j]])

        load_t(0)
        for t in range(NT):
            j = t % 2
            if t + 1 < NT:
                load_t(t + 1)
            ts("dve", y1[j], y1[j], W12[:, t, 0:1], None, ALU.mult, None, [By1[j]], [By1[j]])
            stt("dve", y1[j], y2[j], W12[:, t, 1:2], y1[j], ALU.mult, ALU.add, [By2[j], By1[j]], [By1[j]])
            tt("pool", y1[j], y1[j], bc["g2"], ALU.mult, [By1[j], Bbc], [By1[j]])
            stt("dve", y1[j], xt[j], ALPHA, y1[j], ALU.mult, ALU.add, [Bxt[j], By1[j]], [By1[j]])
            rstd, nmr = ln_stats(y1[j], By1[j], tmp[j], Btmp[j])
            act(y1[j], y1[j], AF.Identity, [By1[j], Btmp[j]], [By1[j]], bias=nmr, scale=rstd)
            tt("pool", y1[j], y1[j], bc["l2g"], ALU.mult, [By1[j], Bbc], [By1[j]])
            tt("dve", xt[j], y1[j], bc["l2b"], ALU.add, [By1[j], Bbc], [Bxt[j]])
            ld("sp", xnext[t * 128:(t + 1) * 128, :], xt[j], [Bxt[j]], [Bxn], part=True, sem=Bxt[j])
        arf.pop(); arb.pop()

    phase_ada()
    phase_reset()
    if phases == "all":
        xcur = x_in
        for l in range(L):
            phase_proj(l, xcur)
            phase_reset()
            phase_attn(l)
            phase_reset()
            phase_gla(l)
            phase_reset()
            phase_merge(l, xcur, xB)
            phase_reset()
            phase_moe(l, xB, out if l == L - 1 else xA)
            phase_reset()
            xcur = xA
    else:
        if "proj" in phases:
            phase_proj(0, x_in)
            phase_reset()
        if "attn" in phases:
            phase_attn(0)
            phase_reset()
        if "gla" in phases:
            phase_gla(0)
            phase_reset()
        if "merge" in phases:
            phase_merge(0, x_in, xB)
            phase_reset()
        if "moe" in phases:
            phase_moe(0, xB, out)
            phase_reset()
    P.emit(nc, st)
    st.close()
    nc._prog = P
    return nc


def make_in_map(inp, b, S):
    f = lambda a: np.ascontiguousarray(np.asarray(a, dtype=np.float32))
    m = {}
    m["x"] = f(inp["x"][b, :S])
    m["cT"] = f(np.asarray(inp["c"][b]).reshape(8, 128).T)
    for k_ in ("w_ada", "b_ada", "w_in", "w_alpha", "diff_norm_g", "gla_norm_g", "w_branch_a", "w_branch_b",
               "w_out", "ln1_g", "ln1_b", "ln2_g", "ln2_b"):
        m[k_] = f(inp[k_])
    m["b_gatesT"] = f(np.asarray(inp["b_gates"]).reshape(DEPTH, 16, 128).transpose(0, 2, 1))
    m["b_alphaT"] = f(np.asarray(inp["b_alpha"]).reshape(DEPTH, 4, 128).transpose(0, 2, 1))
    m["lam4"] = f(np.stack([np.asarray(inp[k_]) for k_ in ("lambda_q1", "lambda_k1", "lambda_q2", "lambda_k2")], axis=1))
    m["w_router"] = f(np.concatenate([np.asarray(inp["w_router_g"]), np.asarray(inp["w_router_e"])], axis=2))
    m["b_router"] = f(np.concatenate([np.asarray(inp["b_router_g"]), np.asarray(inp["b_router_e"])], axis=1))
    m["w_gate_e"] = f(np.asarray(inp["w_gate_e"]).reshape(DEPTH, NE, 8, 128, DEXP).transpose(0, 1, 3, 2, 4)).reshape(DEPTH * NE * 128, 8 * DEXP)
    m["w_up_e"] = f(np.asarray(inp["w_up_e"]).reshape(DEPTH, NE, 8, 128, DEXP).transpose(0, 1, 3, 2, 4)).reshape(DEPTH * NE * 128, 8 * DEXP)
    m["w_down_e"] = f(np.asarray(inp["w_down_e"]).reshape(DEPTH, NE, 4, 128, D).transpose(0, 1, 3, 2, 4)).reshape(DEPTH * NE * 128, 4 * D)
    m.update(host_consts(S))
    m["iota_p"] = np.arange(128, dtype=np.float32).reshape(128, 1)
    nslot = ((2 * S + NE * (BLK - 1)) // BLK + 1) * BLK
    nblk = nslot // BLK
    m["blkstart"] = np.broadcast_to((np.arange(nblk, dtype=np.float32) * BLK)[None, :], (128, nblk)).copy()
    m["iotaG"] = (np.arange(8, dtype=np.float32)[None, :] * 128 + np.arange(128, dtype=np.float32)[:, None]).copy()
    return m


_NC_CACHE = {}


def kernel(**inputs):
    S = 4096
    nb = 8
    if "nc" not in _NC_CACHE:
        _NC_CACHE["nc"] = build(S, DEPTH)
    nc = _NC_CACHE["nc"]
    inp = {k_: np.asarray(v) for k_, v in inputs.items()}
    shared = make_in_map(inp, 0, S)
    in_maps = []
    for b in range(nb):
        m = dict(shared)
        m["x"] = np.ascontiguousarray(inp["x"][b], dtype=np.float32)
        m["cT"] = np.ascontiguousarray(inp["c"][b].reshape(8, 128).T, dtype=np.float32)
        in_maps.append(m)
    res = run_bass_kernel_spmd(nc, in_maps, core_ids=list(range(nb)))
    return np.stack([np.asarray(r["out"], dtype=np.float32) for r in res.results], axis=0)
```

```python
import math
from contextlib import ExitStack
import numpy as np
import concourse.bass as bass
import concourse.mybir as mybir
from concourse.bass_utils import run_bass_kernel_spmd

F32 = mybir.dt.float32
BF16 = mybir.dt.bfloat16
I32 = mybir.dt.int32
AF = mybir.ActivationFunctionType
ALU = mybir.AluOpType
AX = mybir.AxisListType

D = 1024
DEPTH = 4
NH_A = 8
NH_B = 4
DK_B = 128
DV_B = 256
RANK = 16
TAU = 16.0
CH = 64
NG = 4
EPG = 8
NE = 32
DEXP = 512
EPS = 1e-5
ALPHA = (2.0 * DEPTH) ** 0.25
D_IN = 8208
C_QA, C_KA, C_VA, C_QB, C_KB, C_VB, C_GB, C_AB, C_GT = 0, 1024, 2048, 3072, 3584, 4096, 5120, 6144, 6160
BLK = 256

ENGINES = ("pe", "act", "dve", "pool", "sp")
SEM_ROT = 30000


class Buf:
    __slots__ = ("name", "lw", "readers", "dcount")

    def __init__(self, name):
        self.name = name
        self.lw = None
        self.readers = []
        self.dcount = 0


class Ins:
    __slots__ = ("eng", "fn", "deps", "is_dma", "sig", "tok", "part", "dbuf", "barrier")

    def __init__(self, eng, fn, is_dma=False, part=False):
        self.eng = eng
        self.fn = fn
        self.deps = []
        self.is_dma = is_dma
        self.sig = False
        self.tok = None
        self.part = part
        self.dbuf = None
        self.barrier = None


class Prog:
    def __init__(self):
        self.ins = []
        self.bufs = []
        self.last_on = {e: None for e in ENGINES}

    def buf(self, name="b"):
        b = Buf(name)
        self.bufs.append(b)
        return b

    def _deps(self, I, reads, writes):
        deps = []
        for b in reads:
            if b.lw is not None:
                deps.append((b.lw, "raw"))
        for b in writes:
            if b.lw is not None:
                if not (I.is_dma and I.part and b.lw.is_dma and b.lw.part and not b.readers):
                    deps.append((b.lw, "waw"))
            for r in b.readers:
                deps.append((r, "war"))
        out = []
        seen = set()
        for d, kind in deps:
            if d is I or id(d) in seen:
                continue
            if (not d.is_dma) and (not I.is_dma) and d.eng == I.eng:
                if I.eng == "pe":
                    continue
                if kind == "war":
                    continue
            seen.add(id(d))
            out.append(d)
        for d in out:
            d.sig = True
        I.deps = out
        for b in reads:
            b.readers.append(I)
        for b in writes:
            b.lw = I
            b.readers = []

    def op(self, eng, fn, reads=(), writes=()):
        I = Ins(eng, fn)
        self._deps(I, list(reads), list(writes))
        self.ins.append(I)
        self.last_on[eng] = I
        return I

    def dma(self, eng, fn, reads=(), writes=(), part=False, sem=None):
        writes = list(writes)
        assert len(writes) == 1
        I = Ins(eng, fn, is_dma=True, part=part)
        I.dbuf = sem if sem is not None else writes[0]
        self._deps(I, list(reads), writes)
        self.ins.append(I)
        return I

    def barrier(self):
        lasts = [self.last_on[e] for e in ENGINES if self.last_on[e] is not None]
        for d in lasts:
            d.sig = True
        for e in ENGINES:
            I = Ins(e, None)
            I.barrier = (list(lasts), None)
            self.ins.append(I)
        for b in self.bufs:
            b.lw = None
            b.readers = []

    def emit(self, nc, stack):
        esems = {e: [] for e in ENGINES}
        ecount = {e: 0 for e in ENGINES}
        nsem = [0]
        recs = []
        free = []
        brec = {}
        last_use = {}
        for idx, I in enumerate(self.ins):
            if I.is_dma:
                last_use[id(I.dbuf)] = idx

        def new_sem(name):
            nsem[0] += 1
            return stack.enter_context(nc.semaphore(name))

        for idx, I in enumerate(self.ins):
            if I.barrier is not None:
                I.barrier = (I.barrier[0], [(r[0], r[1]) for r in recs if r[1] > 0])
                for bid in list(brec.keys()):
                    if last_use.get(bid, -1) < idx:
                        free.append(brec.pop(bid))
                continue
            if I.is_dma:
                b = I.dbuf
                if id(b) not in brec:
                    if free:
                        brec[id(b)] = free.pop()
                    else:
                        r = [new_sem(f"d{nsem[0]}"), 0]
                        recs.append(r)
                        brec[id(b)] = r
                r = brec[id(b)]
                r[1] += 16
                I.tok = (r[0], r[1])
            elif I.sig:
                e = I.eng
                if not esems[e] or ecount[e] >= SEM_ROT:
                    esems[e].append(new_sem(f"e_{e}{len(esems[e])}"))
                    ecount[e] = 0
                ecount[e] += 1
                I.tok = (esems[e][-1], ecount[e])
        self.n_sems = nsem[0]
        streams = {e: [I for I in self.ins if I.eng == e] for e in ENGINES}

        def run_stream(e, eng):
            waited = {}

            def wait(tok):
                sem, val = tok
                k = id(sem)
                if waited.get(k, 0) >= val:
                    return
                waited[k] = val
                eng.wait_ge(sem, val)

            for I in streams[e]:
                if I.barrier is not None:
                    lasts, dcounts = I.barrier
                    for d in lasts:
                        if d.tok is not None and not (d.eng == e and e == "pe"):
                            wait(d.tok)
                    for hs, cnt in dcounts:
                        wait((hs, cnt))
                    continue
                for d in I.deps:
                    wait(d.tok)
                r = I.fn(eng)
                if I.tok is not None:
                    r.then_inc(I.tok[0], 16 if I.is_dma else 1)

        with nc.Block() as block:
            @block.tensor
            def _(eng):
                run_stream("pe", eng)

            @block.scalar
            def _(eng):
                run_stream("act", eng)

            @block.vector
            def _(eng):
                run_stream("dve", eng)

            @block.gpsimd
            def _(eng):
                run_stream("pool", eng)

            @block.sync
            def _(eng):
                run_stream("sp", eng)


class Arena:
    def __init__(self, ap2d, n):
        self.ap = ap2d
        self.n = n
        self.off = 0
        self.mark = 0

    def reset(self):
        self.off = self.mark

    def keep(self):
        self.mark = self.off

    def push(self):
        self.stack = getattr(self, "stack", [])
        self.stack.append(self.mark)
        self.mark = self.off

    def pop(self):
        self.mark = self.stack.pop()

    def alloc(self, n, parts=128):
        assert self.off + n <= self.n, f"arena overflow {self.off}+{n}>{self.n}"
        a = self.ap[0:parts, self.off:self.off + n]
        self.off += n
        return a

    def alloc3(self, a, b, parts=128):
        return self.alloc(a * b, parts).rearrange("p (a b) -> p a b", b=b)


class K:
    pass


def slopes():
    return [2.0 ** (-8.0 * (i + 1) / NH_A) for i in range(NH_A)]


def host_consts(S):
    c = {}
    c["ident"] = np.eye(128, dtype=np.float32)
    ki = np.arange(128)[:, None]
    qi = np.arange(128)[None, :]
    c["tri"] = (qi >= ki).astype(np.float32)
    c["ustrict"] = (ki < qi).astype(np.float32)
    s64 = np.arange(64)[:, None]
    t64 = np.arange(64)[None, :]
    c["gmask"] = (s64 <= t64).astype(np.float32)
    qpos = np.arange(S) % 512
    c["qaug"] = np.stack([qpos // 16, qpos % 16, np.ones(S)]).astype(np.float32)
    kpos = np.arange(S) % 128
    sl = np.array(slopes())
    c["kaug"] = np.stack([np.stack([np.full(S, -128.0 * s), np.full(S, -8.0 * s), 8.0 * s * kpos])
                          for s in sl]).astype(np.float32)
    dl = np.arange(-3, 33)
    c["biascol"] = np.broadcast_to((-sl[:, None] * 128.0 * dl[None, :]).reshape(1, -1), (128, 8 * 36)).astype(np.float32).copy()
    return c


def build(S, L=DEPTH, dbg=(), phases="all"):
    nc = bass.Bass("TRN2", target_bir_lowering=False)
    NT = S // 128
    NQB = S // 512
    NCH = S // CH
    NSLOT = ((2 * S + NE * (BLK - 1)) // BLK + 1) * BLK
    NBLK = NSLOT // BLK
    P = Prog()
    st = ExitStack()

    def inp(name, shape, dt=F32):
        return nc.dram_tensor(name, list(shape), dt, kind="ExternalInput")

    def scratch(name, shape, dt):
        kind = "ExternalOutput" if name in dbg else "Internal"
        return nc.dram_tensor(name, list(shape), dt, kind=kind)

    x_in = inp("x", [S, D])
    cT = inp("cT", [128, 8])
    w_ada = inp("w_ada", [DEPTH, D, 6 * D])
    b_ada = inp("b_ada", [DEPTH, 6 * D])
    w_in = inp("w_in", [DEPTH, D, D_IN])
    b_gatesT = inp("b_gatesT", [DEPTH, 128, 16])
    w_alpha = inp("w_alpha", [DEPTH, RANK, 512])
    b_alphaT = inp("b_alphaT", [DEPTH, 128, 4])
    lam_in = inp("lam4", [DEPTH, 4, 64])
    diff_g = inp("diff_norm_g", [DEPTH, 128])
    gla_g = inp("gla_norm_g", [DEPTH, 256])
    w_ba = inp("w_branch_a", [DEPTH, D, D])
    w_bb = inp("w_branch_b", [DEPTH, D, D])
    w_out = inp("w_out", [DEPTH, D, D])
    ln1_g = inp("ln1_g", [DEPTH, D])
    ln1_b = inp("ln1_b", [DEPTH, D])
    w_r = inp("w_router", [DEPTH, D, 36])
    b_r = inp("b_router", [DEPTH, 36])
    w_ge = inp("w_gate_e", [DEPTH * NE * 128, 8 * DEXP])
    w_ue = inp("w_up_e", [DEPTH * NE * 128, 8 * DEXP])
    w_de = inp("w_down_e", [DEPTH * NE * 128, 4 * D])
    ln2_g = inp("ln2_g", [DEPTH, D])
    ln2_b = inp("ln2_b", [DEPTH, D])
    c_ident = inp("ident", [128, 128])
    c_tri = inp("tri", [128, 128])
    c_ustrict = inp("ustrict", [128, 128])
    c_gmask = inp("gmask", [64, 64])
    c_qaug = inp("qaug", [3, S])
    c_kaug = inp("kaug", [8, 3, S])
    c_biascol = inp("biascol", [128, 8 * 36])
    c_iota = inp("iota_p", [128, 1])
    out = nc.dram_tensor("out", [S, D], F32, kind="ExternalOutput")

    xA = scratch("xA", [S, D], F32)
    xB = scratch("xB", [S, D], F32)
    adaD = scratch("adaD", [DEPTH, 6 * D], F32)
    qaT = scratch("qaT", [1024, S], BF16)
    kaT = scratch("kaT", [1024, S], BF16)
    qbT = scratch("qbT", [512, S], BF16)
    kbT = scratch("kbT", [512, S], BF16)
    abT = scratch("abT", [16, S], BF16)
    gtT = scratch("gtT", [2048, S], BF16)
    va = scratch("va", [S, 1024], BF16)
    vb = scratch("vb", [S, 1024], BF16)
    gb = scratch("gb", [S, 1024], BF16)
    oaT = scratch("oaT", [1024, S], BF16)
    obT = scratch("obT", [1024, S], BF16)
    u2d = scratch("u2d", [S, D], BF16)
    XB = scratch("XB", [NSLOT, D], BF16)
    YB = scratch("YB", [NSLOT, D], F32)

    NF, NBF, NI = 22016, 57 * 1024, 1024
    arf = Arena(st.enter_context(nc.sbuf_tensor("arena_f", [128, NF], F32))[:, :], NF)
    arb = Arena(st.enter_context(nc.sbuf_tensor("arena_b", [128, NBF], BF16))[:, :], NBF)
    ari = Arena(st.enter_context(nc.sbuf_tensor("arena_i", [128, NI], I32))[:, :], NI)
    psf_t = st.enter_context(nc.psum_tensor("psf", [128, 6, 512], F32))
    psb_t = st.enter_context(nc.psum_tensor("psb", [128, 2, 1024], BF16))
    psf = [psf_t[:, i, :] for i in range(6)]
    psb = [psb_t[:, i, :] for i in range(2)]
    Bpsf = [P.buf(f"psf{i}") for i in range(6)]
    Bpsb = [P.buf(f"psb{i}") for i in range(2)]

    def B(name="b"):
        return P.buf(name)

    def mm(o, lhsT, rhs, start, stop, reads, writes):
        P.op("pe", lambda e: e.matmul(o, lhsT=lhsT, rhs=rhs, start=start, stop=stop), reads, writes)

    def tr(o, in_, ident, reads, writes):
        P.op("pe", lambda e: e.transpose(o, in_, ident), reads, writes)

    def act(o, in_, func, reads, writes, bias=None, scale=None, accum_out=None):
        kw = {}
        if bias is not None:
            kw["bias"] = bias
        if scale is not None:
            kw["scale"] = scale
        if accum_out is not None:
            kw["accum_out"] = accum_out
        P.op("act", lambda e: e.activation(out=o, in_=in_, func=func, **kw), reads, writes)

    def tt(eng, o, in0, in1, op, reads, writes):
        P.op(eng, lambda e: e.tensor_tensor(out=o, in0=in0, in1=in1, op=op), reads, writes)

    def ts(eng, o, in0, s1, s2, op0, op1, reads, writes, accum_out=None):
        kw = {}
        if accum_out is not None:
            kw["accum_out"] = accum_out
        if s2 is None:
            P.op(eng, lambda e: e.tensor_scalar(out=o, in0=in0, scalar1=s1, scalar2=None, op0=op0, **kw), reads, writes)
        else:
            P.op(eng, lambda e: e.tensor_scalar(out=o, in0=in0, scalar1=s1, scalar2=s2, op0=op0, op1=op1, **kw), reads, writes)

    def stt(eng, o, in0, scalar, in1, op0, op1, reads, writes):
        P.op(eng, lambda e: e.scalar_tensor_tensor(out=o, in0=in0, scalar=scalar, in1=in1, op0=op0, op1=op1), reads, writes)

    def cp(eng, o, in_, reads, writes):
        if eng == "act":
            P.op("act", lambda e: e.activation(out=o, in_=in_, func=AF.Copy), reads, writes)
        else:
            P.op(eng, lambda e: e.tensor_copy(out=o, in_=in_), reads, writes)

    def ld(q, o, in_, reads, writes, part=False, sem=None):
        P.dma(q, lambda e: e.dma_start(out=o, in_=in_), reads, writes, part=part, sem=sem)

    def rmax(o, in_, reads, writes):
        P.op("dve", lambda e: e.reduce_max(out=o, in_=in_, axis=AX.X), reads, writes)

    def rsum(o, in_, reads, writes):
        P.op("dve", lambda e: e.reduce_sum(out=o, in_=in_, axis=AX.X), reads, writes)

    def recip(o, in_, reads, writes):
        P.op("dve", lambda e: e.reciprocal(out=o, in_=in_), reads, writes)

    def memset(eng, o, val, writes):
        P.op(eng, lambda e: e.memset(o, val), (), writes)

    epsc = None

    def rsqrt_(o, in_, scale, Bt):
        act(o, in_, AF.Ln, [Bt, Bc2[7]], [Bt], bias=epsc[0:o.shape[0], :], scale=scale)
        act(o, o, AF.Exp, [Bt], [Bt], scale=-0.5)

    def ln_stats(xt, Bx, tmpf, Bt, tag=""):
        stats = tmpf[:, 0:12].rearrange("p (a b) -> p a b", b=6)
        mv = tmpf[:, 12:14]
        for hh in range(2):
            P.op("dve", (lambda h2: (lambda e: e.bn_stats(out=stats[:, h2, :], in_=xt[:, h2 * 512:(h2 + 1) * 512])))(hh), [Bx], [Bt])
        P.op("dve", lambda e: e.bn_aggr(out=mv, in_=stats), [Bt], [Bt])
        rstd = tmpf[:, 14:15]
        nmr = tmpf[:, 15:16]
        rsqrt_(rstd, mv[:, 1:2], 1.0, Bt)
        stt("dve", nmr, mv[:, 0:1], -1.0, rstd, ALU.mult, ALU.mult, [Bt], [Bt])
        return rstd, nmr

    ident_b = arb.alloc(128)
    ident_f = arf.alloc(128)
    tri_b = arb.alloc(128)
    ustr_b = arb.alloc(128)
    ones_b = arb.alloc(128)
    gmask_f = arf.alloc(64, parts=64)
    biascol = arf.alloc(8 * 36)
    iota_p = arf.alloc(1)
    Bconst = B("const")
    Bc2 = [B("c2_%d" % i) for i in range(8)]
    ld("pool", ident_b, c_ident[:, :], [], [Bc2[0]])
    ld("sp", ident_f, c_ident[:, :], [], [Bc2[1]])
    ld("pool", tri_b, c_tri[:, :], [], [Bc2[2]])
    ld("pool", ustr_b, c_ustrict[:, :], [], [Bc2[3]])
    ld("sp", gmask_f, c_gmask[:, :], [], [Bc2[4]])
    ld("sp", biascol, c_biascol[:, :], [], [Bc2[5]])
    ld("sp", iota_p, c_iota[:, :], [], [Bc2[6]])
    memset("dve", ones_b, 1.0, [Bc2[7]])
    epsc = arf.alloc(1)
    memset("dve", epsc, EPS, [Bc2[7]])
    arf.keep(); arb.keep(); ari.keep()
    P.barrier()
    CONST = Bc2

    def phase_reset():
        P.barrier()
        arf.reset(); arb.reset(); ari.reset()

    def phase_ada():
        ct = arf.alloc(8)
        condT = arf.alloc(8)
        row = arf.alloc(6 * D, parts=1)
        brow = arf.alloc(6 * D, parts=1)
        wt = [arf.alloc3(8, 512) for _ in range(2)]
        Bct, Brow, Bbrow = B(), B(), B()
        Bwt = [B(), B()]
        ld("sp", ct, cT[:, :], [], [Bct])
        act(condT, ct, AF.Silu, [Bct], [Bct])
        for l in range(L):
            ld("sp", brow, b_ada[l:l + 1, :], [], [Bbrow])
            for nb in range(12):
                w = wt[nb % 2]
                Bw = Bwt[nb % 2]
                ld("sp", w, w_ada[l, :, nb * 512:(nb + 1) * 512].rearrange("(kc p) n -> p kc n", p=128), [], [Bw])
                for kc in range(8):
                    mm(psf[nb % 2][0:1, :], condT[:, kc:kc + 1], w[:, kc, :], kc == 0, kc == 7, [Bct, Bw], [Bpsf[nb % 2]])
                tt("dve", row[:, nb * 512:(nb + 1) * 512], psf[nb % 2][0:1, :], brow[:, nb * 512:(nb + 1) * 512], ALU.add,
                   [Bpsf[nb % 2], Bbrow], [Brow])
            for c0 in (1024, 4096):
                ts("dve", row[:, c0:c0 + 1024], row[:, c0:c0 + 1024], 1.0, None, ALU.add, None, [Brow], [Brow])
            ld("sp", adaD[l:l + 1, :], row, [Brow], [Bada], part=True, sem=Brow)

    Bada = B("adaD")

    def phase_proj(l, xsrc):
        uT = arb.alloc3(8, S)
        BuTs = [B("uT") for _ in range(NQB)]
        sc = arf.alloc(D)
        sh = arf.alloc(D)
        Bmod = B("mod")
        ld("sp", sc, adaD[l:l + 1, 1024:2048].partition_broadcast(128), [Bada], [Bmod], part=True)
        ld("sp", sh, adaD[l:l + 1, 0:1024].partition_broadcast(128), [Bada], [Bmod], part=True)
        xt = [arf.alloc(D) for _ in range(2)]
        Bxt = [B(), B()]
        xn = [arf.alloc(D) for _ in range(2)]
        Bxn = [B(), B()]
        ub = [arb.alloc(D) for _ in range(2)]
        Bub = [B(), B()]
        tmp = [arf.alloc(16) for _ in range(2)]
        Btmp = [B(), B()]
        Bx = B("xsrc")
        for t in range(NT):
            i = t % 2
            ld("sp", xt[i], xsrc[t * 128:(t + 1) * 128, :], [Bx], [Bxt[i]])
            rstd, nmr = ln_stats(xt[i], Bxt[i], tmp[i], Btmp[i])
            act(xn[i], xt[i], AF.Identity, [Bxt[i], Btmp[i]], [Bxn[i]], bias=nmr, scale=rstd)
            tt("pool", xn[i], xn[i], sc, ALU.mult, [Bxn[i], Bmod], [Bxn[i]])
            tt("dve", ub[i], xn[i], sh, ALU.add, [Bxn[i], Bmod], [Bub[i]])
            pb = psb[t % 2]
            for kc in range(8):
                tr(pb[:, kc * 128:(kc + 1) * 128], ub[i][:, kc * 128:(kc + 1) * 128], ident_b, [Bub[i]], [Bpsb[t % 2]])
            cp("act", uT[:, :, t * 128:(t + 1) * 128], pb.rearrange("p (a b) -> p a b", b=128), [Bpsb[t % 2]], [BuTs[t // 4]])

        wblk = [arb.alloc3(8, 512) for _ in range(2)]
        Bw = [B(), B()]
        stg = [arb.alloc(S) for _ in range(2)]
        Bstg = [B(), B()]
        stt_ = [arb.alloc3(4, 512) for _ in range(2)]
        Bstt = [B(), B()]
        bgT = arf.alloc(16)
        Bbg = B()
        ld("sp", bgT, b_gatesT[l, :, :], [], [Bbg])
        wab = arb.alloc3(8, 16)
        Bwab = B()
        blocks = []
        for j in range(2):
            blocks.append((C_QA + 512 * j, "fm", qaT, 512 * j, None))
        for j in range(2):
            blocks.append((C_KA + 512 * j, "fm", kaT, 512 * j, None))
        blocks.append((C_QB, "fm", qbT, 0, None))
        blocks.append((C_KB, "fm", kbT, 0, None))
        for j in range(4):
            blocks.append((C_GT + 512 * j, "fm", gtT, 512 * j, "sig"))
        for j in range(2):
            blocks.append((C_VA + 512 * j, "tm", va, 512 * j, None))
        for j in range(2):
            blocks.append((C_VB + 512 * j, "tm", vb, 512 * j, None))
        for j in range(2):
            blocks.append((C_GB + 512 * j, "tm", gb, 512 * j, "silu"))
        Bdst = {id(t_): B() for t_ in (qaT, kaT, qbT, kbT, gtT, va, vb, gb, abT)}
        pi = 0
        si = 0
        for bi, (c0, kind, dst, d0, fn) in enumerate(blocks):
            w = wblk[bi % 2]
            ld("pool", w, w_in[l, :, c0:c0 + 512].rearrange("(kc p) n -> p kc n", p=128), [], [Bw[bi % 2]])
            if kind == "fm":
                for nch in range(4):
                    sg = stg[si % 2]
                    Bs = Bstg[si % 2]
                    si += 1
                    for tb in range(NQB):
                        pp = pi % 4
                        pi += 1
                        for kc in range(8):
                            mm(psf[pp], w[:, kc, nch * 128:(nch + 1) * 128], uT[:, kc, tb * 512:(tb + 1) * 512],
                               kc == 0, kc == 7, [Bw[bi % 2], BuTs[tb]], [Bpsf[pp]])
                        if fn == "sig":
                            gi = (d0 + nch * 128) // 128
                            act(sg[:, tb * 512:(tb + 1) * 512], psf[pp], AF.Sigmoid, [Bpsf[pp], Bbg], [Bs], bias=bgT[:, gi:gi + 1])
                        elif tb % 2 == 0:
                            cp("dve", sg[:, tb * 512:(tb + 1) * 512], psf[pp], [Bpsf[pp]], [Bs])
                        else:
                            cp("act", sg[:, tb * 512:(tb + 1) * 512], psf[pp], [Bpsf[pp]], [Bs])
                    r0 = d0 + nch * 128
                    ld("sp", dst[r0:r0 + 128, :], sg, [Bs], [Bdst[id(dst)]], part=True, sem=Bs)
            else:
                for t4 in range(NT // 4):
                    sg = stt_[si % 2]
                    Bs = Bstt[si % 2]
                    si += 1
                    for tq in range(4):
                        t = t4 * 4 + tq
                        pp = pi % 4
                        pi += 1
                        for kc in range(8):
                            mm(psf[pp], uT[:, kc, t * 128:(t + 1) * 128], w[:, kc, :], kc == 0, kc == 7,
                               [Bw[bi % 2], BuTs[t // 4]], [Bpsf[pp]])
                        if fn == "silu":
                            act(sg[:, tq, :], psf[pp], AF.Silu, [Bpsf[pp]], [Bs])
                        elif tq % 2 == 0:
                            cp("dve", sg[:, tq, :], psf[pp], [Bpsf[pp]], [Bs])
                        else:
                            cp("act", sg[:, tq, :], psf[pp], [Bpsf[pp]], [Bs])
                    ld("sp", dst[t4 * 512:(t4 + 1) * 512, d0:d0 + 512].rearrange("(a p) n -> p a n", p=128), sg, [Bs],
                       [Bdst[id(dst)]], part=True, sem=Bs)
        ld("pool", wab, w_in[l, :, C_AB:C_AB + 16].rearrange("(kc p) n -> p kc n", p=128), [], [Bwab])
        sg = stg[si % 2]
        Bs = Bstg[si % 2]
        for tb in range(NQB):
            pp = pi % 4
            pi += 1
            for kc in range(8):
                mm(psf[pp][0:16, :], wab[:, kc, :], uT[:, kc, tb * 512:(tb + 1) * 512], kc == 0, kc == 7, [Bwab, BuTs[tb]], [Bpsf[pp]])
            cp("dve", sg[0:16, tb * 512:(tb + 1) * 512], psf[pp][0:16, :], [Bpsf[pp]], [Bs])
        ld("sp", abT[:, :], sg[0:16, :], [Bs], [Bdst[id(abT)]], sem=Bs)

    BoaT = B("oaT")

    def phase_attn(l):
        lam_init = 0.8 - 0.6 * math.exp(-0.3 * l)
        lv = arf.alloc(256)
        Blam = B("lam")
        ld("sp", lv, lam_in[l:l + 1, :, :].rearrange("a b c -> a (b c)").partition_broadcast(128), [], [Blam])
        lv4 = lv.rearrange("p (a b c) -> p a b c", a=2, b=2)
        pr = arf.alloc3(2, 64)
        s12 = arf.alloc(2)
        e12 = arf.alloc(2)
        neglam = arf.alloc(1)
        tt("dve", pr, lv4[:, :, 0, :], lv4[:, :, 1, :], ALU.mult, [Blam], [Blam])
        P.op("dve", lambda e: e.reduce_sum(out=s12, in_=pr, axis=AX.X), [Blam], [Blam])
        act(e12, s12, AF.Exp, [Blam], [Blam])
        tt("dve", neglam, e12[:, 0:1], e12[:, 1:2], ALU.subtract, [Blam], [Blam])
        ts("dve", neglam, neglam, lam_init, -1.0, ALU.add, ALU.mult, [Blam], [Blam])
        gA = arf.alloc(128)
        BgA = B("gA")
        ld("sp", gA, diff_g[l:l + 1, :].partition_broadcast(128), [], [BgA])
        ts("dve", gA, gA, 1.0 - lam_init, None, ALU.mult, None, [BgA], [BgA])

        QT = [[arb.alloc(S) for c in range(2)] for p in range(2)]
        KT = [[arb.alloc(S) for c in range(2)] for p in range(2)]
        VA = [arb.alloc3(NT, 129) for p in range(2)]
        BQ = [[B("Q") for c in range(2)] for p in range(2)]
        BK = [[B("K") for c in range(2)] for p in range(2)]
        BV = [B("V") for p in range(2)]
        for p in range(2):
            memset("pool", VA[p][:, :, 128:129], 1.0, [BV[p]])
        PT = [arb.alloc(512) for _ in range(3)]
        BPT = [B("PT") for _ in range(3)]
        o0 = [arf.alloc(128) for _ in range(4)]
        Bo0 = [B("o0") for _ in range(4)]
        sm = [arf.alloc(8) for _ in range(8)]
        Bsm = [B("sm") for _ in range(8)]
        of = [arf.alloc(128) for _ in range(2)]
        Bof = [B("of") for _ in range(2)]
        junk = arf.alloc(128)
        Bjunk = B("junk")
        obf = [arb.alloc(128) for _ in range(2)]
        Bobf = [B("obf") for _ in range(2)]
        ostg = [arb.alloc(512) for _ in range(2)]
        Bostg = [B("ostg") for _ in range(2)]
        Bsrc = B("attn_src")

        def load_head(h):
            p = h % 2
            for c in range(2):
                r0 = h * 128 + c * 64
                ld("sp", QT[p][c][0:64, :], qaT[r0:r0 + 64, :], [Bsrc], [BQ[p][c]], part=True)
                ld("pool", QT[p][c][64:67, :], c_qaug[:, :], [], [BQ[p][c]], part=True)
                ld("sp", KT[p][c][0:64, :], kaT[r0:r0 + 64, :], [Bsrc], [BK[p][c]], part=True)
                ld("pool", KT[p][c][64:67, :], c_kaug[h, :, :], [], [BK[p][c]], part=True)
            ld("sp", VA[p][:, :, 0:128], va[:, h * 128:(h + 1) * 128].rearrange("(t p) d -> p t d", p=128), [Bsrc], [BV[p]])

        load_head(0)
        sl_ = slopes()
        jobs = []
        for h in range(NH_A):
            dmax = int((60.0 / sl_[h] + 127.0) // 128.0)
            for qb in range(NQB):
                for c in range(2):
                    k0 = max(0, 4 * qb - dmax)
                    for kt in range(k0, 4 * (qb + 1)):
                        jobs.append((h, qb, c, kt, k0))
        state = {"pti": 0, "si": 0}
        Sbank = [psf[0], psf[1], psb[1].bitcast(F32)]
        BSbank = [Bpsf[0], Bpsf[1], Bpsb[1]]
        PT4 = PT + [arb.alloc(512)]
        BPT4 = BPT + [B("PT")]
        of4 = [arf.alloc(128) for _ in range(4)]
        Bof4 = [B("of4") for _ in range(4)]
        ssq = [arf.alloc(8) for _ in range(2)]
        Bssq = [B("ssq") for _ in range(2)]
        LA = 2

        def issue_S(job):
            h, qb, c, kt, k0 = job
            p = h % 2
            j = kt - 4 * qb
            col0 = 128 * max(j, 0)
            sb_ = state["si"] % 3
            state["si"] += 1
            mm(Sbank[sb_][:, col0:512], KT[p][c][0:67, kt * 128:(kt + 1) * 128],
               QT[p][c][0:67, qb * 512 + col0:(qb + 1) * 512], True, True, [BK[p][c], BQ[p][c]], [BSbank[sb_]])
            pt = PT4[state["pti"] % 4]
            Bp = BPT4[state["pti"] % 4]
            state["pti"] += 1
            bi_ = h * 36 + (4 * qb - kt + 3)
            act(pt[:, col0:512], Sbank[sb_][:, col0:512], AF.Exp, [BSbank[sb_]], [Bp],
                bias=biascol[:, bi_:bi_ + 1], scale=0.125)
            if j >= 0:
                tt("pool", pt[:, col0:col0 + 128], pt[:, col0:col0 + 128], tri_b, ALU.mult, [Bp], [Bp])
            return pt, Bp

        loaded = {0}
        pend = [issue_S(jobs[i_]) for i_ in range(min(LA, len(jobs)))]
        gi = 0
        for ji, job in enumerate(jobs):
            h, qb, c, kt, k0 = job
            p = h % 2
            if h + 1 < NH_A and (h + 1) not in loaded:
                load_head(h + 1)
                loaded.add(h + 1)
            pt, Bp = pend.pop(0)
            if ji + LA < len(jobs):
                pend.append(issue_S(jobs[ji + LA]))
            j = kt - 4 * qb
            Ob = [2 + 2 * c, 3 + 2 * c]
            O = [psf[Ob[qt // 2]][:, (qt % 2) * 256:(qt % 2) * 256 + 129] for qt in range(4)]
            for qt in range(max(j, 0), 4):
                mm(O[qt], pt[:, qt * 128:(qt + 1) * 128], VA[p][:, kt, :], kt == k0 and qt % 2 == 0, kt == 4 * qb + qt,
                   [Bp, BV[p]], [Bpsf[Ob[qt // 2]]])
            if kt != 4 * qb + 3:
                continue
            g_ = gi % 2
            sq = ssq[g_]
            Bq_ = Bssq[g_]
            if c == 0:
                for qt in range(4):
                    Bo = Bpsf[Ob[qt // 2]]
                    recip(sq[:, 4 + qt:5 + qt], O[qt][:, 128:129], [Bo], [Bq_])
                    ts("dve", o0[qt], O[qt][:, 0:128], sq[:, 4 + qt:5 + qt], None, ALU.mult, None, [Bo, Bq_], [Bo0[qt]])
                continue
            gi += 1
            for qt in range(4):
                Bo = Bpsf[Ob[qt // 2]]
                recip(sq[:, 4 + qt:5 + qt], O[qt][:, 128:129], [Bo], [Bq_])
                tt("dve", sq[:, 4 + qt:5 + qt], sq[:, 4 + qt:5 + qt], neglam, ALU.mult, [Bq_, Blam], [Bq_])
                stt("dve", of4[qt], O[qt][:, 0:128], sq[:, 4 + qt:5 + qt], o0[qt], ALU.mult, ALU.add, [Bo, Bq_, Bo0[qt]], [Bof4[qt]])
                tt("pool", junk, of4[qt], of4[qt], ALU.mult, [Bof4[qt]], [Bjunk])
                rsum(sq[:, qt:qt + 1], junk, [Bjunk], [Bq_])
            act(sq[:, 0:4], sq[:, 0:4], AF.Ln, [Bq_, Bc2[7]], [Bq_], bias=epsc, scale=1.0 / 128.0)
            act(sq[:, 0:4], sq[:, 0:4], AF.Exp, [Bq_], [Bq_], scale=-0.5)
            for qt in range(4):
                e_ = qt % 2
                stt("dve", obf[e_], of4[qt], sq[:, qt:qt + 1], gA, ALU.mult, ALU.mult, [Bof4[qt], Bq_, BgA], [Bobf[e_]])
                tr(psb[0][:, qt * 128:(qt + 1) * 128], obf[e_], ident_b, [Bobf[e_]], [Bpsb[0]])
            tb_ = gi % 2
            sg = ostg[tb_]
            cp("dve", sg, psb[0][:, 0:512], [Bpsb[0]], [Bostg[tb_]])
            ld("sp", oaT[h * 128:(h + 1) * 128, qb * 512:(qb + 1) * 512], sg, [Bostg[tb_]], [BoaT],
               part=True, sem=Bostg[tb_])

    BobT = B("obT")

    def phase_gla(l):
        HP = NH_B
        G = 2
        NGRP = NCH // G
        Bsrc = B("gla_src")
        rmask = arf.alloc(S)
        Brm = B("rmask")
        memset("pool", rmask, 1.0, [Brm])
        memset("pool", rmask.rearrange("p (c t) -> p c t", t=CH)[:, :, 0:1], 0.0, [Brm])
        onec = arf.alloc(1)
        memset("dve", onec, 1.0, [Brm])
        gG = arf.alloc(256)
        ld("sp", gG, gla_g[l:l + 1, :].partition_broadcast(128), [], [Brm], part=True)
        bal = arf.alloc(4)
        Bbal = B("bal")
        ld("sp", bal, b_alphaT[l, :, :], [], [Bbal])
        ts("dve", bal, bal, -1.0, None, ALU.mult, None, [Bbal], [Bbal])
        abt = arb.alloc(S)
        Babt = B("abt")
        ld("sp", abt[0:16, :], abT[:, :], [Bsrc], [Babt])
        wal = arb.alloc(512)
        Bwal = B("wal")
        ld("pool", wal[0:16, :], w_alpha[l, :, :], [], [Bwal])
        cum = arf.alloc(S)
        Ee = arf.alloc(S)
        Ei = arf.alloc(S)
        Bcum, BE, BEi = B("cum"), B("E"), B("Ei")
        qtl = [arb.alloc(S) for _ in range(HP)]
        ktl = [arb.alloc(S) for _ in range(HP)]
        lastE = [arf.alloc(NCH) for _ in range(HP)]
        Sst = [arf.alloc(256) for _ in range(HP)]
        Sbf = [arb.alloc(256) for _ in range(HP)]
        Bq = [B("qt") for _ in range(HP)]
        Bk = [B("kt") for _ in range(HP)]
        BlE = [B("lastE") for _ in range(HP)]
        BS = [B("S") for _ in range(HP)]
        BSb = [B("Sb") for _ in range(HP)]
        for i in range(HP):
            h = i
            ld("sp", qtl[i], qbT[h * 128:(h + 1) * 128, :], [Bsrc], [Bq[i]])
            ld("sp", ktl[i], kbT[h * 128:(h + 1) * 128, :], [Bsrc], [Bk[i]])
        for i in range(HP):
            h = i
            for tb in range(NQB):
                pp = tb % 2
                mm(psf[pp], wal[0:16, h * 128:(h + 1) * 128], abt[0:16, tb * 512:(tb + 1) * 512], True, True,
                   [Bwal, Babt], [Bpsf[pp]])
                act(cum[:, tb * 512:(tb + 1) * 512], psf[pp], AF.Exp, [Bpsf[pp], Bbal], [Bcum], bias=bal[:, h:h + 1], scale=-1.0)
            act(cum, cum, AF.Ln, [Bcum, Brm], [Bcum], bias=onec, scale=1.0)
            P.op("dve", lambda e: e.tensor_tensor_scan(out=cum, data0=rmask, data1=cum, initial=0.0, op0=ALU.mult, op1=ALU.add),
                 [Bcum, Brm], [Bcum])
            act(Ee, cum, AF.Exp, [Bcum], [BE], scale=-1.0 / TAU)
            act(Ei, cum, AF.Exp, [Bcum], [BEi], scale=1.0 / TAU)
            stt("dve", qtl[i], qtl[i], DK_B ** -0.5, Ee, ALU.mult, ALU.mult, [Bq[i], BE], [Bq[i]])
            tt("pool", ktl[i], ktl[i], Ei, ALU.mult, [Bk[i], BEi], [Bk[i]])
            cp("dve", lastE[i], Ee.rearrange("p (c t) -> p c t", t=CH)[:, :, CH - 1], [BE], [BlE[i]])
        vt = [[arb.alloc3(G, 256) for _ in range(2)] for i in range(HP)]
        gt = [[arb.alloc3(G, 256) for _ in range(2)] for i in range(HP)]
        Bvt = [[B("vt") for _ in range(2)] for i in range(HP)]
        Bgt = [[B("gt") for _ in range(2)] for i in range(HP)]
        attm = [arb.alloc(64) for i in range(HP)]
        Battm = [B("attm") for i in range(HP)]
        khc = [arb.alloc(128) for i in range(HP)]
        Bkhc = [B("khc") for i in range(HP)]
        onf = [arf.alloc(256) for i in range(HP)]
        Bonf = [B("onf") for i in range(HP)]
        obc = [arb.alloc(256) for i in range(HP)]
        Bobc = [B("obc") for i in range(HP)]
        smg = [[arf.alloc(4) for _ in range(2)] for i in range(HP)]
        Bsmg = [[B("smg") for _ in range(2)] for i in range(HP)]
        junk = [arf.alloc(256) for _ in range(2)]
        Bjunk = [B("junk") for _ in range(2)]
        obst = [[arb.alloc3(2, G * CH) for _ in range(2)] for i in range(HP)]
        Bobst = [[B("obst") for _ in range(2)] for i in range(HP)]

        def load_group(g):
            for i in range(HP):
                h = i
                t0 = g * G * CH
                ld("sp", vt[i][g % 2][0:64], vb[t0:t0 + G * CH, h * 256:(h + 1) * 256].rearrange("(c p) f -> p c f", p=CH),
                   [Bsrc], [Bvt[i][g % 2]])
                ld("sp", gt[i][g % 2][0:64], gb[t0:t0 + G * CH, h * 256:(h + 1) * 256].rearrange("(c p) f -> p c f", p=CH),
                   [Bsrc], [Bgt[i][g % 2]])

        load_group(0)
        for g in range(NGRP):
            if g + 1 < NGRP:
                load_group(g + 1)
            for ci in range(G):
                c = g * G + ci
                for i in range(HP):
                    h = i
                    cs = slice(c * CH, (c + 1) * CH)
                    pa = c % 2
                    po = 2 + i // 2
                    ps_ = 4 + i // 2
                    oc = slice((i % 2) * 256, (i % 2) * 256 + 256)
                    v_c = vt[i][g % 2][0:64, ci, :]
                    Bv = Bvt[i][g % 2]
                    mm(psf[pa][0:64, i * 64:(i + 1) * 64], ktl[i][:, cs], qtl[i][:, cs], True, True, [Bk[i], Bq[i]], [Bpsf[pa]])
                    tt("dve", attm[i][0:64, :], psf[pa][0:64, i * 64:(i + 1) * 64], gmask_f, ALU.mult, [Bpsf[pa]], [Battm[i]])
                    tr(psb[0][0:64, i * 128:(i + 1) * 128], ktl[i][:, cs], ident_b, [Bk[i]], [Bpsb[0]])
                    cp("act", khc[i][0:64, :], psb[0][0:64, i * 128:(i + 1) * 128], [Bpsb[0]], [Bkhc[i]])
                    if c > 0:
                        mm(psf[po][0:64, oc], qtl[i][:, cs], Sbf[i], True, False, [Bq[i], BSb[i]], [Bpsf[po]])
                    mm(psf[po][0:64, oc], attm[i][0:64, :], v_c, c == 0, True, [Battm[i], Bv], [Bpsf[po]])
                    if c + 1 < NCH:
                        mm(psf[ps_][:, oc], khc[i][0:64, :], v_c, True, True, [Bkhc[i], Bv], [Bpsf[ps_]])
                        if c == 0:
                            ts("dve", Sst[i], psf[ps_][:, oc], lastE[i][:, c:c + 1], None, ALU.mult, None, [Bpsf[ps_], BlE[i]], [BS[i]])
                        else:
                            tt("dve", Sst[i], Sst[i], psf[ps_][:, oc], ALU.add, [BS[i], Bpsf[ps_]], [BS[i]])
                            ts("dve", Sst[i], Sst[i], lastE[i][:, c:c + 1], None, ALU.mult, None, [BS[i], BlE[i]], [BS[i]])
                        cp("pool", Sbf[i], Sst[i], [BS[i]], [BSb[i]])
                    sm_ = smg[i][c % 2]
                    Bs_ = Bsmg[i][c % 2]
                    jk = junk[i % 2]
                    act(jk[0:64, :], psf[po][0:64, oc], AF.Square, [Bpsf[po]], [Bjunk[i % 2], Bs_], accum_out=sm_[0:64, 0:1])
                    act(sm_[0:64, 1:2], sm_[0:64, 0:1], AF.Ln, [Bs_, Bc2[7]], [Bs_], bias=epsc[0:64, :], scale=1.0 / 256.0)
                    act(sm_[0:64, 1:2], sm_[0:64, 1:2], AF.Exp, [Bs_], [Bs_], scale=-0.5)
                    stt("dve", onf[i][0:64, :], psf[po][0:64, oc], sm_[0:64, 1:2], gG[0:64, :], ALU.mult, ALU.mult,
                        [Bpsf[po], Bs_, Brm], [Bonf[i]])
                    tt("pool", obc[i][0:64, :], onf[i][0:64, :], gt[i][g % 2][0:64, ci, :], ALU.mult, [Bonf[i], Bgt[i][g % 2]], [Bobc[i]])
                    for j in range(2):
                        col = i * 256 + j * (G * CH) + ci * CH
                        tr(psb[1][:, col:col + CH], obc[i][0:64, j * 128:(j + 1) * 128], ident_b[0:64, 0:64], [Bobc[i]], [Bpsb[1]])
                    if ci == G - 1:
                        sg = obst[i][g % 2]
                        Bs2 = Bobst[i][g % 2]
                        cp("act", sg, psb[1][:, i * 256:(i + 1) * 256].rearrange("p (j t) -> p j t", t=G * CH), [Bpsb[1]], [Bs2])
                        t0 = g * G * CH
                        for j in range(2):
                            r0 = h * 256 + j * 128
                            ld("sp", obT[r0:r0 + 128, t0:t0 + G * CH], sg[:, j, :], [Bs2], [BobT], part=True, sem=Bs2)

    RT = {}

    def phase_merge(l, xcur, xmid):
        TB = 256
        NTB = S // TB
        Bsrc = B("mg_src")
        A1s = arf.alloc3(NT, 32); A2s = arf.alloc3(NT, 32); RK = arf.alloc3(NT, 32)
        W12 = arf.alloc3(NT, 2)
        Asum = arf.alloc(32)
        Asb = arb.alloc(32)
        BA1, BA2, BRK, BW12, BAs, BAsb = B("A1s"), B("A2s"), B("RK"), B("W12"), B("Asum"), B("Asb")
        RT.update(A1s=A1s, A2s=A2s, RK=RK, W12=W12, Asum=Asum, Asb=Asb, BA1=BA1, BA2=BA2, BRK=BRK, BW12=BW12, BAs=BAs, BAsb=BAsb)
        arf.push(); arb.push()
        wts = []
        Bwts = []
        for wi, wsrc in enumerate((w_ba, w_bb, w_out)):
            w_ = arb.alloc3(8, 1024)
            Bw_ = B("mgw")
            for kc in range(8):
                ld("pool", w_[:, kc, :], wsrc[l, kc * 128:(kc + 1) * 128, :], [], [Bw_], part=True)
            wts.append(w_)
            Bwts.append(Bw_)
        wba_, wbb_, wout_ = wts
        wr = arb.alloc3(8, 36)
        Bwr = B("wr")
        ld("pool", wr, w_r[l, :, :].rearrange("(kc p) n -> p kc n", p=128), [], [Bwr])
        Bbc = B("bcast")
        bc = {}
        for nm, src in (("g1", adaD[l:l + 1, 2048:3072]), ("sh2", adaD[l:l + 1, 3072:4096]), ("sc2", adaD[l:l + 1, 4096:5120]),
                        ("l1g", ln1_g[l:l + 1, :]), ("l1b", ln1_b[l:l + 1, :])):
            bc[nm] = arf.alloc(D)
            ld("sp", bc[nm], src.partition_broadcast(128), [Bada], [Bbc], part=True)
        brb = arf.alloc(36)
        ld("sp", brb, b_r[l:l + 1, :].partition_broadcast(128), [], [Bbc], part=True)
        oab = [arb.alloc3(8, TB) for _ in range(2)]
        obb = [arb.alloc3(8, TB) for _ in range(2)]
        gtb = [arb.alloc3(16, TB) for _ in range(2)]
        Bin = [B("mg_in") for _ in range(2)]
        mxT = [arb.alloc3(8, TB) for _ in range(2)]
        BmxT = [B("mxT") for _ in range(2)]
        m1 = [arf.alloc(TB) for _ in range(2)]
        Bm1 = [B("m1") for _ in range(2)]
        xt = [arf.alloc(D) for _ in range(2)]
        Bxt = [B("xt") for _ in range(2)]
        rr = [arf.alloc(D) for _ in range(2)]
        Brr = [B("rr") for _ in range(2)]
        tmp = [arf.alloc(16) for _ in range(4)]
        Btmp = [B("tmp") for _ in range(4)]
        u2b = [arb.alloc(D) for _ in range(2)]
        Bu2b = [B("u2b") for _ in range(2)]
        u2T = [arb.alloc3(8, 128) for _ in range(2)]
        Bu2T = [B("u2T") for _ in range(2)]
        rs = [arf.alloc(128) for _ in range(2)]
        Brs = [B("rs") for _ in range(2)]
        a12b = [arb.alloc(32) for _ in range(2)]
        Ba12 = [B("a12b") for _ in range(2)]
        Bxm = B("xmid")
        Bu2d = B("u2d")

        def load_blk(tb):
            i = tb % 2
            ld("sp", oab[i], oaT[:, tb * TB:(tb + 1) * TB].rearrange("(kc p) t -> p kc t", p=128), [Bsrc], [Bin[i]], part=True)
            ld("sp", obb[i], obT[:, tb * TB:(tb + 1) * TB].rearrange("(kc p) t -> p kc t", p=128), [Bsrc], [Bin[i]], part=True)
            ld("sp", gtb[i], gtT[:, tb * TB:(tb + 1) * TB].rearrange("(kc p) t -> p kc t", p=128), [Bsrc], [Bin[i]], part=True)

        load_blk(0)
        for tb in range(NTB):
            i = tb % 2
            if tb + 1 < NTB:
                load_blk(tb + 1)
            for nch in range(8):
                pA, pB = (2 * nch) % 4, (2 * nch + 1) % 4
                for kc in range(8):
                    mm(psf[pA][:, 0:TB], wba_[:, kc, nch * 128:(nch + 1) * 128], oab[i][:, kc, :], kc == 0, kc == 7, [Bwts[0], Bin[i]], [Bpsf[pA]])
                for kc in range(8):
                    mm(psf[pB][:, 0:TB], wbb_[:, kc, nch * 128:(nch + 1) * 128], obb[i][:, kc, :], kc == 0, kc == 7, [Bwts[1], Bin[i]], [Bpsf[pB]])
                mi = nch % 2
                tt("dve", m1[mi], psf[pA][:, 0:TB], gtb[i][:, nch, :], ALU.mult, [Bpsf[pA], Bin[i]], [Bm1[mi]])
                tt("dve", mxT[i][:, nch, :], psf[pB][:, 0:TB], gtb[i][:, 8 + nch, :], ALU.mult, [Bpsf[pB], Bin[i]], [BmxT[i]])
                tt("pool", mxT[i][:, nch, :], mxT[i][:, nch, :], m1[mi], ALU.add, [BmxT[i], Bm1[mi]], [BmxT[i]])
            for tq in range(TB // 128):
                t = tb * (TB // 128) + tq
                j = t % 2
                ld("sp", xt[j], xcur[t * 128:(t + 1) * 128, :], [Bsrc], [Bxt[j]])
                for half in range(2):
                    py = 4 + half
                    for kc in range(8):
                        mm(psf[py], mxT[i][:, kc, tq * 128:(tq + 1) * 128], wout_[:, kc, half * 512:(half + 1) * 512], kc == 0, kc == 7,
                           [BmxT[i], Bwts[2]], [Bpsf[py]])
                    tt("dve", rr[j][:, half * 512:(half + 1) * 512], psf[py], bc["g1"][:, half * 512:(half + 1) * 512], ALU.mult,
                       [Bpsf[py], Bbc], [Brr[j]])
                stt("dve", rr[j], xt[j], ALPHA, rr[j], ALU.mult, ALU.add, [Bxt[j], Brr[j]], [Brr[j]])
                rstd, nmr = ln_stats(rr[j], Brr[j], tmp[2 * j], Btmp[2 * j])
                act(rr[j], rr[j], AF.Identity, [Brr[j], Btmp[2 * j]], [Brr[j]], bias=nmr, scale=rstd)
                tt("pool", rr[j], rr[j], bc["l1g"], ALU.mult, [Brr[j], Bbc], [Brr[j]])
                tt("dve", xt[j], rr[j], bc["l1b"], ALU.add, [Brr[j], Bbc], [Bxt[j]])
                ld("sp", xmid[t * 128:(t + 1) * 128, :], xt[j], [Bxt[j]], [Bxm], part=True, sem=Bxt[j])
                rstd2, nmr2 = ln_stats(xt[j], Bxt[j], tmp[2 * j + 1], Btmp[2 * j + 1])
                act(rr[j], xt[j], AF.Identity, [Bxt[j], Btmp[2 * j + 1]], [Brr[j]], bias=nmr2, scale=rstd2)
                tt("pool", rr[j], rr[j], bc["sc2"], ALU.mult, [Brr[j], Bbc], [Brr[j]])
                tt("dve", u2b[j], rr[j], bc["sh2"], ALU.add, [Brr[j], Bbc], [Bu2b[j]])
                ld("sp", u2d[t * 128:(t + 1) * 128, :], u2b[j], [Bu2b[j]], [Bu2d], part=True, sem=Bu2b[j])
                for kc in range(8):
                    tr(psb[0][:, kc * 128:(kc + 1) * 128], u2b[j][:, kc * 128:(kc + 1) * 128], ident_b, [Bu2b[j]], [Bpsb[0]])
                cp("act", u2T[j], psb[0].rearrange("p (a b) -> p a b", b=128), [Bpsb[0]], [Bu2T[j]])
                RBk = psb[1].bitcast(F32)
                BRBk = Bpsb[1]
                for kc in range(8):
                    mm(RBk[:, 0:36], u2T[j][:, kc, :], wr[:, kc, :], kc == 0, kc == 7, [Bu2T[j], Bwr], [BRBk])
                R_ = rs[j]
                BR = Brs[j]
                lg = R_[:, 0:36]
                tt("dve", lg, RBk[:, 0:36], brb, ALU.add, [BRBk, Bbc], [BR])
                gl = lg[:, 0:4]
                el = lg[:, 4:36].rearrange("p (g e) -> p g e", e=8)
                gmax, ngmax, gsum, gw = R_[:, 36:37], R_[:, 37:38], R_[:, 38:39], R_[:, 39:40]
                ohg = R_[:, 40:44]
                gex = R_[:, 44:48]
                ein = R_[:, 48:56]
                ein2 = R_[:, 56:64]
                mk1 = R_[:, 64:72]
                mk2 = R_[:, 72:80]
                mx1, mx2, dd, ex, den, w1, w2 = (R_[:, 80 + q:81 + q] for q in range(7))
                a12 = R_[:, 90:122]
                rmax(gmax, gl, [BR], [BR])
                ts("dve", ohg, gl, gmax, None, ALU.is_equal, None, [BR], [BR])
                ts("dve", ngmax, gmax, -1.0, None, ALU.mult, None, [BR], [BR])
                act(gex, gl, AF.Exp, [BR], [BR], bias=ngmax, scale=1.0, accum_out=gsum)
                recip(gw, gsum, [BR], [BR])
                ts("dve", ein, el[:, 0, :], ohg[:, 0:1], None, ALU.mult, None, [BR], [BR])
                for g_ in range(1, 4):
                    stt("dve", ein, el[:, g_, :], ohg[:, g_:g_ + 1], ein, ALU.mult, ALU.add, [BR], [BR])
                rmax(mx1, ein, [BR], [BR])
                ts("dve", mk1, ein, mx1, None, ALU.is_equal, None, [BR], [BR])
                stt("dve", ein2, mk1, -1.0e30, ein, ALU.mult, ALU.add, [BR], [BR])
                rmax(mx2, ein2, [BR], [BR])
                ts("dve", mk2, ein2, mx2, None, ALU.is_equal, None, [BR], [BR])
                tt("dve", dd, mx2, mx1, ALU.subtract, [BR], [BR])
                act(ex, dd, AF.Exp, [BR], [BR])
                ts("dve", den, ex, 1.0, None, ALU.add, None, [BR], [BR])
                recip(w1, den, [BR], [BR])
                tt("dve", w2, ex, w1, ALU.mult, [BR], [BR])
                tt("dve", W12[:, t, 0:1], w1, gw, ALU.mult, [BR], [BW12])
                tt("dve", W12[:, t, 1:2], w2, gw, ALU.mult, [BR], [BW12])
                a1v = A1s[:, t, :].rearrange("p (g e) -> p g e", e=8)
                a2v = A2s[:, t, :].rearrange("p (g e) -> p g e", e=8)
                tt("dve", a1v, ohg.unsqueeze(2).to_broadcast([128, 4, 8]), mk1.unsqueeze(1).to_broadcast([128, 4, 8]), ALU.mult, [BR], [BA1])
                tt("dve", a2v, ohg.unsqueeze(2).to_broadcast([128, 4, 8]), mk2.unsqueeze(1).to_broadcast([128, 4, 8]), ALU.mult, [BR], [BA2])
                tt("dve", a12, A1s[:, t, :], A2s[:, t, :], ALU.add, [BA1, BA2], [BR])
                cp("dve", a12b[j], a12, [BR], [Ba12[j]])
                mm(RBk[:, 64:96], ustr_b, a12b[j], True, t == 0, [Ba12[j]], [BRBk])
                if t > 0:
                    mm(RBk[:, 64:96], ones_b, Asb, False, True, [BAsb], [BRBk])
                cp("dve", RK[:, t, :], RBk[:, 64:96], [BRBk], [BRK])
                if t == 0:
                    cp("dve", Asum, a12, [BR], [BAs])
                else:
                    tt("dve", Asum, Asum, a12, ALU.add, [BAs, BR], [BAs])
                cp("dve", Asb, Asum, [BAs], [BAsb])

    c_blkstart = inp("blkstart", [128, NBLK])
    c_iotaG = inp("iotaG", [128, 8])

    def phase_moe(l, xmid, xnext):
        A1s, A2s, RK, W12, Asb = RT["A1s"], RT["A2s"], RT["RK"], RT["W12"], RT["Asb"]
        Bg = B("moe_glob")
        cnt = arf.alloc(32); pad = arf.alloc(32); pend = arf.alloc(32); pst = arf.alloc(32); one32 = arf.alloc(32)
        PR = arf.alloc3(NT, 32)
        D1f = arf.alloc(NT); D2f = arf.alloc(NT)
        D1i = ari.alloc(NT); D2i = ari.alloc(NT)
        bst = arf.alloc(NBLK); ble = arf.alloc(NBLK)
        cmpb = arf.alloc3(NBLK, 32)
        iog = arf.alloc(8)
        ixGf = arf.alloc3(NBLK, 8); ixDf = arf.alloc3(NBLK, 4)
        ixG = ari.alloc3(NBLK, 8); ixD = ari.alloc3(NBLK, 4)
        ld("sp", bst, c_blkstart[:, :], [], [Bg], part=True)
        ld("sp", iog, c_iotaG[:, :], [], [Bg], part=True)
        memset("dve", one32, 1.0, [Bg])
        mm(psf[0][:, 0:32], ones_b, Asb, True, True, [], [Bpsf[0]])
        cp("dve", cnt, psf[0][:, 0:32], [Bpsf[0]], [Bg])
        KC = S // BLK + 1
        cmp2 = cmpb.rearrange("p a b -> p (a b)")[:, 0:32 * KC].rearrange("p (e k) -> p e k", k=KC)
        tt("dve", cmp2, cnt.unsqueeze(2).to_broadcast([128, 32, KC]), bst[:, 0:KC].unsqueeze(1).to_broadcast([128, 32, KC]), ALU.is_gt, [Bg], [Bg])
        P.op("dve", lambda e: e.reduce_sum(out=pad, in_=cmp2, axis=AX.X), [Bg], [Bg])
        ts("dve", pad, pad, float(BLK), None, ALU.mult, None, [Bg], [Bg])
        P.op("dve", lambda e: e.tensor_tensor_scan(out=pend, data0=one32, data1=pad, initial=0.0, op0=ALU.mult, op1=ALU.add), [Bg], [Bg])
        tt("dve", pst, pend, pad, ALU.subtract, [Bg], [Bg])
        tt("dve", PR, RK, pst.unsqueeze(1).to_broadcast([128, NT, 32]), ALU.add, [Bg], [Bg])
        tt("dve", A1s, A1s, PR, ALU.mult, [Bg], [Bg])
        tt("dve", A2s, A2s, PR, ALU.mult, [Bg], [Bg])
        P.op("dve", lambda e: e.reduce_sum(out=D1f, in_=A1s, axis=AX.X), [Bg], [Bg])
        P.op("dve", lambda e: e.reduce_sum(out=D2f, in_=A2s, axis=AX.X), [Bg], [Bg])
        cp("dve", D1i, D1f, [Bg], [Bg])
        cp("dve", D2i, D2f, [Bg], [Bg])
        tt("dve", cmpb, pend.unsqueeze(1).to_broadcast([128, NBLK, 32]), bst.unsqueeze(2).to_broadcast([128, NBLK, 32]), ALU.is_le, [Bg], [Bg])
        P.op("dve", lambda e: e.reduce_sum(out=ble, in_=cmpb, axis=AX.X), [Bg], [Bg])
        ts("dve", ble, ble, float(NE - 1), None, ALU.min, None, [Bg], [Bg])
        ts("dve", bst, ble, 128.0, float(l * NE * 128), ALU.mult, ALU.add, [Bg], [Bg])
        tt("dve", ixGf[:, :, 0], bst, iog[:, 0:1].to_broadcast([128, NBLK]), ALU.add, [Bg], [Bg])
        cp("dve", ixG[:, :, 0], ixGf[:, :, 0], [Bg], [Bg])
        if "dest" in dbg:
            for nm_, ap_, n_ in (("d_cnt", cnt, 32), ("d_pad", pad, 32), ("d_pend", pend, 32), ("d_pst", pst, 32),
                                 ("d_A1", A1s.rearrange("p a b -> p (a b)"), NT * 32), ("d_RK", RK.rearrange("p a b -> p (a b)"), NT * 32),
                                 ("d_PR", PR.rearrange("p a b -> p (a b)"), NT * 32), ("d_W12", W12.rearrange("p a b -> p (a b)"), NT * 2)):
                dt_ = nc.dram_tensor(nm_, [128, n_], F32, kind="ExternalOutput")
                ld("sp", dt_[:, :], ap_, [Bg], [B("dd")])
            dd_ = nc.dram_tensor("dest", [128, 2 * NT + NBLK], F32, kind="ExternalOutput")
            ld("sp", dd_[:, 0:NT], D1f, [Bg], [B("dd")])
            ld("sp", dd_[:, NT:2 * NT], D2f, [Bg], [B("dd")])
            ld("sp", dd_[:, 2 * NT:], ble, [Bg], [B("dd")])

        BXB = B("XB")
        Bu2src = B("u2src")
        ut = [arb.alloc(D) for _ in range(2)]
        But = [B("ut") for _ in range(2)]
        for t in range(NT):
            j = t % 2
            ld("sp", ut[j], u2d[t * 128:(t + 1) * 128, :], [Bu2src], [But[j]])
            for Di in (D1i, D2i):
                P.dma("pool", (lambda src_, off_: (lambda e: e.indirect_dma_start(
                    out=XB[:, :], out_offset=bass.IndirectOffsetOnAxis(ap=off_, axis=0), in_=src_, in_offset=None)))(ut[j], Di[:, t:t + 1]),
                    [But[j], Bg], [BXB], part=True, sem=But[j])
        P.barrier()

        NA = BLK // 128
        xb = [arb.alloc3(NA, D) for _ in range(2)]
        Bxb = [B("xb") for _ in range(2)]
        xbT = [arb.alloc3(8, BLK) for _ in range(2)]
        BxbT = [B("xbT") for _ in range(2)]
        NWB = 2
        Wg = [arb.alloc3(8, DEXP) for _ in range(NWB)]
        Wu = [arb.alloc3(8, DEXP) for _ in range(NWB)]
        Wd = [arb.alloc3(4, D) for _ in range(NWB)]
        BWg = [B("Wg") for _ in range(NWB)]
        BWu = [B("Wu") for _ in range(NWB)]
        BWd = [B("Wd") for _ in range(NWB)]
        sgf = [arf.alloc(BLK) for _ in range(2)]
        Bsgf = [B("sgf") for _ in range(2)]
        hT = [arb.alloc3(4, BLK) for _ in range(2)]
        BhT = [B("hT") for _ in range(2)]
        yst = [arf.alloc(D) for _ in range(2)]
        Byst = [B("yst") for _ in range(2)]
        BYB = B("YB")

        def gath(dst, src2d, off):
            return lambda e: e.indirect_dma_start(out=dst, out_offset=None, in_=src2d, in_offset=bass.IndirectOffsetOnAxis(ap=off, axis=0))

        def load_x(b):
            i = b % 2
            ld("sp", xb[i], XB[b * BLK:(b + 1) * BLK, :].rearrange("(a p) d -> p a d", p=128), [BXB], [Bxb[i]])

        def load_w(b):
            w_ = b % NWB
            P.dma("pool", gath(Wg[w_].rearrange("p a b -> p (a b)"), w_ge[:, :], ixG[:, b, 0:1]), [Bg], [BWg[w_]])
            P.dma("pool", gath(Wu[w_].rearrange("p a b -> p (a b)"), w_ue[:, :], ixG[:, b, 0:1]), [Bg], [BWu[w_]])
            P.dma("pool", gath(Wd[w_].rearrange("p a b -> p (a b)"), w_de[:, :], ixG[:, b, 0:1]), [Bg], [BWd[w_]])

        load_x(0)
        for b0 in range(min(NWB - 1, NBLK)):
            load_w(b0)
        ysi = 0
        for b in range(NBLK):
            i = b % 2
            wi = b % NWB
            if b + 1 < NBLK:
                load_x(b + 1)
            if b + NWB - 1 < NBLK:
                load_w(b + NWB - 1)
            for a in range(NA):
                for kc in range(8):
                    tr(psb[a % 2][:, kc * 128:(kc + 1) * 128], xb[i][:, a, kc * 128:(kc + 1) * 128], ident_b, [Bxb[i]], [Bpsb[a % 2]])
                cp("act" if a % 2 else "dve", xbT[i][:, :, a * 128:(a + 1) * 128], psb[a % 2].rearrange("p (k s) -> p k s", s=128), [Bpsb[a % 2]], [BxbT[i]])
            for hc in range(4):
                pg, pu = (2 * hc) % 4, (2 * hc + 1) % 4
                for kc in range(8):
                    mm(psf[pg][:, 0:BLK], Wg[wi][:, kc, hc * 128:(hc + 1) * 128], xbT[i][:, kc, :], kc == 0, kc == 7, [BWg[wi], BxbT[i]], [Bpsf[pg]])
                for kc in range(8):
                    mm(psf[pu][:, 0:BLK], Wu[wi][:, kc, hc * 128:(hc + 1) * 128], xbT[i][:, kc, :], kc == 0, kc == 7, [BWu[wi], BxbT[i]], [Bpsf[pu]])
                si_ = hc % 2
                act(sgf[si_], psf[pg][:, 0:BLK], AF.Silu, [Bpsf[pg]], [Bsgf[si_]])
                tt("dve", hT[i][:, hc, :], psf[pu][:, 0:BLK], sgf[si_], ALU.mult, [Bpsf[pu], Bsgf[si_]], [BhT[i]])
            for a in range(NA):
                ys = yst[ysi % 2]
                Bys = Byst[ysi % 2]
                ysi += 1
                for half in range(2):
                    py = 4 + half
                    for hc in range(4):
                        mm(psf[py], hT[i][:, hc, a * 128:(a + 1) * 128], Wd[wi][:, hc, half * 512:(half + 1) * 512], hc == 0, hc == 3,
                           [BhT[i], BWd[wi]], [Bpsf[py]])
                    cp("act" if half else "dve", ys[:, half * 512:(half + 1) * 512], psf[py], [Bpsf[py]], [Bys])
                r0 = b * BLK + a * 128
                ld("sp", YB[r0:r0 + 128, :], ys, [Bys], [BYB], part=True, sem=Bys)
        P.barrier()

        Bbc = B("bc2")
        bc = {}
        for nm, src in (("g2", adaD[l:l + 1, 5120:6144]), ("l2g", ln2_g[l:l + 1, :]), ("l2b", ln2_b[l:l + 1, :])):
            bc[nm] = arf.alloc(D)
            ld("sp", bc[nm], src.partition_broadcast(128), [], [Bbc], part=True)
        y1 = [arf.alloc(D) for _ in range(2)]
        y2 = [arf.alloc(D) for _ in range(2)]
        By1 = [B("y1") for _ in range(2)]
        By2 = [B("y2") for _ in range(2)]
        xt = [arf.alloc(D) for _ in range(2)]
        Bxt = [B("xt2") for _ in range(2)]
        tmp = [arf.alloc(16) for _ in range(2)]
        Btmp = [B("tmp2") for _ in range(2)]
        Bxn = B("xnext")
        Bxs = B("xmid_src")

        def load_t(t):
            j = t % 2
            ld("sp", xt[j], xmid[t * 128:(t + 1) * 128, :], [Bxs], [Bxt[j]])
            P.dma("pool", gath(y1[j], YB[:, :], D1i[:, t:t + 1]), [BYB, Bg], [By1[j]])
            P.dma("pool", gath(y2[j], YB[:, :], D2i[:, t:t + 1]), [BYB, Bg], [By2[j]])

        load_t(0)
        for t in range(NT):
            j = t % 2
            if t + 1 < NT:
                load_t(t + 1)
            ts("dve", y1[j], y1[j], W12[:, t, 0:1], None, ALU.mult, None, [By1[j]], [By1[j]])
            stt("dve", y1[j], y2[j], W12[:, t, 1:2], y1[j], ALU.mult, ALU.add, [By2[j], By1[j]], [By1[j]])
            tt("pool", y1[j], y1[j], bc["g2"], ALU.mult, [By1[j], Bbc], [By1[j]])
            stt("dve", y1[j], xt[j], ALPHA, y1[j], ALU.mult, ALU.add, [Bxt[j], By1[j]], [By1[j]])
            rstd, nmr = ln_stats(y1[j], By1[j], tmp[j], Btmp[j])
            act(y1[j], y1[j], AF.Identity, [By1[j], Btmp[j]], [By1[j]], bias=nmr, scale=rstd)
            tt("pool", y1[j], y1[j], bc["l2g"], ALU.mult, [By1[j], Bbc], [By1[j]])
            tt("dve", xt[j], y1[j], bc["l2b"], ALU.add, [By1[j], Bbc], [Bxt[j]])
            ld("sp", xnext[t * 128:(t + 1) * 128, :], xt[j], [Bxt[j]], [Bxn], part=True, sem=Bxt[j])
        arf.pop(); arb.pop()

    phase_ada()
    phase_reset()
    if phases == "all":
        xcur = x_in
        for l in range(L):
            phase_proj(l, xcur)
            phase_reset()
            phase_attn(l)
            phase_reset()
            phase_gla(l)
            phase_reset()
            phase_merge(l, xcur, xB)
            phase_reset()
            phase_moe(l, xB, out if l == L - 1 else xA)
            phase_reset()
            xcur = xA
    else:
        if "proj" in phases:
            phase_proj(0, x_in)
            phase_reset()
        if "attn" in phases:
            phase_attn(0)
            phase_reset()
        if "gla" in phases:
            phase_gla(0)
            phase_reset()
        if "merge" in phases:
            phase_merge(0, x_in, xB)
            phase_reset()
        if "moe" in phases:
            phase_moe(0, xB, out)
            phase_reset()
    P.emit(nc, st)
    st.close()
    nc._prog = P
    return nc


def make_in_map(inp, b, S):
    f = lambda a: np.ascontiguousarray(np.asarray(a, dtype=np.float32))
    m = {}
    m["x"] = f(inp["x"][b, :S])
    m["cT"] = f(np.asarray(inp["c"][b]).reshape(8, 128).T)
    for k_ in ("w_ada", "b_ada", "w_in", "w_alpha", "diff_norm_g", "gla_norm_g", "w_branch_a", "w_branch_b",
               "w_out", "ln1_g", "ln1_b", "ln2_g", "ln2_b"):
        m[k_] = f(inp[k_])
    m["b_gatesT"] = f(np.asarray(inp["b_gates"]).reshape(DEPTH, 16, 128).transpose(0, 2, 1))
    m["b_alphaT"] = f(np.asarray(inp["b_alpha"]).reshape(DEPTH, 4, 128).transpose(0, 2, 1))
    m["lam4"] = f(np.stack([np.asarray(inp[k_]) for k_ in ("lambda_q1", "lambda_k1", "lambda_q2", "lambda_k2")], axis=1))
    m["w_router"] = f(np.concatenate([np.asarray(inp["w_router_g"]), np.asarray(inp["w_router_e"])], axis=2))
    m["b_router"] = f(np.concatenate([np.asarray(inp["b_router_g"]), np.asarray(inp["b_router_e"])], axis=1))
    m["w_gate_e"] = f(np.asarray(inp["w_gate_e"]).reshape(DEPTH, NE, 8, 128, DEXP).transpose(0, 1, 3, 2, 4)).reshape(DEPTH * NE * 128, 8 * DEXP)
    m["w_up_e"] = f(np.asarray(inp["w_up_e"]).reshape(DEPTH, NE, 8, 128, DEXP).transpose(0, 1, 3, 2, 4)).reshape(DEPTH * NE * 128, 8 * DEXP)
    m["w_down_e"] = f(np.asarray(inp["w_down_e"]).reshape(DEPTH, NE, 4, 128, D).transpose(0, 1, 3, 2, 4)).reshape(DEPTH * NE * 128, 4 * D)
    m.update(host_consts(S))
    m["iota_p"] = np.arange(128, dtype=np.float32).reshape(128, 1)
    nslot = ((2 * S + NE * (BLK - 1)) // BLK + 1) * BLK
    nblk = nslot // BLK
    m["blkstart"] = np.broadcast_to((np.arange(nblk, dtype=np.float32) * BLK)[None, :], (128, nblk)).copy()
    m["iotaG"] = (np.arange(8, dtype=np.float32)[None, :] * 128 + np.arange(128, dtype=np.float32)[:, None]).copy()
    return m


_NC_CACHE = {}


def kernel(**inputs):
    S = 4096
    nb = 8
    if "nc" not in _NC_CACHE:
        _NC_CACHE["nc"] = build(S, DEPTH)
    nc = _NC_CACHE["nc"]
    inp = {k_: np.asarray(v) for k_, v in inputs.items()}
    shared = make_in_map(inp, 0, S)
    in_maps = []
    for b in range(nb):
        m = dict(shared)
        m["x"] = np.ascontiguousarray(inp["x"][b], dtype=np.float32)
        m["cT"] = np.ascontiguousarray(inp["c"][b].reshape(8, 128).T, dtype=np.float32)
        in_maps.append(m)
    res = run_bass_kernel_spmd(nc, in_maps, core_ids=list(range(nb)))
    return np.stack([np.asarray(r["out"], dtype=np.float32) for r in res.results], axis=0)
```
